# Optimizing a Trainium2 kernel written in Bass

```python
import jax, jax.numpy as jnp
from jax import lax
import numpy as np

D_MODEL = 2048
BATCH = 2
SEQ = 8192
DEPTH = 2

EPS = 1e-6
N_MEM = 256
D_MIX = D_MODEL
D_ATT = D_MIX // 2
D_RNN = D_MIX - D_ATT
N_ATT_HEADS = 8
QK_NOPE_DIM = 128
QK_ROPE_DIM = 64
QK_HEAD_DIM = QK_NOPE_DIM + QK_ROPE_DIM
V_HEAD_DIM = D_ATT // N_ATT_HEADS
Q_LORA_RANK = 512
KV_LORA_RANK = 256
ROPE_BASE = 10000.0
Q_BLOCK = 128
N_RNN_BLOCKS = 8
RNN_BLOCK = D_RNN // N_RNN_BLOCKS
CONV_WIDTH = 4
LRU_C = 8.0
N_MEM_HEADS = 4
MEM_HEAD_DIM = 128
D_MEM_ATT = N_MEM_HEADS * MEM_HEAD_DIM
N_GROUPS = 4
EXPERTS_PER_GROUP = 8
N_EXPERTS = N_GROUPS * EXPERTS_PER_GROUP
TOP_K = 2
D_EXPERT = 512
MOE_BLOCK = 128
D_IN_PROJ = Q_LORA_RANK + KV_LORA_RANK + QK_ROPE_DIM + 2 * D_RNN
IN_SPLITS = (Q_LORA_RANK,
             Q_LORA_RANK + KV_LORA_RANK,
             Q_LORA_RANK + KV_LORA_RANK + QK_ROPE_DIM,
             Q_LORA_RANK + KV_LORA_RANK + QK_ROPE_DIM + D_RNN)

kernel_name = 'hybrid_mla_rglru_memxattn_hmoe'


def rms_norm(x, g):
    xf = x.astype(jnp.float32)
    y = xf * lax.rsqrt(jnp.mean(xf * xf, axis=-1, keepdims=True) + EPS)
    return (y * g.astype(jnp.float32)).astype(x.dtype)


def rope_tables(seq):
    inv = 1.0 / (ROPE_BASE ** (jnp.arange(0, QK_ROPE_DIM, 2, dtype=jnp.float32) / QK_ROPE_DIM))
    ang = jnp.arange(seq, dtype=jnp.float32)[:, None] * inv[None, :]
    return jnp.cos(ang), jnp.sin(ang)


def apply_rope(x, cos, sin):
    x1, x2 = jnp.split(x, 2, axis=-1)
    c = cos[None, :, None, :]
    s = sin[None, :, None, :]
    return jnp.concatenate([x1 * c - x2 * s, x1 * s + x2 * c], axis=-1).astype(x.dtype)


def causal_block_attention(q, k, v):
    b, s, h, dq = q.shape
    nb = s // Q_BLOCK
    scale = dq ** -0.5
    qb = q.reshape(b, nb, Q_BLOCK, h, dq).transpose(1, 0, 2, 3, 4)
    kpos = jnp.arange(s)

    def one_block(args):
        qi, i = args
        sc = jnp.einsum('bqhd,bkhd->bhqk', qi, k, preferred_element_type=jnp.float32) * scale
        qpos = i * Q_BLOCK + jnp.arange(Q_BLOCK)
        mask = kpos[None, :] <= qpos[:, None]
        sc = jnp.where(mask[None, None], sc, -jnp.inf)
        p = jax.nn.softmax(sc, axis=-1).astype(v.dtype)
        return jnp.einsum('bhqk,bkhd->bqhd', p, v)

    out = lax.map(one_block, (qb, jnp.arange(nb)))
    return out.transpose(1, 0, 2, 3, 4).reshape(b, s, h, v.shape[-1])


def mla_group(c_q, c_kv, k_pe, q_lora_g, kv_lora_g, w_uq, w_ukv, q_head_g, k_head_g, cos, sin):
    b, s, _ = c_q.shape
    q = (rms_norm(c_q, q_lora_g) @ w_uq).reshape(b, s, N_ATT_HEADS, QK_HEAD_DIM)
    kv = (rms_norm(c_kv, kv_lora_g) @ w_ukv).reshape(b, s, N_ATT_HEADS, QK_NOPE_DIM + V_HEAD_DIM)
    k_nope, v = kv[..., :QK_NOPE_DIM], kv[..., QK_NOPE_DIM:]
    k_rope = jnp.broadcast_to(k_pe[:, :, None, :], (b, s, N_ATT_HEADS, QK_ROPE_DIM))
    k = jnp.concatenate([k_nope, k_rope], axis=-1)
    q = rms_norm(q, q_head_g)
    k = rms_norm(k, k_head_g)
    q = jnp.concatenate([q[..., :QK_NOPE_DIM], apply_rope(q[..., QK_NOPE_DIM:], cos, sin)], axis=-1)
    k = jnp.concatenate([k[..., :QK_NOPE_DIM], apply_rope(k[..., QK_NOPE_DIM:], cos, sin)], axis=-1)
    return causal_block_attention(q, k, v).reshape(b, s, D_ATT)


def rglru_group(xr, gate, conv_w, conv_b, w_rg, b_rg, w_ig, b_ig, lam):
    b, s, _ = xr.shape
    xp = jnp.pad(xr, ((0, 0), (CONV_WIDTH - 1, 0), (0, 0)))
    xc = conv_b
    for j in range(CONV_WIDTH):
        xc = xc + xp[:, j:j + s] * conv_w[j]
    xblk = xc.reshape(b, s, N_RNN_BLOCKS, RNN_BLOCK)
    r = jax.nn.sigmoid(jnp.einsum('bsnc,ncd->bsnd', xblk, w_rg).reshape(b, s, D_RNN) + b_rg)
    i = jax.nn.sigmoid(jnp.einsum('bsnc,ncd->bsnd', xblk, w_ig).reshape(b, s, D_RNN) + b_ig)
    log_a = (-LRU_C * r.astype(jnp.float32)) * jax.nn.softplus(-lam.astype(jnp.float32))
    a = jnp.exp(log_a)
    u = jnp.sqrt(-jnp.expm1(2.0 * log_a)) * (i * xc).astype(jnp.float32)

    def combine(left, right):
        a1, b1 = left
        a2, b2 = right
        return a1 * a2, a2 * b1 + b2

    _, h = lax.associative_scan(combine, (a, u), axis=1)
    return h.astype(xr.dtype) * jax.nn.gelu(gate)


def memory_xattn(x, mem, norm_g, mem_norm_g, w_q, w_k, w_v, q_g, k_g, w_o):
    b, s, _ = x.shape
    m_len = mem.shape[1]
    h = rms_norm(x, norm_g)
    m = rms_norm(mem, mem_norm_g)
    q = rms_norm((h @ w_q).reshape(b, s, N_MEM_HEADS, MEM_HEAD_DIM), q_g)
    k = rms_norm((m @ w_k).reshape(b, m_len, N_MEM_HEADS, MEM_HEAD_DIM), k_g)
    v = (m @ w_v).reshape(b, m_len, N_MEM_HEADS, MEM_HEAD_DIM)
    sc = jnp.einsum('bshd,bmhd->bhsm', q, k, preferred_element_type=jnp.float32) * (MEM_HEAD_DIM ** -0.5)
    p = jax.nn.softmax(sc, axis=-1).astype(v.dtype)
    o = jnp.einsum('bhsm,bmhd->bshd', p, v).reshape(b, s, D_MEM_ATT)
    return o @ w_o


def hierarchical_moe(x, norm_g, w_rgrp, b_rgrp, w_rexp, b_rexp, w_gate, w_up, w_down):
    b, s, d = x.shape
    t = b * s
    h = rms_norm(x, norm_g).reshape(t, d)
    g_prob = jax.nn.softmax((h @ w_rgrp).astype(jnp.float32) + b_rgrp, axis=-1)
    g_w, g_idx = lax.top_k(g_prob, 1)
    e_logits = ((h @ w_rexp).astype(jnp.float32) + b_rexp).reshape(t, N_GROUPS, EXPERTS_PER_GROUP)
    sel = jnp.broadcast_to(g_idx[:, :, None], (t, 1, EXPERTS_PER_GROUP))
    e_prob = jax.nn.softmax(jnp.take_along_axis(e_logits, sel, axis=1)[:, 0], axis=-1)
    e_w, e_loc = lax.top_k(e_prob, TOP_K)
    e_w = e_w / jnp.sum(e_w, axis=-1, keepdims=True)
    weights = g_w * e_w
    eid = g_idx * EXPERTS_PER_GROUP + e_loc
    n_asg = t * TOP_K
    eid_f = eid.reshape(n_asg)
    w_f = weights.reshape(n_asg)
    tok_f = jnp.repeat(jnp.arange(t, dtype=jnp.int32), TOP_K)
    order = jnp.argsort(eid_f)
    e_sorted = eid_f[order]
    counts = jnp.bincount(eid_f, length=N_EXPERTS)
    padded = (counts + MOE_BLOCK - 1) // MOE_BLOCK * MOE_BLOCK
    pad_end = jnp.cumsum(padded)
    pad_start = pad_end - padded
    start = jnp.cumsum(counts) - counts
    dest = pad_start[e_sorted] + jnp.arange(n_asg) - start[e_sorted]
    n_rows = ((n_asg + MOE_BLOCK - 1) // MOE_BLOCK + N_EXPERTS) * MOE_BLOCK
    n_blk = n_rows // MOE_BLOCK
    buf_tok = jnp.zeros((n_rows,), jnp.int32).at[dest].set(tok_f[order])
    buf_w = jnp.zeros((n_rows,), jnp.float32).at[dest].set(w_f[order])
    blk_e = jnp.minimum(jnp.searchsorted(pad_end, jnp.arange(n_blk) * MOE_BLOCK, side='right'), N_EXPERTS - 1)
    xb = h[buf_tok].reshape(n_blk, MOE_BLOCK, d)

    def expert_block(args):
        xi, e = args
        return (jax.nn.silu(xi @ w_gate[e]) * (xi @ w_up[e])) @ w_down[e]

    yb = lax.map(expert_block, (xb, blk_e)).reshape(n_rows, d)
    y = jax.ops.segment_sum(yb * buf_w[:, None].astype(yb.dtype), buf_tok, num_segments=t)
    return y.reshape(b, s, d)


def _normal(k, shape, scale):
    return jax.random.normal(k, shape, jnp.float32) * scale


def _gain(k, shape):
    return 1.0 + 0.02 * jax.random.normal(k, shape, jnp.float32)


def setup_inputs(seed: int = 0) -> dict:
    key = jax.random.key(seed)
    ks = jax.random.split(key, 40)
    L = DEPTH
    u = jax.random.uniform(ks[20], (L, D_RNN), jnp.float32, 0.9, 0.999)
    a0 = u ** (1.0 / LRU_C)
    lam = jnp.log(a0) - jnp.log1p(-a0)
    return {
        'x': _normal(ks[0], (BATCH, SEQ, D_MODEL), 1.0),
        'mem': _normal(ks[1], (BATCH, N_MEM, D_MODEL), 1.0),
        'mix_norm_g': _gain(ks[2], (L, D_MODEL)),
        'w_in': _normal(ks[3], (L, D_MODEL, D_IN_PROJ), D_MODEL ** -0.5),
        'q_lora_norm_g': _gain(ks[4], (L, Q_LORA_RANK)),
        'kv_lora_norm_g': _gain(ks[5], (L, KV_LORA_RANK)),
        'w_uq': _normal(ks[6], (L, Q_LORA_RANK, N_ATT_HEADS * QK_HEAD_DIM), Q_LORA_RANK ** -0.5),
        'w_ukv': _normal(ks[7], (L, KV_LORA_RANK, N_ATT_HEADS * (QK_NOPE_DIM + V_HEAD_DIM)), KV_LORA_RANK ** -0.5),
        'att_q_norm_g': _gain(ks[8], (L, QK_HEAD_DIM)),
        'att_k_norm_g': _gain(ks[9], (L, QK_HEAD_DIM)),
        'conv_w': _normal(ks[10], (L, CONV_WIDTH, D_RNN), CONV_WIDTH ** -0.5),
        'conv_b': _normal(ks[11], (L, D_RNN), 0.01),
        'w_rgate': _normal(ks[12], (L, N_RNN_BLOCKS, RNN_BLOCK, RNN_BLOCK), RNN_BLOCK ** -0.5),
        'b_rgate': _normal(ks[13], (L, D_RNN), 0.01),
        'w_igate': _normal(ks[14], (L, N_RNN_BLOCKS, RNN_BLOCK, RNN_BLOCK), RNN_BLOCK ** -0.5),
        'b_igate': _normal(ks[15], (L, D_RNN), 0.01),
        'lru_lambda': lam,
        'att_out_norm_g': _gain(ks[16], (L, D_ATT)),
        'rnn_out_norm_g': _gain(ks[17], (L, D_RNN)),
        'w_out': _normal(ks[18], (L, D_MIX, D_MODEL), D_MIX ** -0.5),
        'xattn_norm_g': _gain(ks[19], (L, D_MODEL)),
        'mem_norm_g': _gain(ks[21], (L, D_MODEL)),
        'w_mq': _normal(ks[22], (L, D_MODEL, D_MEM_ATT), D_MODEL ** -0.5),
        'w_mk': _normal(ks[23], (L, D_MODEL, D_MEM_ATT), D_MODEL ** -0.5),
        'w_mv': _normal(ks[24], (L, D_MODEL, D_MEM_ATT), D_MODEL ** -0.5),
        'mem_q_norm_g': _gain(ks[25], (L, MEM_HEAD_DIM)),
        'mem_k_norm_g': _gain(ks[26], (L, MEM_HEAD_DIM)),
        'w_mo': _normal(ks[27], (L, D_MEM_ATT, D_MODEL), D_MEM_ATT ** -0.5),
        'moe_norm_g': _gain(ks[28], (L, D_MODEL)),
        'w_router_group': _normal(ks[29], (L, D_MODEL, N_GROUPS), D_MODEL ** -0.5),
        'b_router_group': _normal(ks[30], (L, N_GROUPS), 0.01),
        'w_router_expert': _normal(ks[31], (L, D_MODEL, N_EXPERTS), D_MODEL ** -0.5),
        'b_router_expert': _normal(ks[32], (L, N_EXPERTS), 0.01),
        'w_exp_gate': _normal(ks[33], (L, N_EXPERTS, D_MODEL, D_EXPERT), D_MODEL ** -0.5),
        'w_exp_up': _normal(ks[34], (L, N_EXPERTS, D_MODEL, D_EXPERT), D_MODEL ** -0.5),
        'w_exp_down': _normal(ks[35], (L, N_EXPERTS, D_EXPERT, D_MODEL), D_EXPERT ** -0.5),
    }


def reference(x, mem, mix_norm_g, w_in, q_lora_norm_g, kv_lora_norm_g, w_uq, w_ukv,
              att_q_norm_g, att_k_norm_g, conv_w, conv_b, w_rgate, b_rgate, w_igate, b_igate,
              lru_lambda, att_out_norm_g, rnn_out_norm_g, w_out, xattn_norm_g, mem_norm_g,
              w_mq, w_mk, w_mv, mem_q_norm_g, mem_k_norm_g, w_mo, moe_norm_g,
              w_router_group, b_router_group, w_router_expert, b_router_expert,
              w_exp_gate, w_exp_up, w_exp_down):
    cos, sin = rope_tables(x.shape[1])
    for l in range(DEPTH):
        z = rms_norm(x, mix_norm_g[l]) @ w_in[l]
        c_q, c_kv, k_pe, xr, gate = jnp.split(z, IN_SPLITS, axis=-1)
        k_pe = apply_rope(rms_norm(k_pe[:, :, None, :], att_k_norm_g[l][QK_NOPE_DIM:]), cos, sin)[:, :, 0, :] * 0 + k_pe if False else k_pe
        y_att = mla_group(c_q, c_kv, k_pe, q_lora_norm_g[l], kv_lora_norm_g[l], w_uq[l], w_ukv[l],
                          att_q_norm_g[l], att_k_norm_g[l], cos, sin)
        y_rnn = rglru_group(xr, gate, conv_w[l], conv_b[l], w_rgate[l], b_rgate[l],
                            w_igate[l], b_igate[l], lru_lambda[l])
        y_mix = jnp.concatenate([rms_norm(y_att, att_out_norm_g[l]),
                                 rms_norm(y_rnn, rnn_out_norm_g[l])], axis=-1)
        x = x + y_mix @ w_out[l]
        x = x + memory_xattn(x, mem, xattn_norm_g[l], mem_norm_g[l], w_mq[l], w_mk[l], w_mv[l],
                             mem_q_norm_g[l], mem_k_norm_g[l], w_mo[l])
        x = x + hierarchical_moe(x, moe_norm_g[l], w_router_group[l], b_router_group[l],
                                 w_router_expert[l], b_router_expert[l],
                                 w_exp_gate[l], w_exp_up[l], w_exp_down[l])
    return x
```

```python
import numpy as np
from contextlib import ExitStack
import concourse.bass as bass
import concourse.mybir as mybir
from concourse.bass_utils import run_bass_kernel_spmd

F32 = mybir.dt.float32
BF16 = mybir.dt.bfloat16
AF = mybir.ActivationFunctionType
ALU = mybir.AluOpType
AX = mybir.AxisListType
I32 = mybir.dt.int32
CAP = 640

EPS = 1e-6
D = 2048
NCORES = 8
CPB = 4
DIN = 2880
NH = 8
NEXP = 32
DEXP = 512
NMEM = 256


class Buf:
    def __init__(self, t):
        self.t = t
        self.w = None
        self.r = []

    def __getitem__(self, k):
        return self.t[k]


class Sched:
    SAME_ENGINE_SYNC = True
    NDMA = 12

    def __init__(self, nc, es):
        self.nc = nc
        self.es = es
        self.eng = {'pe': nc.tensor, 'act': nc.scalar, 'dve': nc.vector,
                    'pool': nc.gpsimd, 'sp': nc.sync}
        self.sem = {}
        self.cnt = {}
        for e in ('pe', 'act', 'dve', 'pool'):
            self.sem[e] = es.enter_context(nc.semaphore('s_' + e))
            self.cnt[e] = 0
        self.dq = {}
        for q in ('sp', 'pool', 'act'):
            lst = []
            for i in range(self.NDMA):
                k = ('d', q, i)
                self.sem[k] = es.enter_context(nc.semaphore('d_%s_%d' % (q, i)))
                self.cnt[k] = 0
                lst.append(k)
            self.dq[q] = [lst, 0]
        self.seen = {e: {} for e in self.eng}
        self.n_ins = 0
        self.uid = 0

    def sbuf(self, es, name, shape, dt):
        self.uid += 1
        return Buf(es.enter_context(self.nc.sbuf_tensor('%s_%d' % (name, self.uid), list(shape), dt)))

    def psum(self, es, name, shape, dt=F32):
        self.uid += 1
        return Buf(es.enter_context(self.nc.psum_tensor('%s_%d' % (name, self.uid), list(shape), dt)))

    def _deps(self, e, reads, writes):
        deps = {}

        def add(tok):
            if tok is None:
                return
            k, v = tok
            if deps.get(k, 0) < v:
                deps[k] = v
        for b in reads:
            add(b.w)
        for b in writes:
            if b.w is not None and b.w[0] != e:
                add(b.w)
            for t in b.r:
                if t[0] != e:
                    add(t)
        eng = self.eng[e]
        for k, v in deps.items():
            if k == e and (e == 'pe' or not self.SAME_ENGINE_SYNC):
                continue
            if self.seen[e].get(k, 0) >= v:
                continue
            eng.wait_ge(self.sem[k], v)
            self.seen[e][k] = v

    def _post(self, tok, reads, writes):
        for b in reads:
            b.r.append(tok)
            if len(b.r) > 48:
                d = {}
                for k, v in b.r:
                    if d.get(k, 0) < v:
                        d[k] = v
                b.r = list(d.items())
        for b in writes:
            b.w = tok
            b.r = []

    def op(self, e, name, R=(), W=(), **kw):
        self._deps(e, R, W)
        ins = getattr(self.eng[e], name)(**kw)
        self.cnt[e] += 1
        ins.then_inc(self.sem[e], 1)
        tok = (e, self.cnt[e])
        self._post(tok, R, W)
        self.n_ins += 1
        return tok

    def dve(self, name, R=(), W=(), **kw):
        return self.op('dve', name, R, W, **kw)

    def act(self, R=(), W=(), **kw):
        return self.op('act', 'activation', R, W, **kw)

    def pe(self, name, R=(), W=(), **kw):
        return self.op('pe', name, R, W, **kw)

    def pool(self, name, R=(), W=(), **kw):
        return self.op('pool', name, R, W, **kw)

    def dma(self, q, out, in_, R=(), W=(), **kw):
        self._deps(q, R, W)
        lst, i = self.dq[q]
        k = lst[i % len(lst)]
        self.dq[q][1] = i + 1
        ins = self.eng[q].dma_start(out=out, in_=in_, **kw)
        self.cnt[k] += 16
        ins.then_inc(self.sem[k], 16)
        tok = (k, self.cnt[k])
        self._post(tok, R, W)
        self.n_ins += 1
        return tok

    def indirect(self, R=(), W=(), **kw):
        q = 'pool'
        self._deps(q, R, W)
        lst, i = self.dq[q]
        k = lst[i % len(lst)]
        self.dq[q][1] = i + 1
        ins = self.eng[q].indirect_dma_start(**kw)
        self.cnt[k] += 16
        ins.then_inc(self.sem[k], 16)
        tok = (k, self.cnt[k])
        self._post(tok, R, W)
        self.n_ins += 1
        return tok

    def barrier(self):
        for e, eng in self.eng.items():
            for k, v in self.cnt.items():
                if v == 0 or k == e and e == 'pe':
                    continue
                if self.seen[e].get(k, 0) >= v:
                    continue
                eng.wait_ge(self.sem[k], v)
                self.seen[e][k] = v


def bc(ap, shape):
    return ap.to_broadcast(list(shape))


def rstd_calc(S, ss, out, inv_n, post=1.0):
    sb, sap = ss
    ob, oap = out
    S.dve('tensor_scalar', R=[sb], W=[ob], out=oap, in0=sap, scalar1=inv_n, scalar2=EPS,
          op0=ALU.mult, op1=ALU.add)
    S.act(R=[ob], W=[ob], out=oap, in_=oap, func=AF.Sqrt)
    S.dve('reciprocal', R=[ob], W=[ob], out=oap, in_=oap)
    if post != 1.0:
        S.dve('tensor_scalar', R=[ob], W=[ob], out=oap, in0=oap, scalar1=post, scalar2=None, op0=ALU.mult)


def load_consts(S, es, io):
    c = {}
    c['idf'] = S.sbuf(es, 'idf', [128, 128], F32)
    c['idb'] = S.sbuf(es, 'idb', [128, 128], BF16)
    S.dma('sp', c['idf'][:], io['ident'], W=[c['idf']])
    S.dve('tensor_copy', R=[c['idf']], W=[c['idb']], out=c['idb'][:], in_=c['idf'][:])
    return c


def norm_T(S, xt, xnT_dst, dstbuf, wk, c):
    junk, ss, rstd, xn, tp = wk['junk'], wk['ss'], wk['rstd'], wk['xn'], wk['tp']
    S.act(R=[xt], W=[junk, ss], out=junk[:, 0:D], in_=xt[:], func=AF.Square, accum_out=ss[:, 0:1])
    rstd_calc(S, (ss, ss[:, 0:1]), (rstd, rstd[:, 0:1]), 1.0 / D)
    S.act(R=[xt, rstd], W=[xn], out=xn[:, 0:1024], in_=xt[:, 0:1024], func=AF.Copy, scale=rstd[:, 0:1])
    S.dve('tensor_scalar', R=[xt, rstd], W=[xn], out=xn[:, 1024:2048], in0=xt[:, 1024:2048],
          scalar1=rstd[:, 0:1], scalar2=None, op0=ALU.mult)
    for half in range(2):
        t = tp[half]
        for k in range(8):
            kk = half * 8 + k
            S.pe('transpose', R=[xn, c['idb']], W=[t], out=t[:, k * 128:(k + 1) * 128],
                 in_=xn[:, kk * 128:(kk + 1) * 128], identity=c['idb'][:])
        src = t[:, :].rearrange('p (k t) -> p k t', k=8)
        if half == 0:
            S.act(R=[t], W=[dstbuf], out=xnT_dst[:, 0:8, :], in_=src, func=AF.Copy)
        else:
            S.dve('tensor_copy', R=[t], W=[dstbuf], out=xnT_dst[:, 8:16, :], in_=src)


def load_w_scaled(S, dst, dst_ap_fn, w_dram, g, nk, c0, c1, stg, q='sp'):
    for k in range(nk):
        st = stg[k % len(stg)]
        n = c1 - c0
        S.dma(q, st[:, 0:n], w_dram[k * 128:(k + 1) * 128, c0:c1], W=[st])
        if k % 2 == 0:
            S.dve('tensor_scalar', R=[st, g], W=[dst], out=dst_ap_fn(k), in0=st[:, 0:n],
                  scalar1=g[:, k:k + 1], scalar2=None, op0=ALU.mult)
        else:
            S.act(R=[st, g], W=[dst], out=dst_ap_fn(k), in_=st[:, 0:n], func=AF.Copy, scale=g[:, k:k + 1])


def emit_A(S, nc, T, io):
    NT = T // 128
    G = min(512, T)
    NG = T // G
    with ExitStack() as es:
        c = load_consts(S, es, io)
        xnT_all = S.sbuf(es, 'xnT_all', [128, 16, T], BF16)
        with ExitStack() as e1:
            w1 = S.sbuf(e1, 'w1', [128, 16, 832], BF16)
            wuq = S.sbuf(e1, 'wuq', [128, 4, 1536], BF16)
            wukv = S.sbuf(e1, 'wukv', [128, 2, 2048], BF16)
            stg = [S.sbuf(e1, 'stg', [128, 2048], F32) for _ in range(2)]
            g_mix = S.sbuf(e1, 'g_mix', [128, 16], F32)
            g_q = S.sbuf(e1, 'g_q', [128, 4], F32)
            g_kv = S.sbuf(e1, 'g_kv', [128, 2], F32)
            gqh = S.sbuf(e1, 'gqh', [128, 192], F32)
            gkh = S.sbuf(e1, 'gkh', [128, 192], F32)
            S.dma('sp', g_mix[:], io['g_mix'], W=[g_mix])
            S.dma('sp', g_q[:], io['g_q'], W=[g_q])
            S.dma('sp', g_kv[:], io['g_kv'], W=[g_kv])
            S.dma('sp', gqh[:], io['gqh'].partition_broadcast(128), W=[gqh])
            S.dma('sp', gkh[:], io['gkh'].partition_broadcast(128), W=[gkh])
            load_w_scaled(S, w1, lambda k: w1[:, k, :], io['w_in'], g_mix, 16, 0, 832, stg)
            load_w_scaled(S, wuq, lambda k: wuq[:, k, :], io['w_uq'], g_q, 4, 0, 1536, stg)
            load_w_scaled(S, wukv, lambda k: wukv[:, k, :], io['w_ukv'], g_kv, 2, 0, 2048, stg)
            xts = [S.sbuf(e1, 'xt', [128, D], F32) for _ in range(2)]
            wk = {'junk': S.sbuf(e1, 'junk', [128, D], BF16), 'ss': S.sbuf(e1, 'ss', [128, 4], F32),
                  'rstd': S.sbuf(e1, 'rstd', [128, 4], F32), 'xn': S.sbuf(e1, 'xn', [128, D], BF16),
                  'tp': [S.psum(e1, 'tp', [128, 1024], BF16) for _ in range(2)]}
            zps = S.psum(e1, 'zps', [128, 1024], F32)
            big = S.psum(e1, 'big', [128, 2048], F32)
            zt = S.sbuf(e1, 'zt', [128, 832], F32)
            cs = S.sbuf(e1, 'cs', [128, 32], F32)
            sn = S.sbuf(e1, 'sn', [128, 32], F32)
            sm = S.sbuf(e1, 'sm', [128, 16], F32)
            cn = S.sbuf(e1, 'cn', [128, 512], BF16)
            cT = S.sbuf(e1, 'cT', [128, 4, 128], BF16)
            sq = S.sbuf(e1, 'sq', [128, 1536], F32)
            ssh = S.sbuf(e1, 'ssh', [128, 8], F32)
            rsh = S.sbuf(e1, 'rsh', [128, 8], F32)
            qn = S.sbuf(e1, 'qn', [128, 8, 192], F32)
            qf = S.sbuf(e1, 'qf', [128, 8, 192], BF16)
            tr = S.sbuf(e1, 'tr', [128, 8, 32], F32)
            tr2 = S.sbuf(e1, 'tr2', [128, 8, 32], F32)
            kr = S.sbuf(e1, 'kr', [128, 64], F32)
            kpg = S.sbuf(e1, 'kpg', [128, 64], F32)
            vf = S.sbuf(e1, 'vf', [128, 8, 128], BF16)
            oTn = S.sbuf(e1, 'oTn', [128, 8, 128], BF16)
            oTr = S.sbuf(e1, 'oTr', [64, 8, 128], BF16)
            tp = wk['tp']

            def head_T_out(src, dst_dram, i):
                for h in range(8):
                    S.pe('transpose', R=[src, c['idb']], W=[tp[0]], out=tp[0][:, h * 128:(h + 1) * 128],
                         in_=src[:, h, 0:128], identity=c['idb'][:])
                for h in range(8):
                    S.pe('transpose', R=[src, c['idb']], W=[tp[1]], out=tp[1][0:64, h * 128:(h + 1) * 128],
                         in_=src[:, h, 128:192], identity=c['idb'][:])
                S.act(R=[tp[0]], W=[oTn], out=oTn[:], in_=tp[0][:, :].rearrange('p (h t) -> p h t', h=8),
                      func=AF.Copy)
                S.dve('tensor_copy', R=[tp[1]], W=[oTr], out=oTr[:],
                      in_=tp[1][0:64, :].rearrange('p (h t) -> p h t', h=8))
                S.dma('pool', dst_dram[:, 0:128, i * 128:(i + 1) * 128].rearrange('h d t -> d h t'), oTn[:], R=[oTn])
                S.dma('pool', dst_dram[:, 128:192, i * 128:(i + 1) * 128].rearrange('h d t -> d h t'), oTr[:], R=[oTr])

            def rope(src, x1, x2, o1, o2):
                sbuf_src, sbuf_dst = src
                shp = list(x1.shape)
                cb = cs[:, :].unsqueeze(1).to_broadcast(shp) if len(shp) == 3 else cs[:, :]
                sb_ = sn[:, :].unsqueeze(1).to_broadcast(shp) if len(shp) == 3 else sn[:, :]
                a1 = tr[:, 0:shp[1], :] if len(shp) == 3 else tr[:, 0, :]
                a2 = tr2[:, 0:shp[1], :] if len(shp) == 3 else tr2[:, 0, :]
                S.dve('tensor_tensor', R=[sbuf_src, cs], W=[tr], out=a1, in0=x1, in1=cb, op=ALU.mult)
                S.dve('tensor_tensor', R=[sbuf_src, sn], W=[tr2], out=a2, in0=x2, in1=sb_, op=ALU.mult)
                S.dve('tensor_tensor', R=[tr, tr2], W=[sbuf_dst], out=o1, in0=a1, in1=a2, op=ALU.subtract)
                S.dve('tensor_tensor', R=[sbuf_src, sn], W=[tr], out=a1, in0=x1, in1=sb_, op=ALU.mult)
                S.dve('tensor_tensor', R=[sbuf_src, cs], W=[tr2], out=a2, in0=x2, in1=cb, op=ALU.mult)
                S.dve('tensor_tensor', R=[tr, tr2], W=[sbuf_dst], out=o2, in0=a1, in1=a2, op=ALU.add)

            for i in range(NT):
                xt = xts[i % 2]
                S.dma('sp', xt[:], io['x'][i * 128:(i + 1) * 128, :], W=[xt])
                S.dma('sp', cs[:], io['cos'][i * 128:(i + 1) * 128, :], W=[cs])
                S.dma('sp', sn[:], io['sin'][i * 128:(i + 1) * 128, :], W=[sn])
                norm_T(S, xt, xnT_all[:, :, i * 128:(i + 1) * 128], xnT_all, wk, c)
                for (c0, c1) in ((0, 512), (512, 832)):
                    for k in range(16):
                        S.pe('matmul', R=[xnT_all, w1], W=[zps], out=zps[:, c0:c1],
                             lhsT=xnT_all[:, k, i * 128:(i + 1) * 128], rhs=w1[:, k, c0:c1],
                             start=(k == 0), stop=(k == 15))
                S.act(R=[zps], W=[zt], out=zt[:, 0:832], in_=zps[:, 0:832], func=AF.Copy)
                S.act(R=[zt], W=[wk['junk'], sm], out=wk['junk'][:, 0:512], in_=zt[:, 0:512], func=AF.Square,
                      accum_out=sm[:, 0:1])
                rstd_calc(S, (sm, sm[:, 0:1]), (sm, sm[:, 1:2]), 1.0 / 512)
                S.dve('tensor_scalar', R=[zt, sm], W=[cn], out=cn[:, 0:512], in0=zt[:, 0:512],
                      scalar1=sm[:, 1:2], scalar2=None, op0=ALU.mult)
                for k in range(4):
                    S.pe('transpose', R=[cn, c['idb']], W=[tp[0]], out=tp[0][:, k * 128:(k + 1) * 128],
                         in_=cn[:, k * 128:(k + 1) * 128], identity=c['idb'][:])
                S.dve('tensor_copy', R=[tp[0]], W=[cT], out=cT[:],
                      in_=tp[0][:, 0:512].rearrange('p (k t) -> p k t', k=4))
                for cb_ in range(3):
                    for k in range(4):
                        S.pe('matmul', R=[cT, wuq], W=[big], out=big[:, cb_ * 512:(cb_ + 1) * 512],
                             lhsT=cT[:, k, :], rhs=wuq[:, k, cb_ * 512:(cb_ + 1) * 512],
                             start=(k == 0), stop=(k == 3))
                S.act(R=[big], W=[sq], out=sq[:, 0:1536], in_=big[:, 0:1536], func=AF.Square)
                S.dve('tensor_reduce', R=[sq], W=[ssh], out=ssh[:],
                      in_=sq[:, 0:1536].rearrange('p (h d) -> p h d', h=8), axis=AX.X, op=ALU.add)
                rstd_calc(S, (ssh, ssh[:]), (rsh, rsh[:]), 1.0 / 192, post=192.0 ** -0.5)
                S.dve('tensor_tensor', R=[big, rsh], W=[qn], out=qn[:],
                      in0=big[:, 0:1536].rearrange('p (h d) -> p h d', h=8),
                      in1=bc(rsh[:, :].unsqueeze(2), [128, 8, 192]), op=ALU.mult)
                S.dve('tensor_tensor', R=[qn, gqh], W=[qn], out=qn[:], in0=qn[:],
                      in1=bc(gqh[:, :].unsqueeze(1), [128, 8, 192]), op=ALU.mult)
                S.act(R=[qn], W=[qf], out=qf[:, :, 0:128], in_=qn[:, :, 0:128], func=AF.Copy)
                rope((qn, qf), qn[:, :, 128:160], qn[:, :, 160:192], qf[:, :, 128:160], qf[:, :, 160:192])
                head_T_out(qf, io['QT'], i)
                S.act(R=[zt], W=[wk['junk'], sm], out=wk['junk'][:, 0:256], in_=zt[:, 512:768], func=AF.Square,
                      accum_out=sm[:, 2:3])
                rstd_calc(S, (sm, sm[:, 2:3]), (sm, sm[:, 3:4]), 1.0 / 256)
                S.dve('tensor_scalar', R=[zt, sm], W=[cn], out=cn[:, 0:256], in0=zt[:, 512:768],
                      scalar1=sm[:, 3:4], scalar2=None, op0=ALU.mult)
                for k in range(2):
                    S.pe('transpose', R=[cn, c['idb']], W=[tp[0]], out=tp[0][:, k * 128:(k + 1) * 128],
                         in_=cn[:, k * 128:(k + 1) * 128], identity=c['idb'][:])
                S.dve('tensor_copy', R=[tp[0]], W=[cT], out=cT[:, 0:2, :],
                      in_=tp[0][:, 0:256].rearrange('p (k t) -> p k t', k=2))
                for cb_ in range(4):
                    for k in range(2):
                        S.pe('matmul', R=[cT, wukv], W=[big], out=big[:, cb_ * 512:(cb_ + 1) * 512],
                             lhsT=cT[:, k, :], rhs=wukv[:, k, cb_ * 512:(cb_ + 1) * 512],
                             start=(k == 0), stop=(k == 1))
                kvv = big[:, :].rearrange('p (h d) -> p h d', h=8)
                S.act(R=[big], W=[sq], out=sq[:, 0:1024].rearrange('p (h d) -> p h d', h=8),
                      in_=kvv[:, :, 0:128], func=AF.Square)
                S.dve('tensor_reduce', R=[sq], W=[ssh], out=ssh[:],
                      in_=sq[:, 0:1024].rearrange('p (h d) -> p h d', h=8), axis=AX.X, op=ALU.add)
                S.act(R=[zt], W=[wk['junk'], sm], out=wk['junk'][:, 0:64], in_=zt[:, 768:832], func=AF.Square,
                      accum_out=sm[:, 4:5])
                S.dve('tensor_scalar', R=[ssh, sm], W=[ssh], out=ssh[:], in0=ssh[:], scalar1=sm[:, 4:5],
                      scalar2=None, op0=ALU.add)
                rstd_calc(S, (ssh, ssh[:]), (rsh, rsh[:]), 1.0 / 192)
                S.dve('tensor_tensor', R=[big, rsh], W=[qn], out=qn[:, :, 0:128], in0=kvv[:, :, 0:128],
                      in1=bc(rsh[:, :].unsqueeze(2), [128, 8, 128]), op=ALU.mult)
                S.dve('tensor_tensor', R=[qn, gkh], W=[qf], out=qf[:, :, 0:128], in0=qn[:, :, 0:128],
                      in1=bc(gkh[:, 0:128].unsqueeze(1), [128, 8, 128]), op=ALU.mult)
                S.dve('tensor_tensor', R=[zt, gkh], W=[kpg], out=kpg[:], in0=zt[:, 768:832], in1=gkh[:, 128:192],
                      op=ALU.mult)
                rope((kpg, kr), kpg[:, 0:32], kpg[:, 32:64], kr[:, 0:32], kr[:, 32:64])
                S.dve('tensor_tensor', R=[kr, rsh], W=[qf], out=qf[:, :, 128:192],
                      in0=bc(kr[:, :].unsqueeze(1), [128, 8, 64]), in1=bc(rsh[:, :].unsqueeze(2), [128, 8, 64]),
                      op=ALU.mult)
                S.act(R=[big], W=[vf], out=vf[:], in_=kvv[:, :, 128:256], func=AF.Copy)
                S.dma('pool', io['V'][:, :, i, :].rearrange('h p d -> p h d'), vf[:], R=[vf])
                head_T_out(qf, io['KT'], i)
        S.barrier()
        with ExitStack() as e2:
            w2 = S.sbuf(e2, 'w2', [128, 16, 2048], BF16)
            wrg = S.sbuf(e2, 'wrg', [128, 8, 128], BF16)
            wig = S.sbuf(e2, 'wig', [128, 8, 128], BF16)
            pv = S.sbuf(e2, 'pv', [128, 8, 8], F32)
            c1 = S.sbuf(e2, 'c1', [128, 8], F32)
            c2 = S.sbuf(e2, 'c2', [128, 8], F32)
            xhT = S.sbuf(e2, 'xhT', [128, 16, 128], BF16)
            g_mix = S.sbuf(e2, 'g_mix2', [128, 16], F32)
            e2a = ExitStack()
            stg = [S.sbuf(e2a, 'stg2', [128, 2048], F32) for _ in range(2)]
            S.dma('sp', g_mix[:], io['g_mix'], W=[g_mix])
            load_w_scaled(S, w2, lambda k: w2[:, k, :], io['w_in'], g_mix, 16, 832, 2880, stg)
            S.dma('sp', stg[0][:, 0:1024].rearrange('c (n d) -> c n d', n=8), io['w_rg'].rearrange('n c d -> c n d'),
                  W=[stg[0]])
            S.dve('tensor_copy', R=[stg[0]], W=[wrg], out=wrg[:], in_=stg[0][:, 0:1024].rearrange('c (n d) -> c n d', n=8))
            S.dma('sp', stg[1][:, 0:1024].rearrange('c (n d) -> c n d', n=8), io['w_ig'].rearrange('n c d -> c n d'),
                  W=[stg[1]])
            S.dve('tensor_copy', R=[stg[1]], W=[wig], out=wig[:], in_=stg[1][:, 0:1024].rearrange('c (n d) -> c n d', n=8))
            S.dma('sp', pv[:], io['lru_vec'], W=[pv])
            S.act(R=[pv], W=[c1], out=c1[:], in_=pv[:, :, 7], func=AF.Exp, scale=-1.0)
            S.act(R=[c1], W=[c2], out=c2[:], in_=c1[:], func=AF.Ln, bias=1.0)
            S.dve('tensor_scalar', R=[c2], W=[c1], out=c1[:], in0=c2[:], scalar1=-8.0, scalar2=None, op0=ALU.mult)
            S.dve('tensor_scalar', R=[c1], W=[c2], out=c2[:], in0=c1[:], scalar1=2.0, scalar2=None, op0=ALU.mult)
            xt = S.sbuf(e2a, 'xt2', [128, D], F32)
            wk = {'junk': S.sbuf(e2a, 'junk2', [128, D], BF16), 'ss': S.sbuf(e2a, 'ss2', [128, 4], F32),
                  'rstd': S.sbuf(e2a, 'rstd2', [128, 4], F32), 'xn': S.sbuf(e2a, 'xn2', [128, D], BF16),
                  'tp': [S.psum(e2, 'tp2', [128, 1024], BF16) for _ in range(2)]}
            S.dma('sp', xt[:], io['xh'], W=[xt])
            norm_T(S, xt, xhT[:, :, :], xhT, wk, c)
            e2a.close()
            S.barrier()
            xrb = S.sbuf(e2, 'xrb', [128, 8, G + 3], F32)
            hc = S.sbuf(e2, 'hc', [128, 8], F32)
            pc = S.sbuf(e2, 'pc', [128, 8], F32)
            zeros = S.sbuf(e2, 'zeros', [128, G], F32)
            S.dve('memset', W=[zeros], ap=zeros[:], constant=0.0)
            S.dve('memset', W=[hc], ap=hc[:], constant=0.0)
            S.dve('memset', W=[pc], ap=pc[:], constant=1.0)
            ps_x = [S.psum(e2, 'ps_x', [128, 512], F32) for _ in range(2)]
            ps_g = [S.psum(e2, 'ps_g', [128, 512], F32) for _ in range(2)]
            ps_r = S.psum(e2, 'ps_r', [128, 512], F32)
            ps_i = S.psum(e2, 'ps_i', [128, 512], F32)

            def mk(n_):
                return [S.sbuf(e2, n_, [128, G], F32) for _ in range(2)]
            xc_, r_, i_, a_, a2_, hl_, pl_, gg_ = (mk('xc'), mk('r'), mk('i'), mk('a'), mk('a2'), mk('hl'),
                                                   mk('pl'), mk('gg'))
            xcb_ = [S.sbuf(e2, 'xcb', [128, G], BF16) for _ in range(2)]
            it = 0
            for gi in range(NG):
                for n in range(8):
                    b = it % 2
                    it += 1
                    xc, r, ig, a, a2, hl, pl, gg, xcb = (xc_[b], r_[b], i_[b], a_[b], a2_[b], hl_[b], pl_[b],
                                                         gg_[b], xcb_[b])
                    px, pg = ps_x[b], ps_g[b]
                    cx = n * 128
                    cg = 1024 + n * 128
                    if gi == 0:
                        for k in range(16):
                            S.pe('matmul', R=[w2, xhT], W=[ps_r], out=ps_r[:, 0:3], lhsT=w2[:, k, cx:cx + 128],
                                 rhs=xhT[:, k, 125:128], start=(k == 0), stop=(k == 15))
                        S.dve('tensor_copy', R=[ps_r], W=[xrb], out=xrb[:, n, 0:3], in_=ps_r[:, 0:3])
                    else:
                        S.dve('tensor_copy', R=[xrb], W=[xrb], out=xrb[:, n, 0:3], in_=xrb[:, n, G:G + 3])
                    for k in range(16):
                        S.pe('matmul', R=[w2, xnT_all], W=[px], out=px[:, 0:G], lhsT=w2[:, k, cx:cx + 128],
                             rhs=xnT_all[:, k, gi * G:(gi + 1) * G], start=(k == 0), stop=(k == 15))
                    for k in range(16):
                        S.pe('matmul', R=[w2, xnT_all], W=[pg], out=pg[:, 0:G], lhsT=w2[:, k, cg:cg + 128],
                             rhs=xnT_all[:, k, gi * G:(gi + 1) * G], start=(k == 0), stop=(k == 15))
                    S.act(R=[px], W=[xrb], out=xrb[:, n, 3:3 + G], in_=px[:, 0:G], func=AF.Copy)
                    S.dve('tensor_scalar', R=[xrb, pv], W=[xc], out=xc[:], in0=xrb[:, n, 0:G], scalar1=pv[:, n, 0:1],
                          scalar2=pv[:, n, 4:5], op0=ALU.mult, op1=ALU.add)
                    for j in (1, 2, 3):
                        S.dve('scalar_tensor_tensor', R=[xrb, pv, xc], W=[xc], out=xc[:], in0=xrb[:, n, j:j + G],
                              scalar=pv[:, n, j:j + 1], in1=xc[:], op0=ALU.mult, op1=ALU.add)
                    S.act(R=[xc], W=[xcb], out=xcb[:], in_=xc[:], func=AF.Copy)
                    S.pe('matmul', R=[wrg, xcb], W=[ps_r], out=ps_r[:, 0:G], lhsT=wrg[:, n, :], rhs=xcb[:],
                         start=True, stop=True)
                    S.pe('matmul', R=[wig, xcb], W=[ps_i], out=ps_i[:, 0:G], lhsT=wig[:, n, :], rhs=xcb[:],
                         start=True, stop=True)
                    S.act(R=[ps_r, pv], W=[r], out=r[:], in_=ps_r[:, 0:G], func=AF.Sigmoid, bias=pv[:, n, 5:6])
                    S.act(R=[ps_i, pv], W=[ig], out=ig[:], in_=ps_i[:, 0:G], func=AF.Sigmoid, bias=pv[:, n, 6:7])
                    S.act(R=[r, c1], W=[a], out=a[:], in_=r[:], func=AF.Exp, scale=c1[:, n:n + 1])
                    S.act(R=[r, c2], W=[a2], out=a2[:], in_=r[:], func=AF.Exp, scale=c2[:, n:n + 1])
                    S.act(R=[pg], W=[gg], out=gg[:], in_=pg[:, 0:G], func=AF.Gelu)
                    S.dve('tensor_scalar', R=[a2], W=[a2], out=a2[:], in0=a2[:], scalar1=-1.0, scalar2=1.0,
                          op0=ALU.mult, op1=ALU.add)
                    S.dve('tensor_scalar', R=[a2], W=[a2], out=a2[:], in0=a2[:], scalar1=0.0, scalar2=None,
                          op0=ALU.max)
                    S.act(R=[a2], W=[a2], out=a2[:], in_=a2[:], func=AF.Sqrt)
                    S.dve('tensor_tensor', R=[ig, xc], W=[ig], out=ig[:], in0=ig[:], in1=xc[:], op=ALU.mult)
                    S.dve('tensor_tensor', R=[ig, a2], W=[ig], out=ig[:], in0=ig[:], in1=a2[:], op=ALU.mult)
                    S.dve('tensor_tensor_scan', R=[a, ig, hc], W=[hl], out=hl[:], data0=a[:], data1=ig[:],
                          initial=hc[:, n:n + 1], op0=ALU.mult, op1=ALU.add)
                    S.dve('tensor_tensor_scan', R=[a, zeros, pc], W=[pl], out=pl[:], data0=a[:], data1=zeros[:],
                          initial=pc[:, n:n + 1], op0=ALU.mult, op1=ALU.add)
                    S.dve('tensor_copy', R=[hl], W=[hc], out=hc[:, n:n + 1], in_=hl[:, G - 1:G])
                    S.dve('tensor_copy', R=[pl], W=[pc], out=pc[:, n:n + 1], in_=pl[:, G - 1:G])
                    S.dve('tensor_tensor', R=[hl, gg], W=[hl], out=hl[:], in0=hl[:], in1=gg[:], op=ALU.mult)
                    S.dve('tensor_tensor', R=[pl, gg], W=[pl], out=pl[:], in0=pl[:], in1=gg[:], op=ALU.mult)
                    S.dma('pool', io['Yl'][n, :, gi * G:(gi + 1) * G], hl[:], R=[hl])
                    S.dma('pool', io['Yp'][n, :, gi * G:(gi + 1) * G], pl[:], R=[pl])
            S.dma('pool', io['carry'][0], pc[:], R=[pc])
            S.dma('pool', io['carry'][1], hc[:], R=[hc])
        S.barrier()


def dram_in(nc, name, shape, dt=F32):
    return nc.dram_tensor(name, list(shape), dt, kind="ExternalInput").ap()


def dram_out(nc, name, shape, dt=F32):
    return nc.dram_tensor(name, list(shape), dt, kind="ExternalOutput").ap()


def build_A(T):
    nc = bass.Bass("TRN2", target_bir_lowering=False)
    io = {}
    for name, shape in (('x', [T, D]), ('xh', [128, D]), ('cos', [T, 32]), ('sin', [T, 32]), ('ident', [128, 128]),
                        ('g_mix', [128, 16]), ('g_q', [128, 4]), ('g_kv', [128, 2]), ('gqh', [192]), ('gkh', [192]),
                        ('w_in', [D, DIN]), ('w_uq', [512, 1536]), ('w_ukv', [256, 2048]),
                        ('w_rg', [8, 128, 128]), ('w_ig', [8, 128, 128]), ('lru_vec', [128, 8, 8])):
        io[name] = dram_in(nc, name, shape)
    io['QT'] = dram_out(nc, 'QT', [NH, 192, T], BF16)
    io['KT'] = dram_out(nc, 'KT', [NH, 192, T], BF16)
    io['V'] = dram_out(nc, 'V', [NH, 128, T // 128, 128], BF16)
    io['Yl'] = dram_out(nc, 'Yl', [8, 128, T])
    io['Yp'] = dram_out(nc, 'Yp', [8, 128, T])
    io['carry'] = dram_out(nc, 'carry', [2, 128, 8])
    with ExitStack() as es:
        S = Sched(nc, es)
        emit_A(S, nc, T, io)
        S.barrier()
    return nc


def col_layout(v, k):
    return np.ascontiguousarray(np.asarray(v, np.float32).reshape(k, 128).T)


def rope_tables(S_len):
    inv = (1.0 / (np.float32(10000.0) ** (np.arange(0, 64, 2, dtype=np.float32) / np.float32(64)))).astype(np.float32)
    ang = np.arange(S_len, dtype=np.float32)[:, None] * inv[None, :]
    return np.cos(ang).astype(np.float32), np.sin(ang).astype(np.float32)


def inputs_A(inp, l, xs, T):
    S_len = CPB * T
    cos, sin = rope_tables(S_len)
    lru_vec = np.stack([col_layout(inp['conv_w'][l][j], 8) for j in range(4)] +
                       [col_layout(inp[n][l], 8) for n in ('conv_b', 'b_rgate', 'b_igate', 'lru_lambda')], axis=2)
    shared = {
        'ident': np.eye(128, dtype=np.float32),
        'g_mix': col_layout(inp['mix_norm_g'][l], 16), 'g_q': col_layout(inp['q_lora_norm_g'][l], 4),
        'g_kv': col_layout(inp['kv_lora_norm_g'][l], 2),
        'gqh': np.ascontiguousarray(inp['att_q_norm_g'][l]), 'gkh': np.ascontiguousarray(inp['att_k_norm_g'][l]),
        'w_in': np.ascontiguousarray(inp['w_in'][l]), 'w_uq': np.ascontiguousarray(inp['w_uq'][l]),
        'w_ukv': np.ascontiguousarray(inp['w_ukv'][l]),
        'w_rg': np.ascontiguousarray(inp['w_rgate'][l]), 'w_ig': np.ascontiguousarray(inp['w_igate'][l]),
        'lru_vec': np.ascontiguousarray(lru_vec),
    }
    maps = []
    for cidx in range(NCORES):
        j = cidx % CPB
        m = dict(shared)
        m['x'] = xs[cidx]
        m['xh'] = xs[cidx - 1][T - 128:T] if j > 0 else np.zeros((128, D), np.float32)
        m['cos'] = np.ascontiguousarray(cos[j * T:(j + 1) * T])
        m['sin'] = np.ascontiguousarray(sin[j * T:(j + 1) * T])
        maps.append(m)
    return maps


def emit_B_attn(S, nc, T, io, c, yattT, ssatt):
    NT = T // 128
    G = min(512, T)
    NG = T // G
    GT = G // 128
    NOTH = CPB - 1
    with ExitStack() as es:
        tri = S.sbuf(es, 'tri', [128, 128], F32)
        kb = S.sbuf(es, 'kb', [128, NOTH], F32)
        onesf = S.sbuf(es, 'onesf', [128, 128], F32)
        S.dma('sp', tri[:], io['tri'], W=[tri])
        S.dma('sp', kb[:], io['kb'], W=[kb])
        S.dve('memset', W=[onesf], ap=onesf[:], constant=1.0)
        hb = []
        for _ in range(2):
            d_ = {
                'qn': S.sbuf(es, 'qn', [128, T], BF16), 'qr': S.sbuf(es, 'qr', [128, T], BF16),
                'kn': S.sbuf(es, 'kn', [128, T], BF16), 'kr': S.sbuf(es, 'kr', [128, T], BF16),
                'v': S.sbuf(es, 'v', [128, NT, 128], BF16),
                'Kn': S.sbuf(es, 'Kn', [128, NOTH, T], BF16), 'Kr': S.sbuf(es, 'Kr', [128, NOTH, T], BF16),
                'V': S.sbuf(es, 'V', [128, NOTH, NT, 128], BF16)}
            for n_ in ('qr', 'kr', 'Kr'):
                S.pool('memset', W=[d_[n_]], ap=d_[n_][:], constant=0.0)
            hb.append(d_)
        ps = [S.psum(es, 'ps', [128, 512], F32) for _ in range(3)]
        po = [S.psum(es, 'po', [128, 512], F32) for _ in range(2)]
        pd = S.psum(es, 'pd', [128, 512], F32)
        pss = S.psum(es, 'pss', [128, 512], F32)
        pT = [S.sbuf(es, 'pT', [128, G], BF16) for _ in range(4)]
        acc = [S.sbuf(es, 'acc', [128, G], F32) for _ in range(2)]
        rec = S.sbuf(es, 'rec', [128, G], F32)
        yf = S.sbuf(es, 'yf', [128, G], F32)
        sq = S.sbuf(es, 'sq', [128, G], F32)
        it = 0
        gi = 0
        for h in range(NH):
            B = hb[h % 2]
            S.dma('sp', B['qn'][:], io['QT'][h, 0:128, :], W=[B['qn']])
            S.dma('sp', B['qr'][0:64, :], io['QT'][h, 128:192, :], W=[B['qr']])
            S.dma('sp', B['kn'][:], io['KT'][h, 0:128, :], W=[B['kn']])
            S.dma('sp', B['kr'][0:64, :], io['KT'][h, 128:192, :], W=[B['kr']])
            S.dma('sp', B['v'][:], io['V'][h], W=[B['v']])
            S.dma('sp', B['Kn'][:], io['KT_oth'][:, h, 0:128, :].rearrange('s d t -> d s t'), W=[B['Kn']])
            S.dma('sp', B['Kr'][0:64], io['KT_oth'][:, h, 128:192, :].rearrange('s d t -> d s t'), W=[B['Kr']])
            for s in range(NOTH):
                S.dma('sp', B['V'][:, s], io['V_oth'][s, h], W=[B['V']])
            for g in range(NG):
                a_ = acc[gi % 2]
                o_ = po[gi % 2]
                gi += 1
                items = []
                for s in range(NOTH):
                    for kt in range(NT):
                        items.append((B['Kn'][:, s, kt * 128:(kt + 1) * 128], B['Kr'][:, s, kt * 128:(kt + 1) * 128],
                                      B['V'][:, s, kt, :], 0, False, kb[:, s:s + 1], ('Kn', 'Kr', 'V')))
                for kt in range((g + 1) * GT):
                    d = max(0, kt - g * GT)
                    items.append((B['kn'][:, kt * 128:(kt + 1) * 128], B['kr'][:, kt * 128:(kt + 1) * 128],
                                  B['v'][:, kt, :], d * 128, kt >= g * GT, None, ('kn', 'kr', 'v')))
                base = it
                it += len(items)

                def qk(idx):
                    kn_ap, kr_ap, v_ap, off, diag, bias, names = items[idx]
                    p_ = ps[(base + idx) % 3]
                    N = G - off
                    q0 = g * G + off
                    S.pe('matmul', R=[B[names[0]], B['qn']], W=[p_], out=p_[:, 0:N], lhsT=kn_ap,
                         rhs=B['qn'][:, q0:q0 + N], start=True, stop=False)
                    S.pe('matmul', R=[B[names[1]], B['qr']], W=[p_], out=p_[:, 0:N], lhsT=kr_ap,
                         rhs=B['qr'][:, q0:q0 + N], start=False, stop=True)

                def rest(idx):
                    kn_ap, kr_ap, v_ap, off, diag, bias, names = items[idx]
                    p_ = ps[(base + idx) % 3]
                    t_ = pT[(base + idx) % 4]
                    N = G - off
                    if bias is None:
                        S.act(R=[p_], W=[t_], out=t_[:, 0:N], in_=p_[:, 0:N], func=AF.Exp)
                    else:
                        S.act(R=[p_, kb], W=[t_], out=t_[:, 0:N], in_=p_[:, 0:N], func=AF.Exp, bias=bias)
                    if diag:
                        S.dve('tensor_tensor', R=[t_, tri], W=[t_], out=t_[:, 0:128], in0=t_[:, 0:128], in1=tri[:],
                              op=ALU.mult)
                    if idx == 0:
                        S.dve('tensor_copy', R=[t_], W=[a_], out=a_[:, 0:G], in_=t_[:, 0:G])
                    else:
                        S.dve('tensor_tensor', R=[t_, a_], W=[a_], out=a_[:, off:G], in0=a_[:, off:G], in1=t_[:, 0:N],
                              op=ALU.add)
                    S.pe('matmul', R=[B[names[2]], t_], W=[o_], out=o_[:, off:G], lhsT=v_ap, rhs=t_[:, 0:N],
                         start=(idx == 0), stop=(idx == len(items) - 1))

                qk(0)
                if len(items) > 1:
                    qk(1)
                for idx in range(len(items)):
                    if idx + 2 < len(items):
                        qk(idx + 2)
                    rest(idx)
                S.pe('matmul', R=[onesf, a_], W=[pd], out=pd[:, 0:G], lhsT=onesf[:], rhs=a_[:, 0:G], start=True, stop=True)
                S.dve('reciprocal', R=[pd], W=[rec], out=rec[:], in_=pd[:, 0:G])
                S.dve('tensor_tensor', R=[o_, rec], W=[yf], out=yf[:], in0=o_[:, 0:G], in1=rec[:], op=ALU.mult)
                S.act(R=[yf], W=[yattT], out=yattT[:, h, g * G:(g + 1) * G], in_=yf[:], func=AF.Copy)
                S.act(R=[yf], W=[sq], out=sq[:], in_=yf[:], func=AF.Square)
                for t in range(GT):
                    S.pe('matmul', R=[sq, onesf], W=[pss], out=pss[:, t:t + 1], lhsT=sq[:, t * 128:(t + 1) * 128],
                         rhs=onesf[:, 0:1], start=True, stop=True)
                if h == 0:
                    S.dve('tensor_copy', R=[pss], W=[ssatt], out=ssatt[:, g * GT:(g + 1) * GT], in_=pss[:, 0:GT])
                else:
                    S.dve('tensor_tensor', R=[pss, ssatt], W=[ssatt], out=ssatt[:, g * GT:(g + 1) * GT],
                          in0=ssatt[:, g * GT:(g + 1) * GT], in1=pss[:, 0:GT], op=ALU.add)
    S.barrier()


def head4_norm(S, wk4, src_ps, gbuf, post, dst):
    sq4, st4, qn4 = wk4
    S.act(R=[src_ps], W=[sq4], out=sq4[:], in_=src_ps[:, 0:512], func=AF.Square)
    S.dve('tensor_reduce', R=[sq4], W=[st4], out=st4[:, 0:4], in_=sq4[:, :].rearrange('p (h d) -> p h d', h=4),
          axis=AX.X, op=ALU.add)
    rstd_calc(S, (st4, st4[:, 0:4]), (st4, st4[:, 4:8]), 1.0 / 128, post=post)
    S.dve('tensor_tensor', R=[src_ps, st4], W=[qn4], out=qn4[:],
          in0=src_ps[:, 0:512].rearrange('p (h d) -> p h d', h=4),
          in1=bc(st4[:, 4:8].unsqueeze(2), [128, 4, 128]), op=ALU.mult)
    S.dve('tensor_tensor', R=[qn4, gbuf], W=[dst], out=dst[:], in0=qn4[:],
          in1=bc(gbuf[:, :].unsqueeze(1), [128, 4, 128]), op=ALU.mult)


def mk_wk(S, es):
    return {'junk': S.sbuf(es, 'junk', [128, D], BF16), 'ss': S.sbuf(es, 'ss', [128, 4], F32),
            'rstd': S.sbuf(es, 'rstd', [128, 4], F32), 'xn': S.sbuf(es, 'xn', [128, D], BF16),
            'tp': [S.psum(es, 'tp', [128, 1024], BF16) for _ in range(2)]}


def mk_wk4(S, es):
    return (S.sbuf(es, 'sq4', [128, 512], F32), S.sbuf(es, 'st4', [128, 8], F32), S.sbuf(es, 'qn4', [128, 4, 128], F32))


def emit_B_mid(S, nc, T, io, c, yattT, ssatt):
    NT = T // 128
    with ExitStack() as es:
        onesf = S.sbuf(es, 'onesf', [128, 128], F32)
        S.dve('memset', W=[onesf], ap=onesf[:], constant=1.0)
        yrT = S.sbuf(es, 'yrT', [128, 8, T], BF16)
        ssr = S.sbuf(es, 'ssr', [128, NT], F32)
        ra = S.sbuf(es, 'ra', [128, NT], F32)
        rr = S.sbuf(es, 'rr', [128, NT], F32)
        g_o = S.sbuf(es, 'g_o', [128, 16], F32)
        S.dma('sp', g_o[:], io['g_out'], W=[g_o])
        pA = S.psum(es, 'pA', [128, 512], F32)
        pB = S.psum(es, 'pB', [128, 512], F32)
        pC = S.psum(es, 'pC', [128, 512], F32)
        with ExitStack() as e1:
            car = S.sbuf(e1, 'car', [128, CPB, 2, 8], F32)
            cf = S.sbuf(e1, 'cf', [128, CPB], F32)
            h0 = S.sbuf(e1, 'h0', [128, 8], F32)
            tmp = S.sbuf(e1, 'tmp8', [128, 8], F32)
            S.dma('sp', car[:], io['carry_all'].rearrange('s a p n -> p s a n'), W=[car])
            S.dma('sp', cf[:], io['cf'], W=[cf])
            S.dve('memset', W=[h0], ap=h0[:], constant=0.0)
            for s_ in range(CPB):
                S.dve('tensor_tensor', R=[car, h0], W=[tmp], out=tmp[:], in0=car[:, s_, 0, :], in1=h0[:], op=ALU.mult)
                S.dve('tensor_tensor', R=[car, tmp], W=[tmp], out=tmp[:], in0=tmp[:], in1=car[:, s_, 1, :], op=ALU.add)
                S.dve('tensor_tensor', R=[tmp, h0], W=[tmp], out=tmp[:], in0=tmp[:], in1=h0[:], op=ALU.subtract)
                S.dve('scalar_tensor_tensor', R=[tmp, cf, h0], W=[h0], out=h0[:], in0=tmp[:], scalar=cf[:, s_:s_ + 1],
                      in1=h0[:], op0=ALU.mult, op1=ALU.add)
            yl = [S.sbuf(e1, 'yl', [128, T], F32) for _ in range(2)]
            yp = [S.sbuf(e1, 'yp', [128, T], F32) for _ in range(2)]
            for n in range(8):
                a, b = yl[n % 2], yp[n % 2]
                S.dma('sp', a[:], io['Yl'][n], W=[a])
                S.dma('sp', b[:], io['Yp'][n], W=[b])
                S.dve('scalar_tensor_tensor', R=[a, b, h0], W=[a], out=a[:], in0=b[:], scalar=h0[:, n:n + 1], in1=a[:],
                      op0=ALU.mult, op1=ALU.add)
                S.act(R=[a], W=[yrT], out=yrT[:, n, :], in_=a[:], func=AF.Copy)
                S.act(R=[a], W=[b], out=b[:], in_=a[:], func=AF.Square)
                for t in range(NT):
                    S.pe('matmul', R=[b, onesf], W=[pC], out=pC[:, t:t + 1], lhsT=b[:, t * 128:(t + 1) * 128],
                         rhs=onesf[:, 0:1], start=True, stop=True)
                if n == 0:
                    S.dve('tensor_copy', R=[pC], W=[ssr], out=ssr[:], in_=pC[:, 0:NT])
                else:
                    S.dve('tensor_tensor', R=[pC, ssr], W=[ssr], out=ssr[:], in0=ssr[:], in1=pC[:, 0:NT], op=ALU.add)
            rstd_calc(S, (ssr, ssr[:]), (rr, rr[:]), 1.0 / 1024)
            rstd_calc(S, (ssatt, ssatt[:]), (ra, ra[:]), 1.0 / 1024)
        S.barrier()
        wout = S.sbuf(es, 'wout', [128, 16, D], BF16)
        xts = [S.sbuf(es, 'xt', [128, D], F32) for _ in range(2)]
        x1s = [S.sbuf(es, 'x1', [128, D], F32) for _ in range(2)]
        load_w_scaled(S, wout, lambda k: wout[:, k, :], io['w_out'], g_o, 16, 0, D, x1s)
        for i in range(NT):
            xt, x1 = xts[i % 2], x1s[i % 2]
            tok = slice(i * 128, (i + 1) * 128)
            S.dma('sp', xt[:], io['x'][tok, :], W=[xt])
            for cb in range(4):
                cs_ = slice(cb * 512, (cb + 1) * 512)
                for k in range(8):
                    S.pe('matmul', R=[yattT, wout], W=[pA], out=pA[:, 0:512], lhsT=yattT[:, k, tok], rhs=wout[:, k, cs_],
                         start=(k == 0), stop=(k == 7))
                for k in range(8):
                    S.pe('matmul', R=[yrT, wout], W=[pB], out=pB[:, 0:512], lhsT=yrT[:, k, tok], rhs=wout[:, 8 + k, cs_],
                         start=(k == 0), stop=(k == 7))
                S.dve('scalar_tensor_tensor', R=[pA, ra, xt], W=[x1], out=x1[:, cs_], in0=pA[:, 0:512],
                      scalar=ra[:, i:i + 1], in1=xt[:, cs_], op0=ALU.mult, op1=ALU.add)
                S.dve('scalar_tensor_tensor', R=[pB, rr, x1], W=[x1], out=x1[:, cs_], in0=pB[:, 0:512],
                      scalar=rr[:, i:i + 1], in1=x1[:, cs_], op0=ALU.mult, op1=ALU.add)
            S.dma('pool', io['x2'][tok, :], x1[:], R=[x1])
    S.barrier()
    with ExitStack() as es:
        onesb = S.sbuf(es, 'onesb', [128, 128], BF16)
        S.dve('memset', W=[onesb], ap=onesb[:], constant=1.0)
        wmq = S.sbuf(es, 'wmq', [128, 16, 512], BF16)
        wmo = S.sbuf(es, 'wmo', [128, 4, D], BF16)
        kmT = S.sbuf(es, 'kmT', [128, 4, NMEM], BF16)
        vm = S.sbuf(es, 'vm', [128, 2, 512], BF16)
        gmq = S.sbuf(es, 'gmq', [128, 128], F32)
        gmk = S.sbuf(es, 'gmk', [128, 128], F32)
        g_x = S.sbuf(es, 'g_x', [128, 16], F32)
        g_m = S.sbuf(es, 'g_m', [128, 16], F32)
        one4 = S.sbuf(es, 'one4', [128, 4], F32)
        wk = mk_wk(S, es)
        wk4 = mk_wk4(S, es)
        qb4 = S.sbuf(es, 'qb4', [128, 4, 128], BF16)
        tp = wk['tp']
        pA = S.psum(es, 'pA', [128, 512], F32)
        pB = S.psum(es, 'pB', [128, 512], F32)
        pC = S.psum(es, 'pC', [128, 512], F32)
        pD = S.psum(es, 'pD', [128, 512], F32)
        xts = [S.sbuf(es, 'xt', [128, D], F32) for _ in range(2)]
        S.dve('memset', W=[one4], ap=one4[:], constant=1.0)
        S.dma('sp', gmq[:], io['g_memq'].partition_broadcast(128), W=[gmq])
        S.dma('sp', gmk[:], io['g_memk'].partition_broadcast(128), W=[gmk])
        S.dma('sp', g_x[:], io['g_xattn'], W=[g_x])
        S.dma('sp', g_m[:], io['g_mem'], W=[g_m])
        load_w_scaled(S, wmq, lambda k: wmq[:, k, :], io['w_mq'], g_x, 16, 0, 512, xts)
        load_w_scaled(S, wmo, lambda k: wmo[:, k, :], io['w_mo'], one4, 4, 0, D, xts)
        with ExitStack() as e1:
            wmk = S.sbuf(e1, 'wmk', [128, 16, 512], BF16)
            wmv = S.sbuf(e1, 'wmv', [128, 16, 512], BF16)
            mT = S.sbuf(e1, 'mT', [128, 16, 128], BF16)
            load_w_scaled(S, wmk, lambda k: wmk[:, k, :], io['w_mk'], g_m, 16, 0, 512, xts)
            load_w_scaled(S, wmv, lambda k: wmv[:, k, :], io['w_mv'], g_m, 16, 0, 512, xts)
            for mt in range(2):
                xt = xts[mt]
                S.dma('sp', xt[:], io['mem'][mt * 128:(mt + 1) * 128, :], W=[xt])
                norm_T(S, xt, mT[:, :, :], mT, wk, c)
                for k in range(16):
                    S.pe('matmul', R=[mT, wmk], W=[pA], out=pA[:, 0:512], lhsT=mT[:, k, :], rhs=wmk[:, k, :],
                         start=(k == 0), stop=(k == 15))
                for k in range(16):
                    S.pe('matmul', R=[mT, wmv], W=[pB], out=pB[:, 0:512], lhsT=mT[:, k, :], rhs=wmv[:, k, :],
                         start=(k == 0), stop=(k == 15))
                S.act(R=[pB], W=[vm], out=vm[:, mt, :], in_=pB[:, 0:512], func=AF.Copy)
                head4_norm(S, wk4, pA, gmk, 1.0, qb4)
                for h in range(4):
                    S.pe('transpose', R=[qb4, c['idb']], W=[tp[0]], out=tp[0][:, h * 128:(h + 1) * 128],
                         in_=qb4[:, h, :], identity=c['idb'][:])
                S.dve('tensor_copy', R=[tp[0]], W=[kmT], out=kmT[:, :, mt * 128:(mt + 1) * 128],
                      in_=tp[0][:, 0:512].rearrange('p (h t) -> p h t', h=4))
        S.barrier()
        xnT = S.sbuf(es, 'xnT', [128, 16, 128], BF16)
        qmT = S.sbuf(es, 'qmT', [128, 4, 128], BF16)
        pm = [S.sbuf(es, 'pm', [128, 512], BF16) for _ in range(2)]
        recm = S.sbuf(es, 'recm', [128, 512], F32)
        oT = S.sbuf(es, 'oT', [128, 4, 128], BF16)
        for i in range(NT):
            x1 = xts[i % 2]
            tok = slice(i * 128, (i + 1) * 128)
            S.dma('sp', x1[:], io['x2'][tok, :], W=[x1])
            norm_T(S, x1, xnT[:, :, :], xnT, wk, c)
            for k in range(16):
                S.pe('matmul', R=[xnT, wmq], W=[pC], out=pC[:, 0:512], lhsT=xnT[:, k, :], rhs=wmq[:, k, :],
                     start=(k == 0), stop=(k == 15))
            head4_norm(S, wk4, pC, gmq, 128.0 ** -0.5, qb4)
            for h in range(4):
                S.pe('transpose', R=[qb4, c['idb']], W=[tp[0]], out=tp[0][:, h * 128:(h + 1) * 128],
                     in_=qb4[:, h, :], identity=c['idb'][:])
            S.dve('tensor_copy', R=[tp[0]], W=[qmT], out=qmT[:], in_=tp[0][:, 0:512].rearrange('p (h t) -> p h t', h=4))
            for mt in range(2):
                for h in range(4):
                    S.pe('matmul', R=[kmT, qmT], W=[pD], out=pD[:, h * 128:(h + 1) * 128],
                         lhsT=kmT[:, h, mt * 128:(mt + 1) * 128], rhs=qmT[:, h, :], start=True, stop=True)
                S.act(R=[pD], W=[pm[mt]], out=pm[mt][:], in_=pD[:, 0:512], func=AF.Exp)
            for h in range(4):
                for mt in range(2):
                    S.pe('matmul', R=[vm, pm[mt]], W=[pA], out=pA[:, h * 128:(h + 1) * 128],
                         lhsT=vm[:, mt, h * 128:(h + 1) * 128], rhs=pm[mt][:, h * 128:(h + 1) * 128],
                         start=(mt == 0), stop=(mt == 1))
            for mt in range(2):
                S.pe('matmul', R=[onesb, pm[mt]], W=[pB], out=pB[:, 0:512], lhsT=onesb[:], rhs=pm[mt][:],
                     start=(mt == 0), stop=(mt == 1))
            S.dve('reciprocal', R=[pB], W=[recm], out=recm[:], in_=pB[:, 0:512])
            S.dve('tensor_tensor', R=[pA, recm], W=[oT], out=oT[:], in0=pA[:, 0:512].rearrange('p (h t) -> p h t', h=4),
                  in1=recm[:, :].rearrange('p (h t) -> p h t', h=4), op=ALU.mult)
            for cb in range(4):
                cs_ = slice(cb * 512, (cb + 1) * 512)
                pX = pC if cb % 2 == 0 else pD
                for h in range(4):
                    S.pe('matmul', R=[oT, wmo], W=[pX], out=pX[:, 0:512], lhsT=oT[:, h, :], rhs=wmo[:, h, cs_],
                         start=(h == 0), stop=(h == 3))
                S.dve('tensor_tensor', R=[pX, x1], W=[x1], out=x1[:, cs_], in0=x1[:, cs_], in1=pX[:, 0:512], op=ALU.add)
            S.dma('pool', io['x2'][tok, :], x1[:], R=[x1])
    S.barrier()


def emit_B_moe(S, nc, T, io, c):
    NT = T // 128
    G = min(512, T)
    NG = T // G
    GT = G // 128
    with ExitStack() as es:
        gm = S.sbuf(es, 'gm', [128, D], F32)
        wr = S.sbuf(es, 'wr', [128, 16, 36], F32)
        br = S.sbuf(es, 'br', [128, 36], F32)
        S.dma('sp', gm[:], io['g_moe'].partition_broadcast(128), W=[gm])
        S.dma('sp', wr[:], io['w_r'].rearrange('(k p) n -> p k n', p=128), W=[wr])
        S.dma('sp', br[:], io['b_r'].partition_broadcast(128), W=[br])
        wb = [{'g': S.sbuf(es, 'weg', [128, 16, DEXP], BF16), 'u': S.sbuf(es, 'weu', [128, 16, DEXP], BF16),
               'd': S.sbuf(es, 'wed', [128, 4, D], BF16)} for _ in range(2)]
        hT = S.sbuf(es, 'hT', [128, 16, G], BF16)
        aT = [S.sbuf(es, 'aT', [128, 4, G], BF16) for _ in range(2)]
        yacc = [S.sbuf(es, 'yacc', [128, D], F32) for _ in range(GT)]
        cw = S.sbuf(es, 'cw', [128, GT, 32], F32)
        hf = S.sbuf(es, 'hf', [128, D], F32)
        hb = S.sbuf(es, 'hb', [128, D], BF16)
        hT32 = S.sbuf(es, 'hT32', [128, 16, 128], F32)
        junk = S.sbuf(es, 'junk', [128, D], BF16)
        st = S.sbuf(es, 'st', [128, 16], F32)
        lg = S.sbuf(es, 'lg', [128, 36], F32)
        oh = S.sbuf(es, 'oh', [128, 4], F32)
        sel = S.sbuf(es, 'sel', [128, 8], F32)
        p8 = S.sbuf(es, 'p8', [128, 8], F32)
        p8b = S.sbuf(es, 'p8b', [128, 8], F32)
        mk1 = S.sbuf(es, 'mk1', [128, 8], F32)
        mk2 = S.sbuf(es, 'mk2', [128, 8], F32)
        sgt = [S.sbuf(es, 'sgt', [128, G], F32) for _ in range(2)]
        tpb = S.psum(es, 'tpb', [128, 1024], BF16)
        tpf = S.psum(es, 'tpf', [128, 512], F32)
        pg = [S.psum(es, 'pg', [128, 512], F32) for _ in range(2)]
        pu = [S.psum(es, 'pu', [128, 512], F32) for _ in range(2)]
        py = [S.psum(es, 'py', [128, 512], F32) for _ in range(2)]
        for g in range(NG):
            for t in range(GT):
                i = g * GT + t
                tok = slice(i * 128, (i + 1) * 128)
                ya = yacc[t]
                S.dma('sp', ya[:], io['x2'][tok, :], W=[ya])
                S.act(R=[ya], W=[junk, st], out=junk[:], in_=ya[:], func=AF.Square, accum_out=st[:, 0:1])
                rstd_calc(S, (st, st[:, 0:1]), (st, st[:, 1:2]), 1.0 / D)
                S.dve('scalar_tensor_tensor', R=[ya, st, gm], W=[hf], out=hf[:], in0=ya[:], scalar=st[:, 1:2], in1=gm[:],
                      op0=ALU.mult, op1=ALU.mult)
                S.act(R=[hf], W=[hb], out=hb[:], in_=hf[:], func=AF.Copy)
                for half in range(2):
                    for k in range(8):
                        kk = half * 8 + k
                        S.pe('transpose', R=[hb, c['idb']], W=[tpb], out=tpb[:, k * 128:(k + 1) * 128],
                             in_=hb[:, kk * 128:(kk + 1) * 128], identity=c['idb'][:])
                    S.act(R=[tpb], W=[hT], out=hT[:, half * 8:(half + 1) * 8, t * 128:(t + 1) * 128],
                          in_=tpb[:, :].rearrange('p (k t) -> p k t', k=8), func=AF.Copy)
                for q4 in range(4):
                    for k in range(4):
                        kk = q4 * 4 + k
                        S.pe('transpose', R=[hf, c['idf']], W=[tpf], out=tpf[:, k * 128:(k + 1) * 128],
                             in_=hf[:, kk * 128:(kk + 1) * 128], identity=c['idf'][:])
                    S.dve('tensor_copy', R=[tpf], W=[hT32], out=hT32[:, q4 * 4:(q4 + 1) * 4, :],
                          in_=tpf[:, :].rearrange('p (k t) -> p k t', k=4))
                pl = py[0]
                for k in range(16):
                    S.pe('matmul', R=[hT32, wr], W=[pl], out=pl[:, 0:36], lhsT=hT32[:, k, :], rhs=wr[:, k, :],
                         start=(k == 0), stop=(k == 15))
                S.dve('tensor_tensor', R=[pl, br], W=[lg], out=lg[:], in0=pl[:, 0:36], in1=br[:], op=ALU.add)
                S.dve('tensor_reduce', R=[lg], W=[st], out=st[:, 2:3], in_=lg[:, 0:4], axis=AX.X, op=ALU.max)
                S.dve('tensor_scalar', R=[st], W=[st], out=st[:, 3:4], in0=st[:, 2:3], scalar1=-1.0, scalar2=None,
                      op0=ALU.mult)
                S.act(R=[lg, st], W=[p8b, st], out=p8b[:, 0:4], in_=lg[:, 0:4], func=AF.Exp, bias=st[:, 3:4],
                      accum_out=st[:, 4:5])
                S.dve('reciprocal', R=[st], W=[st], out=st[:, 5:6], in_=st[:, 4:5])
                S.dve('tensor_scalar', R=[lg, st], W=[oh], out=oh[:], in0=lg[:, 0:4], scalar1=st[:, 2:3], scalar2=None,
                      op0=ALU.is_equal)
                S.dve('tensor_scalar', R=[lg, oh], W=[sel], out=sel[:], in0=lg[:, 4:12], scalar1=oh[:, 0:1], scalar2=None,
                      op0=ALU.mult)
                for gg_ in (1, 2, 3):
                    S.dve('scalar_tensor_tensor', R=[lg, oh, sel], W=[sel], out=sel[:], in0=lg[:, 4 + 8 * gg_:12 + 8 * gg_],
                          scalar=oh[:, gg_:gg_ + 1], in1=sel[:], op0=ALU.mult, op1=ALU.add)
                S.dve('tensor_reduce', R=[sel], W=[st], out=st[:, 6:7], in_=sel[:], axis=AX.X, op=ALU.max)
                S.dve('tensor_scalar', R=[st], W=[st], out=st[:, 7:8], in0=st[:, 6:7], scalar1=-1.0, scalar2=None,
                      op0=ALU.mult)
                S.act(R=[sel, st], W=[p8], out=p8[:], in_=sel[:], func=AF.Exp, bias=st[:, 7:8])
                S.dve('tensor_reduce', R=[p8], W=[st], out=st[:, 8:9], in_=p8[:], axis=AX.X, op=ALU.max)
                S.dve('tensor_scalar', R=[p8, st], W=[mk1], out=mk1[:], in0=p8[:], scalar1=st[:, 8:9], scalar2=None,
                      op0=ALU.is_equal)
                S.dve('scalar_tensor_tensor', R=[mk1, p8], W=[p8b], out=p8b[:], in0=mk1[:], scalar=-2.0, in1=p8[:],
                      op0=ALU.mult, op1=ALU.add)
                S.dve('tensor_reduce', R=[p8b], W=[st], out=st[:, 9:10], in_=p8b[:], axis=AX.X, op=ALU.max)
                S.dve('tensor_scalar', R=[p8b, st], W=[mk2], out=mk2[:], in0=p8b[:], scalar1=st[:, 9:10], scalar2=None,
                      op0=ALU.is_equal)
                S.dve('tensor_tensor', R=[mk1, mk2], W=[mk1], out=mk1[:], in0=mk1[:], in1=mk2[:], op=ALU.add)
                S.dve('tensor_tensor', R=[st], W=[st], out=st[:, 10:11], in0=st[:, 8:9], in1=st[:, 9:10], op=ALU.add)
                S.dve('reciprocal', R=[st], W=[st], out=st[:, 11:12], in_=st[:, 10:11])
                S.dve('tensor_tensor', R=[st], W=[st], out=st[:, 12:13], in0=st[:, 11:12], in1=st[:, 5:6], op=ALU.mult)
                S.dve('scalar_tensor_tensor', R=[p8, st, mk1], W=[p8b], out=p8b[:], in0=p8[:], scalar=st[:, 12:13],
                      in1=mk1[:], op0=ALU.mult, op1=ALU.mult)
                S.dve('tensor_tensor', R=[oh, p8b], W=[cw], out=cw[:, t, :].rearrange('p (a b) -> p a b', a=4),
                      in0=bc(oh[:, :].unsqueeze(2), [128, 4, 8]), in1=bc(p8b[:, :].unsqueeze(1), [128, 4, 8]),
                      op=ALU.mult)
            for e in range(NEXP):
                W_ = wb[e % 2]
                S.dma('pool', W_['g'][:], io['w_eg'][e].rearrange('(k p) f -> p k f', p=128), W=[W_['g']])
                S.dma('pool', W_['u'][:], io['w_eu'][e].rearrange('(k p) f -> p k f', p=128), W=[W_['u']])
                S.dma('pool', W_['d'][:], io['w_ed'][e].rearrange('(k p) n -> p k n', p=128), W=[W_['d']])
                a_ = aT[e % 2]
                for fc in range(4):
                    pg_, pu_, sg_ = pg[fc % 2], pu[fc % 2], sgt[fc % 2]
                    fs = slice(fc * 128, (fc + 1) * 128)
                    for k in range(16):
                        S.pe('matmul', R=[W_['g'], hT], W=[pg_], out=pg_[:, 0:G], lhsT=W_['g'][:, k, fs], rhs=hT[:, k, :],
                             start=(k == 0), stop=(k == 15))
                    for k in range(16):
                        S.pe('matmul', R=[W_['u'], hT], W=[pu_], out=pu_[:, 0:G], lhsT=W_['u'][:, k, fs], rhs=hT[:, k, :],
                             start=(k == 0), stop=(k == 15))
                    S.act(R=[pg_], W=[sg_], out=sg_[:], in_=pg_[:, 0:G], func=AF.Silu)
                    S.dve('tensor_tensor', R=[sg_, pu_], W=[a_], out=a_[:, fc, :], in0=sg_[:], in1=pu_[:, 0:G], op=ALU.mult)
                for t in range(GT):
                    for cb in range(4):
                        py_ = py[(t * 4 + cb) % 2]
                        cs_ = slice(cb * 512, (cb + 1) * 512)
                        for fc in range(4):
                            S.pe('matmul', R=[a_, W_['d']], W=[py_], out=py_[:, 0:512], lhsT=a_[:, fc, t * 128:(t + 1) * 128],
                                 rhs=W_['d'][:, fc, cs_], start=(fc == 0), stop=(fc == 3))
                        S.dve('scalar_tensor_tensor', R=[py_, cw, yacc[t]], W=[yacc[t]], out=yacc[t][:, cs_], in0=py_[:, 0:512],
                              scalar=cw[:, t, e:e + 1], in1=yacc[t][:, cs_], op0=ALU.mult, op1=ALU.add)
            for t in range(GT):
                i = g * GT + t
                S.dma('sp', io['xo'][i * 128:(i + 1) * 128, :], yacc[t][:], R=[yacc[t]])
    S.barrier()


def emit_B(S, nc, T, io, sparse=True):
    with ExitStack() as es:
        c = load_consts(S, es, io)
        with ExitStack() as e1:
            yattT = S.sbuf(e1, 'yattT', [128, NH, T], BF16)
            ssatt = S.sbuf(e1, 'ssatt', [128, T // 128], F32)
            emit_B_attn(S, nc, T, io, c, yattT, ssatt)
            emit_B_mid(S, nc, T, io, c, yattT, ssatt)
        S.barrier()
        if sparse:
            emit_B_moe_sparse(S, nc, T, io, c)
        else:
            emit_B_moe(S, nc, T, io, c)


def build_B(T, sparse=True):
    nc = bass.Bass("TRN2", target_bir_lowering=False)
    io = {}
    NT = T // 128
    for name, shape, dt in (
            ('x', [T, D], F32), ('ident', [128, 128], F32), ('tri', [128, 128], F32), ('kb', [128, CPB - 1], F32),
            ('QT', [NH, 192, T], BF16), ('KT', [NH, 192, T], BF16), ('V', [NH, 128, NT, 128], BF16),
            ('KT_oth', [CPB - 1, NH, 192, T], BF16), ('V_oth', [CPB - 1, NH, 128, NT, 128], BF16),
            ('Yl', [8, 128, T], F32), ('Yp', [8, 128, T], F32), ('carry_all', [CPB, 2, 128, 8], F32),
            ('cf', [128, CPB], F32), ('g_out', [128, 16], F32), ('w_out', [D, D], F32),
            ('g_xattn', [128, 16], F32), ('g_mem', [128, 16], F32), ('g_memq', [128], F32), ('g_memk', [128], F32),
            ('w_mq', [D, 512], F32), ('w_mk', [D, 512], F32), ('w_mv', [D, 512], F32), ('w_mo', [512, D], F32),
            ('mem', [NMEM, D], F32), ('g_moe', [D], F32), ('w_r', [D, 36], F32), ('b_r', [36], F32),
            ('w_eg', [NEXP, D, DEXP], F32), ('w_eu', [NEXP, D, DEXP], F32), ('w_ed', [NEXP, DEXP, D], F32)):
        io[name] = dram_in(nc, name, shape, dt)
    io['x2'] = dram_out(nc, 'x2', [T, D])
    io['xo'] = dram_out(nc, 'xo', [T, D])
    if sparse:
        for name, shape in (('ecap', [32]), ('ustrict', [128, 128]), ('tok2', [128, NT, 2])):
            io[name] = dram_in(nc, name, shape)
        io['cnt'] = dram_out(nc, 'cnt', [1, 32])
        io['Xs'] = nc.dram_tensor('Xs', [NEXP * CAP, D], BF16, kind="Internal").ap()
        io['meta'] = nc.dram_tensor('meta', [NEXP * CAP, 16], F32, kind="Internal").ap()
        io['Yt'] = [nc.dram_tensor('Yt%d' % hh, [2 * T, 1024], F32, kind="Internal").ap() for hh in range(2)]
    with ExitStack() as es:
        S = Sched(nc, es)
        emit_B(S, nc, T, io, sparse)
        S.barrier()
    return nc


def inputs_B(inp, l, xs, resA, T):
    g = lambda n: np.ascontiguousarray(inp[n][l])
    tri = np.triu(np.ones((128, 128), np.float32))
    shared = {
        'ident': np.eye(128, dtype=np.float32), 'tri': tri,
        'g_out': col_layout(np.concatenate([inp['att_out_norm_g'][l], inp['rnn_out_norm_g'][l]]), 16),
        'w_out': g('w_out'), 'g_xattn': col_layout(inp['xattn_norm_g'][l], 16), 'g_mem': col_layout(inp['mem_norm_g'][l], 16),
        'g_memq': g('mem_q_norm_g'), 'g_memk': g('mem_k_norm_g'),
        'w_mq': g('w_mq'), 'w_mk': g('w_mk'), 'w_mv': g('w_mv'), 'w_mo': g('w_mo'),
        'g_moe': g('moe_norm_g'),
        'w_r': np.ascontiguousarray(np.concatenate([inp['w_router_group'][l], inp['w_router_expert'][l]], axis=1)),
        'b_r': np.ascontiguousarray(np.concatenate([inp['b_router_group'][l], inp['b_router_expert'][l]])),
        'w_eg': g('w_exp_gate'), 'w_eu': g('w_exp_up'), 'w_ed': g('w_exp_down'),
    }
    NT = T // 128
    t_idx = (np.arange(NT)[None, :] * 128 + np.arange(128)[:, None]).astype(np.float32)
    shared['ecap'] = (np.arange(32) * CAP).astype(np.float32)
    shared['ustrict'] = np.triu(np.ones((128, 128), np.float32), 1)
    shared['tok2'] = np.ascontiguousarray(np.stack([2 * t_idx, 2 * t_idx + 1], axis=2))
    maps = []
    for cidx in range(NCORES):
        b, j = cidx // CPB, cidx % CPB
        grp = range(b * CPB, (b + 1) * CPB)
        m = dict(shared)
        m['x'] = xs[cidx]
        m['mem'] = np.ascontiguousarray(inp['mem'][b])
        for n in ('QT', 'KT', 'V', 'Yl', 'Yp'):
            m[n] = resA[cidx][n]
        oth = [k for k in grp if k != cidx]
        m['KT_oth'] = np.stack([resA[k]['KT'] for k in oth])
        m['V_oth'] = np.stack([resA[k]['V'] for k in oth])
        m['carry_all'] = np.stack([resA[k]['carry'] for k in grp])
        kb = np.zeros((128, CPB - 1), np.float32)
        for si, k in enumerate(oth):
            if k > cidx:
                kb[:, si] = -30000.0
        m['kb'] = kb
        cf = np.zeros((128, CPB), np.float32)
        cf[:, :j] = 1.0
        m['cf'] = cf
        maps.append(m)
    return maps


_NC_CACHE = {}


def _get_nc(kind, T):
    key = (kind, T)
    if key not in _NC_CACHE:
        _NC_CACHE[key] = build_A(T) if kind == 'A' else build_B(T)
    return _NC_CACHE[key]


def run_layers(inp, T):
    B = inp['x'].shape[0]
    x = np.asarray(inp['x'], np.float32)
    xs = [np.ascontiguousarray(x[c // CPB, (c % CPB) * T:(c % CPB + 1) * T]) for c in range(NCORES)]
    inp = {k: np.asarray(v) for k, v in inp.items()}
    for l in range(2):
        ra = run_bass_kernel_spmd(build_A(T), inputs_A(inp, l, xs, T), core_ids=list(range(NCORES))).results
        mb = inputs_B(inp, l, xs, ra, T)
        rb = run_bass_kernel_spmd(build_B(T, True), mb, core_ids=list(range(NCORES))).results
        if max(float(np.max(r['cnt'])) for r in rb) > CAP:
            for m in mb:
                for n in ('ecap', 'ustrict', 'tok2'):
                    m.pop(n)
            rb = run_bass_kernel_spmd(build_B(T, False), mb, core_ids=list(range(NCORES))).results
        xs = [np.ascontiguousarray(rb[c]['xo']) for c in range(NCORES)]
    out = np.empty((B, CPB * T, D), np.float32)
    for c in range(NCORES):
        out[c // CPB, (c % CPB) * T:(c % CPB + 1) * T] = xs[c]
    return out


def kernel(**inputs):
    return run_layers(inputs, 2048)


def emit_B_moe_sparse(S, nc, T, io, c):
    NT = T // 128
    NB = CAP // 128
    NSLOT = NEXP * CAP
    with ExitStack() as es:
        onesb = S.sbuf(es, 'onesb', [128, 128], BF16)
        S.dve('memset', W=[onesb], ap=onesb[:], constant=1.0)
        breg_slot = nc.gpsimd.to_reg(NSLOT - 1)
        breg_tok = nc.gpsimd.to_reg(2 * T - 1)
        with ExitStack() as e1:
            gm = S.sbuf(e1, 'gm', [128, D], F32)
            wr = S.sbuf(e1, 'wr', [128, 16, 36], F32)
            br = S.sbuf(e1, 'br', [128, 36], F32)
            ecap = S.sbuf(e1, 'ecap', [128, 32], F32)
            ustr = S.sbuf(e1, 'ustr', [128, 128], F32)
            ustb = S.sbuf(e1, 'ustb', [128, 128], BF16)
            tok2 = S.sbuf(e1, 'tok2', [128, NT, 2], F32)
            S.dma('sp', gm[:], io['g_moe'].partition_broadcast(128), W=[gm])
            S.dma('sp', wr[:], io['w_r'].rearrange('(k p) n -> p k n', p=128), W=[wr])
            S.dma('sp', br[:], io['b_r'].partition_broadcast(128), W=[br])
            S.dma('sp', ecap[:], io['ecap'].partition_broadcast(128), W=[ecap])
            S.dma('sp', ustr[:], io['ustrict'], W=[ustr])
            S.dma('sp', tok2[:], io['tok2'], W=[tok2])
            S.dve('tensor_copy', R=[ustr], W=[ustb], out=ustb[:], in_=ustr[:])
            mi = S.sbuf(e1, 'mi', [128, NSLOT // 128, 16], F32)
            S.dve('memset', W=[mi], ap=mi[:], constant=0.0)
            S.dve('memset', W=[mi], ap=mi[:, :, 0:1], constant=1.0e6)
            S.dma('sp', io['meta'].rearrange('(p a) m -> p a m', p=128), mi[:], R=[mi])
            Mall = S.sbuf(e1, 'Mall', [128, NT, 32], BF16)
            ya = S.sbuf(e1, 'ya', [128, D], F32)
            hf = S.sbuf(e1, 'hf', [128, D], F32)
            hbs = [S.sbuf(e1, 'hb', [128, D], BF16) for _ in range(2)]
            hT32 = S.sbuf(e1, 'hT32', [128, 16, 128], F32)
            junk = S.sbuf(e1, 'junk', [128, D], BF16)
            st = S.sbuf(e1, 'st', [128, 16], F32)
            lg = S.sbuf(e1, 'lg', [128, 36], F32)
            oh = S.sbuf(e1, 'oh', [128, 4], F32)
            sel = S.sbuf(e1, 'sel', [128, 8], F32)
            p8 = S.sbuf(e1, 'p8', [128, 8], F32)
            p8b = S.sbuf(e1, 'p8b', [128, 8], F32)
            mk1 = S.sbuf(e1, 'mk1', [128, 8], F32)
            mk2 = S.sbuf(e1, 'mk2', [128, 8], F32)
            M1 = S.sbuf(e1, 'M1', [128, 32], F32)
            M2 = S.sbuf(e1, 'M2', [128, 32], F32)
            Mt = S.sbuf(e1, 'Mt', [128, 32], F32)
            psl = S.sbuf(e1, 'psl', [128, 32], F32)
            pen = S.sbuf(e1, 'pen', [128, 32], F32)
            sk = S.sbuf(e1, 'sk', [128, 2], F32)
            metas = [[S.sbuf(e1, 'mrow', [128, 16], F32) for _ in range(2)] for _ in range(2)]
            skis = [[S.sbuf(e1, 'ski', [128, 1], I32) for _ in range(2)] for _ in range(2)]
            tpf = S.psum(e1, 'tpf', [128, 512], F32)
            plg = S.psum(e1, 'plg', [128, 512], F32)
            ppos = S.psum(e1, 'ppos', [128, 512], F32)
            for i in range(NT):
                tok = slice(i * 128, (i + 1) * 128)
                hb = hbs[i % 2]
                S.dma('sp', ya[:], io['x2'][tok, :], W=[ya])
                S.act(R=[ya], W=[junk, st], out=junk[:], in_=ya[:], func=AF.Square, accum_out=st[:, 0:1])
                rstd_calc(S, (st, st[:, 0:1]), (st, st[:, 1:2]), 1.0 / D)
                S.dve('scalar_tensor_tensor', R=[ya, st, gm], W=[hf], out=hf[:], in0=ya[:], scalar=st[:, 1:2], in1=gm[:],
                      op0=ALU.mult, op1=ALU.mult)
                S.act(R=[hf], W=[hb], out=hb[:], in_=hf[:], func=AF.Copy)
                for q4 in range(4):
                    for k in range(4):
                        kk = q4 * 4 + k
                        S.pe('transpose', R=[hf, c['idf']], W=[tpf], out=tpf[:, k * 128:(k + 1) * 128],
                             in_=hf[:, kk * 128:(kk + 1) * 128], identity=c['idf'][:])
                    S.dve('tensor_copy', R=[tpf], W=[hT32], out=hT32[:, q4 * 4:(q4 + 1) * 4, :],
                          in_=tpf[:, :].rearrange('p (k t) -> p k t', k=4))
                for k in range(16):
                    S.pe('matmul', R=[hT32, wr], W=[plg], out=plg[:, 0:36], lhsT=hT32[:, k, :], rhs=wr[:, k, :],
                         start=(k == 0), stop=(k == 15))
                S.dve('tensor_tensor', R=[plg, br], W=[lg], out=lg[:], in0=plg[:, 0:36], in1=br[:], op=ALU.add)
                S.dve('tensor_reduce', R=[lg], W=[st], out=st[:, 2:3], in_=lg[:, 0:4], axis=AX.X, op=ALU.max)
                S.dve('tensor_scalar', R=[st], W=[st], out=st[:, 3:4], in0=st[:, 2:3], scalar1=-1.0, scalar2=None,
                      op0=ALU.mult)
                S.act(R=[lg, st], W=[p8b, st], out=p8b[:, 0:4], in_=lg[:, 0:4], func=AF.Exp, bias=st[:, 3:4],
                      accum_out=st[:, 4:5])
                S.dve('reciprocal', R=[st], W=[st], out=st[:, 5:6], in_=st[:, 4:5])
                S.dve('tensor_scalar', R=[lg, st], W=[oh], out=oh[:], in0=lg[:, 0:4], scalar1=st[:, 2:3], scalar2=None,
                      op0=ALU.is_equal)
                S.dve('tensor_scalar', R=[lg, oh], W=[sel], out=sel[:], in0=lg[:, 4:12], scalar1=oh[:, 0:1], scalar2=None,
                      op0=ALU.mult)
                for gg_ in (1, 2, 3):
                    S.dve('scalar_tensor_tensor', R=[lg, oh, sel], W=[sel], out=sel[:], in0=lg[:, 4 + 8 * gg_:12 + 8 * gg_],
                          scalar=oh[:, gg_:gg_ + 1], in1=sel[:], op0=ALU.mult, op1=ALU.add)
                S.dve('tensor_reduce', R=[sel], W=[st], out=st[:, 6:7], in_=sel[:], axis=AX.X, op=ALU.max)
                S.dve('tensor_scalar', R=[st], W=[st], out=st[:, 7:8], in0=st[:, 6:7], scalar1=-1.0, scalar2=None,
                      op0=ALU.mult)
                S.act(R=[sel, st], W=[p8], out=p8[:], in_=sel[:], func=AF.Exp, bias=st[:, 7:8])
                S.dve('tensor_reduce', R=[p8], W=[st], out=st[:, 8:9], in_=p8[:], axis=AX.X, op=ALU.max)
                S.dve('tensor_scalar', R=[p8, st], W=[mk1], out=mk1[:], in0=p8[:], scalar1=st[:, 8:9], scalar2=None,
                      op0=ALU.is_equal)
                S.dve('scalar_tensor_tensor', R=[mk1, p8], W=[p8b], out=p8b[:], in0=mk1[:], scalar=-2.0, in1=p8[:],
                      op0=ALU.mult, op1=ALU.add)
                S.dve('tensor_reduce', R=[p8b], W=[st], out=st[:, 9:10], in_=p8b[:], axis=AX.X, op=ALU.max)
                S.dve('tensor_scalar', R=[p8b, st], W=[mk2], out=mk2[:], in0=p8b[:], scalar1=st[:, 9:10], scalar2=None,
                      op0=ALU.is_equal)
                S.dve('tensor_tensor', R=[st], W=[st], out=st[:, 10:11], in0=st[:, 8:9], in1=st[:, 9:10], op=ALU.add)
                S.dve('reciprocal', R=[st], W=[st], out=st[:, 11:12], in_=st[:, 10:11])
                S.dve('tensor_tensor', R=[st], W=[st], out=st[:, 12:13], in0=st[:, 11:12], in1=st[:, 5:6], op=ALU.mult)
                m1r, m2r = metas[i % 2]
                S.dve('memset', W=[m1r], ap=m1r[:], constant=0.0)
                S.dve('memset', W=[m2r], ap=m2r[:], constant=0.0)
                S.dve('tensor_copy', R=[tok2], W=[m1r], out=m1r[:, 0:1], in_=tok2[:, i, 0:1])
                S.dve('tensor_copy', R=[tok2], W=[m2r], out=m2r[:, 0:1], in_=tok2[:, i, 1:2])
                S.dve('tensor_tensor', R=[st], W=[m1r], out=m1r[:, 1:2], in0=st[:, 8:9], in1=st[:, 12:13], op=ALU.mult)
                S.dve('tensor_tensor', R=[st], W=[m2r], out=m2r[:, 1:2], in0=st[:, 9:10], in1=st[:, 12:13], op=ALU.mult)
                S.dve('tensor_tensor', R=[oh, mk1], W=[M1], out=M1[:, :].rearrange('p (a b) -> p a b', a=4),
                      in0=bc(oh[:, :].unsqueeze(2), [128, 4, 8]), in1=bc(mk1[:, :].unsqueeze(1), [128, 4, 8]), op=ALU.mult)
                S.dve('tensor_tensor', R=[oh, mk2], W=[M2], out=M2[:, :].rearrange('p (a b) -> p a b', a=4),
                      in0=bc(oh[:, :].unsqueeze(2), [128, 4, 8]), in1=bc(mk2[:, :].unsqueeze(1), [128, 4, 8]), op=ALU.mult)
                S.dve('tensor_tensor', R=[M1, M2], W=[Mt], out=Mt[:], in0=M1[:], in1=M2[:], op=ALU.add)
                S.dve('tensor_copy', R=[Mt], W=[Mall], out=Mall[:, i, :], in_=Mt[:])
                S.pe('matmul', R=[ustb, Mall], W=[ppos], out=ppos[:, 0:32], lhsT=ustb[:], rhs=Mall[:, i, :],
                     start=True, stop=(i == 0))
                for i2 in range(i):
                    S.pe('matmul', R=[onesb, Mall], W=[ppos], out=ppos[:, 0:32], lhsT=onesb[:], rhs=Mall[:, i2, :],
                         start=False, stop=(i2 == i - 1))
                S.dve('tensor_scalar', R=[ppos], W=[pen], out=pen[:], in0=ppos[:, 0:32], scalar1=float(CAP), scalar2=1.0e6,
                      op0=ALU.is_ge, op1=ALU.mult)
                S.dve('tensor_tensor', R=[ppos, ecap], W=[psl], out=psl[:], in0=ppos[:, 0:32], in1=ecap[:], op=ALU.add)
                S.dve('tensor_tensor', R=[psl, pen], W=[psl], out=psl[:], in0=psl[:], in1=pen[:], op=ALU.add)
                S.dve('tensor_tensor', R=[psl, M1], W=[pen], out=pen[:], in0=psl[:], in1=M1[:], op=ALU.mult)
                S.dve('tensor_reduce', R=[pen], W=[sk], out=sk[:, 0:1], in_=pen[:], axis=AX.X, op=ALU.add)
                S.dve('tensor_tensor', R=[psl, M2], W=[pen], out=pen[:], in0=psl[:], in1=M2[:], op=ALU.mult)
                S.dve('tensor_reduce', R=[pen], W=[sk], out=sk[:, 1:2], in_=pen[:], axis=AX.X, op=ALU.add)
                for kk in range(2):
                    ski = skis[i % 2][kk]
                    mr = metas[i % 2][kk]
                    S.dve('tensor_copy', R=[sk], W=[ski], out=ski[:], in_=sk[:, kk:kk + 1])
                    S.indirect(R=[hb, ski], out=io['Xs'], out_offset=bass.IndirectOffsetOnAxis(ap=ski[:, :], axis=0),
                               in_=hb[:, :], in_offset=None, bounds_check=breg_slot, oob_is_err=False)
                    S.indirect(R=[mr, ski], out=io['meta'], out_offset=bass.IndirectOffsetOnAxis(ap=ski[:, :], axis=0),
                               in_=mr[:, :], in_offset=None, bounds_check=breg_slot, oob_is_err=False)
            for i2 in range(NT):
                S.pe('matmul', R=[onesb, Mall], W=[ppos], out=ppos[:, 0:32], lhsT=onesb[:], rhs=Mall[:, i2, :],
                     start=(i2 == 0), stop=(i2 == NT - 1))
            S.dve('tensor_copy', R=[ppos], W=[psl], out=psl[:], in_=ppos[:, 0:32])
            S.dma('sp', io['cnt'], psl[0:1, :], R=[psl])
        S.barrier()
        with ExitStack() as e2:
            wb = [{'g': S.sbuf(e2, 'weg', [128, 16, DEXP], BF16), 'u': S.sbuf(e2, 'weu', [128, 16, DEXP], BF16),
                   'd': S.sbuf(e2, 'wed', [128, 4, D], BF16)} for _ in range(2)]
            xbs = [S.sbuf(e2, 'xb', [128, D], BF16) for _ in range(2)]
            mts = [S.sbuf(e2, 'mt', [128, 16], F32) for _ in range(2)]
            idxs = [S.sbuf(e2, 'idx', [128, 1], I32) for _ in range(2)]
            xTs = [S.sbuf(e2, 'xT', [128, 16, 128], BF16) for _ in range(2)]
            sgs = [S.sbuf(e2, 'sg', [128, DEXP], F32) for _ in range(2)]
            abs_ = [S.sbuf(e2, 'ab', [128, DEXP], BF16) for _ in range(2)]
            aTs = [S.sbuf(e2, 'aT', [128, 4, 128], BF16) for _ in range(2)]
            ybs = [S.sbuf(e2, 'yb', [128, D], F32) for _ in range(2)]
            tpb = [S.psum(e2, 'tpb', [128, 1024], BF16) for _ in range(2)]
            pg = S.psum(e2, 'pg', [128, 512], F32)
            pu = S.psum(e2, 'pu', [128, 512], F32)
            py = [S.psum(e2, 'py', [128, 512], F32) for _ in range(2)]

            def load_w(e):
                W_ = wb[e % 2]
                S.dma('pool', W_['g'][:], io['w_eg'][e].rearrange('(k p) f -> p k f', p=128), W=[W_['g']])
                S.dma('pool', W_['u'][:], io['w_eu'][e].rearrange('(k p) f -> p k f', p=128), W=[W_['u']])
                S.dma('pool', W_['d'][:], io['w_ed'][e].rearrange('(k p) n -> p k n', p=128), W=[W_['d']])
            load_w(0)
            it = 0
            for e in range(NEXP):
                if e + 1 < NEXP:
                    load_w(e + 1)
                W_ = wb[e % 2]
                for r in range(NB):
                    b = it % 2
                    it += 1
                    xb, mt, idx, xT, sg, ab, aT, yb = xbs[b], mts[b], idxs[b], xTs[b], sgs[b], abs_[b], aTs[b], ybs[b]
                    r0 = e * CAP + r * 128
                    S.dma('sp', xb[:], io['Xs'][r0:r0 + 128, :], W=[xb])
                    S.dma('sp', mt[:], io['meta'][r0:r0 + 128, :], W=[mt])
                    for half in range(2):
                        for k in range(8):
                            kk = half * 8 + k
                            S.pe('transpose', R=[xb, c['idb']], W=[tpb[half]], out=tpb[half][:, k * 128:(k + 1) * 128],
                                 in_=xb[:, kk * 128:(kk + 1) * 128], identity=c['idb'][:])
                        src = tpb[half][:, :].rearrange('p (k t) -> p k t', k=8)
                        if half == 0:
                            S.act(R=[tpb[half]], W=[xT], out=xT[:, 0:8, :], in_=src, func=AF.Copy)
                        else:
                            S.dve('tensor_copy', R=[tpb[half]], W=[xT], out=xT[:, 8:16, :], in_=src)
                    for k in range(16):
                        S.pe('matmul', R=[xT, W_['g']], W=[pg], out=pg[:, 0:DEXP], lhsT=xT[:, k, :], rhs=W_['g'][:, k, :],
                             start=(k == 0), stop=(k == 15))
                    for k in range(16):
                        S.pe('matmul', R=[xT, W_['u']], W=[pu], out=pu[:, 0:DEXP], lhsT=xT[:, k, :], rhs=W_['u'][:, k, :],
                             start=(k == 0), stop=(k == 15))
                    S.act(R=[pg], W=[sg], out=sg[:], in_=pg[:, 0:DEXP], func=AF.Silu)
                    S.dve('tensor_tensor', R=[sg, pu], W=[ab], out=ab[:], in0=sg[:], in1=pu[:, 0:DEXP], op=ALU.mult)
                    for fc in range(4):
                        S.pe('transpose', R=[ab, c['idb']], W=[tpb[0]], out=tpb[0][:, fc * 128:(fc + 1) * 128],
                             in_=ab[:, fc * 128:(fc + 1) * 128], identity=c['idb'][:])
                    S.dve('tensor_copy', R=[tpb[0]], W=[aT], out=aT[:], in_=tpb[0][:, 0:512].rearrange('p (k t) -> p k t', k=4))
                    for cb in range(4):
                        py_ = py[cb % 2]
                        cs_ = slice(cb * 512, (cb + 1) * 512)
                        for fc in range(4):
                            S.pe('matmul', R=[aT, W_['d']], W=[py_], out=py_[:, 0:512], lhsT=aT[:, fc, :], rhs=W_['d'][:, fc, cs_],
                                 start=(fc == 0), stop=(fc == 3))
                        if cb % 2 == 0:
                            S.act(R=[py_, mt], W=[yb], out=yb[:, cs_], in_=py_[:, 0:512], func=AF.Copy, scale=mt[:, 1:2])
                        else:
                            S.dve('tensor_scalar', R=[py_, mt], W=[yb], out=yb[:, cs_], in0=py_[:, 0:512], scalar1=mt[:, 1:2],
                                  scalar2=None, op0=ALU.mult)
                    S.dve('tensor_copy', R=[mt], W=[idx], out=idx[:], in_=mt[:, 0:1])
                    for hh in range(2):
                        S.indirect(R=[yb, idx], out=io['Yt'][hh], out_offset=bass.IndirectOffsetOnAxis(ap=idx[:, :], axis=0),
                                   in_=yb[:, hh * 1024:(hh + 1) * 1024], in_offset=None, bounds_check=breg_tok,
                                   oob_is_err=False)
        S.barrier()
        with ExitStack() as e3:
            yts = [S.sbuf(e3, 'yt', [128, 2, D], F32) for _ in range(2)]
            xs_ = [S.sbuf(e3, 'xc', [128, D], F32) for _ in range(2)]
            for i in range(NT):
                yt, xc = yts[i % 2], xs_[i % 2]
                for hh in range(2):
                    S.dma('sp', yt[:, :, hh * 1024:(hh + 1) * 1024],
                          io['Yt'][hh][i * 256:(i + 1) * 256, :].rearrange('(p k) d -> p k d', k=2), W=[yt])
                S.dma('sp', xc[:], io['x2'][i * 128:(i + 1) * 128, :], W=[xc])
                S.dve('tensor_tensor', R=[xc, yt], W=[xc], out=xc[:], in0=xc[:], in1=yt[:, 0, :], op=ALU.add)
                S.pool('tensor_tensor', R=[xc, yt], W=[xc], out=xc[:], in0=xc[:], in1=yt[:, 1, :], op=ALU.add)
                S.dma('sp', io['xo'][i * 128:(i + 1) * 128, :], xc[:], R=[xc])
    S.barrier()
```

```python
import numpy as np
from contextlib import ExitStack
import concourse.bass as bass
import concourse.mybir as mybir
from concourse.bass_utils import run_bass_kernel_spmd

F32 = mybir.dt.float32
BF16 = mybir.dt.bfloat16
AF = mybir.ActivationFunctionType
ALU = mybir.AluOpType
AX = mybir.AxisListType
I32 = mybir.dt.int32
CAP = 640

EPS = 1e-6
D = 2048
NCORES = 8
CPB = 4
DIN = 2880
NH = 8
NEXP = 32
DEXP = 512
NMEM = 256


class Buf:
    def __init__(self, t):
        self.t = t
        self.w = None
        self.r = []

    def __getitem__(self, k):
        return self.t[k]


class Sched:
    SAME_ENGINE_SYNC = True
    NDMA = 12

    def __init__(self, nc, es):
        self.nc = nc
        self.es = es
        self.eng = {'pe': nc.tensor, 'act': nc.scalar, 'dve': nc.vector,
                    'pool': nc.gpsimd, 'sp': nc.sync}
        self.sem = {}
        self.cnt = {}
        for e in ('pe', 'act', 'dve', 'pool'):
            self.sem[e] = es.enter_context(nc.semaphore('s_' + e))
            self.cnt[e] = 0
        self.dq = {}
        for q in ('sp', 'pool', 'act'):
            lst = []
            for i in range(self.NDMA):
                k = ('d', q, i)
                self.sem[k] = es.enter_context(nc.semaphore('d_%s_%d' % (q, i)))
                self.cnt[k] = 0
                lst.append(k)
            self.dq[q] = [lst, 0]
        self.seen = {e: {} for e in self.eng}
        self.n_ins = 0
        self.uid = 0

    def sbuf(self, es, name, shape, dt):
        self.uid += 1
        return Buf(es.enter_context(self.nc.sbuf_tensor('%s_%d' % (name, self.uid), list(shape), dt)))

    def psum(self, es, name, shape, dt=F32):
        self.uid += 1
        return Buf(es.enter_context(self.nc.psum_tensor('%s_%d' % (name, self.uid), list(shape), dt)))

    def _deps(self, e, reads, writes):
        deps = {}

        def add(tok):
            if tok is None:
                return
            k, v = tok
            if deps.get(k, 0) < v:
                deps[k] = v
        for b in reads:
            add(b.w)
        for b in writes:
            if b.w is not None and b.w[0] != e:
                add(b.w)
            for t in b.r:
                if t[0] != e:
                    add(t)
        eng = self.eng[e]
        for k, v in deps.items():
            if k == e and (e == 'pe' or not self.SAME_ENGINE_SYNC):
                continue
            if self.seen[e].get(k, 0) >= v:
                continue
            eng.wait_ge(self.sem[k], v)
            self.seen[e][k] = v

    def _post(self, tok, reads, writes):
        for b in reads:
            b.r.append(tok)
            if len(b.r) > 48:
                d = {}
                for k, v in b.r:
                    if d.get(k, 0) < v:
                        d[k] = v
                b.r = list(d.items())
        for b in writes:
            b.w = tok
            b.r = []

    def op(self, e, name, R=(), W=(), **kw):
        self._deps(e, R, W)
        ins = getattr(self.eng[e], name)(**kw)
        self.cnt[e] += 1
        ins.then_inc(self.sem[e], 1)
        tok = (e, self.cnt[e])
        self._post(tok, R, W)
        self.n_ins += 1
        return tok

    def dve(self, name, R=(), W=(), **kw):
        return self.op('dve', name, R, W, **kw)

    def act(self, R=(), W=(), **kw):
        return self.op('act', 'activation', R, W, **kw)

    def pe(self, name, R=(), W=(), **kw):
        return self.op('pe', name, R, W, **kw)

    def pool(self, name, R=(), W=(), **kw):
        return self.op('pool', name, R, W, **kw)

    def dma(self, q, out, in_, R=(), W=(), **kw):
        self._deps(q, R, W)
        lst, i = self.dq[q]
        k = lst[i % len(lst)]
        self.dq[q][1] = i + 1
        ins = self.eng[q].dma_start(out=out, in_=in_, **kw)
        self.cnt[k] += 16
        ins.then_inc(self.sem[k], 16)
        tok = (k, self.cnt[k])
        self._post(tok, R, W)
        self.n_ins += 1
        return tok

    def indirect(self, R=(), W=(), **kw):
        q = 'pool'
        self._deps(q, R, W)
        lst, i = self.dq[q]
        k = lst[i % len(lst)]
        self.dq[q][1] = i + 1
        ins = self.eng[q].indirect_dma_start(**kw)
        self.cnt[k] += 16
        ins.then_inc(self.sem[k], 16)
        tok = (k, self.cnt[k])
        self._post(tok, R, W)
        self.n_ins += 1
        return tok

    def barrier(self):
        for e, eng in self.eng.items():
            for k, v in self.cnt.items():
                if v == 0 or k == e and e == 'pe':
                    continue
                if self.seen[e].get(k, 0) >= v:
                    continue
                eng.wait_ge(self.sem[k], v)
                self.seen[e][k] = v


def bc(ap, shape):
    return ap.to_broadcast(list(shape))


def rstd_calc(S, ss, out, inv_n, post=1.0):
    sb, sap = ss
    ob, oap = out
    S.dve('tensor_scalar', R=[sb], W=[ob], out=oap, in0=sap, scalar1=inv_n, scalar2=EPS,
          op0=ALU.mult, op1=ALU.add)
    S.act(R=[ob], W=[ob], out=oap, in_=oap, func=AF.Sqrt)
    S.dve('reciprocal', R=[ob], W=[ob], out=oap, in_=oap)
    if post != 1.0:
        S.dve('tensor_scalar', R=[ob], W=[ob], out=oap, in0=oap, scalar1=post, scalar2=None, op0=ALU.mult)


def load_consts(S, es, io):
    c = {}
    c['idf'] = S.sbuf(es, 'idf', [128, 128], F32)
    c['idb'] = S.sbuf(es, 'idb', [128, 128], BF16)
    S.dma('sp', c['idf'][:], io['ident'], W=[c['idf']])
    S.dve('tensor_copy', R=[c['idf']], W=[c['idb']], out=c['idb'][:], in_=c['idf'][:])
    return c


def norm_T(S, xt, xnT_dst, dstbuf, wk, c):
    junk, ss, rstd, xn, tp = wk['junk'], wk['ss'], wk['rstd'], wk['xn'], wk['tp']
    S.act(R=[xt], W=[junk, ss], out=junk[:, 0:D], in_=xt[:], func=AF.Square, accum_out=ss[:, 0:1])
    rstd_calc(S, (ss, ss[:, 0:1]), (rstd, rstd[:, 0:1]), 1.0 / D)
    S.act(R=[xt, rstd], W=[xn], out=xn[:, 0:1024], in_=xt[:, 0:1024], func=AF.Copy, scale=rstd[:, 0:1])
    S.dve('tensor_scalar', R=[xt, rstd], W=[xn], out=xn[:, 1024:2048], in0=xt[:, 1024:2048],
          scalar1=rstd[:, 0:1], scalar2=None, op0=ALU.mult)
    for half in range(2):
        t = tp[half]
        for k in range(8):
            kk = half * 8 + k
            S.pe('transpose', R=[xn, c['idb']], W=[t], out=t[:, k * 128:(k + 1) * 128],
                 in_=xn[:, kk * 128:(kk + 1) * 128], identity=c['idb'][:])
        src = t[:, :].rearrange('p (k t) -> p k t', k=8)
        if half == 0:
            S.act(R=[t], W=[dstbuf], out=xnT_dst[:, 0:8, :], in_=src, func=AF.Copy)
        else:
            S.dve('tensor_copy', R=[t], W=[dstbuf], out=xnT_dst[:, 8:16, :], in_=src)


def load_w_scaled(S, dst, dst_ap_fn, w_dram, g, nk, c0, c1, stg, q='sp'):
    for k in range(nk):
        st = stg[k % len(stg)]
        n = c1 - c0
        S.dma(q, st[:, 0:n], w_dram[k * 128:(k + 1) * 128, c0:c1], W=[st])
        if k % 2 == 0:
            S.dve('tensor_scalar', R=[st, g], W=[dst], out=dst_ap_fn(k), in0=st[:, 0:n],
                  scalar1=g[:, k:k + 1], scalar2=None, op0=ALU.mult)
        else:
            S.act(R=[st, g], W=[dst], out=dst_ap_fn(k), in_=st[:, 0:n], func=AF.Copy, scale=g[:, k:k + 1])


def emit_A(S, nc, T, io):
    NT = T // 128
    G = min(512, T)
    NG = T // G
    with ExitStack() as es:
        c = load_consts(S, es, io)
        xnT_all = S.sbuf(es, 'xnT_all', [128, 16, T], BF16)
        with ExitStack() as e1:
            w1 = S.sbuf(e1, 'w1', [128, 16, 832], BF16)
            wuq = S.sbuf(e1, 'wuq', [128, 4, 1536], BF16)
            wukv = S.sbuf(e1, 'wukv', [128, 2, 2048], BF16)
            stg = [S.sbuf(e1, 'stg', [128, 2048], F32) for _ in range(2)]
            g_mix = S.sbuf(e1, 'g_mix', [128, 16], F32)
            g_q = S.sbuf(e1, 'g_q', [128, 4], F32)
            g_kv = S.sbuf(e1, 'g_kv', [128, 2], F32)
            gqh = S.sbuf(e1, 'gqh', [128, 192], F32)
            gkh = S.sbuf(e1, 'gkh', [128, 192], F32)
            S.dma('sp', g_mix[:], io['g_mix'], W=[g_mix])
            S.dma('sp', g_q[:], io['g_q'], W=[g_q])
            S.dma('sp', g_kv[:], io['g_kv'], W=[g_kv])
            S.dma('sp', gqh[:], io['gqh'].partition_broadcast(128), W=[gqh])
            S.dma('sp', gkh[:], io['gkh'].partition_broadcast(128), W=[gkh])
            load_w_scaled(S, w1, lambda k: w1[:, k, :], io['w_in'], g_mix, 16, 0, 832, stg)
            load_w_scaled(S, wuq, lambda k: wuq[:, k, :], io['w_uq'], g_q, 4, 0, 1536, stg)
            load_w_scaled(S, wukv, lambda k: wukv[:, k, :], io['w_ukv'], g_kv, 2, 0, 2048, stg)
            xts = [S.sbuf(e1, 'xt', [128, D], F32) for _ in range(2)]
            wk = {'junk': S.sbuf(e1, 'junk', [128, D], BF16), 'ss': S.sbuf(e1, 'ss', [128, 4], F32),
                  'rstd': S.sbuf(e1, 'rstd', [128, 4], F32), 'xn': S.sbuf(e1, 'xn', [128, D], BF16),
                  'tp': [S.psum(e1, 'tp', [128, 1024], BF16) for _ in range(2)]}
            zps = S.psum(e1, 'zps', [128, 1024], F32)
            big = S.psum(e1, 'big', [128, 2048], F32)
            zt = S.sbuf(e1, 'zt', [128, 832], F32)
            cs = S.sbuf(e1, 'cs', [128, 32], F32)
            sn = S.sbuf(e1, 'sn', [128, 32], F32)
            sm = S.sbuf(e1, 'sm', [128, 16], F32)
            cn = S.sbuf(e1, 'cn', [128, 512], BF16)
            cT = S.sbuf(e1, 'cT', [128, 4, 128], BF16)
            sq = S.sbuf(e1, 'sq', [128, 1536], F32)
            ssh = S.sbuf(e1, 'ssh', [128, 8], F32)
            rsh = S.sbuf(e1, 'rsh', [128, 8], F32)
            qn = S.sbuf(e1, 'qn', [128, 8, 192], F32)
            qf = S.sbuf(e1, 'qf', [128, 8, 192], BF16)
            tr = S.sbuf(e1, 'tr', [128, 8, 32], F32)
            tr2 = S.sbuf(e1, 'tr2', [128, 8, 32], F32)
            kr = S.sbuf(e1, 'kr', [128, 64], F32)
            kpg = S.sbuf(e1, 'kpg', [128, 64], F32)
            vf = S.sbuf(e1, 'vf', [128, 8, 128], BF16)
            oTn = S.sbuf(e1, 'oTn', [128, 8, 128], BF16)
            oTr = S.sbuf(e1, 'oTr', [64, 8, 128], BF16)
            tp = wk['tp']

            def head_T_out(src, dst_dram, i):
                for h in range(8):
                    S.pe('transpose', R=[src, c['idb']], W=[tp[0]], out=tp[0][:, h * 128:(h + 1) * 128],
                         in_=src[:, h, 0:128], identity=c['idb'][:])
                for h in range(8):
                    S.pe('transpose', R=[src, c['idb']], W=[tp[1]], out=tp[1][0:64, h * 128:(h + 1) * 128],
                         in_=src[:, h, 128:192], identity=c['idb'][:])
                S.act(R=[tp[0]], W=[oTn], out=oTn[:], in_=tp[0][:, :].rearrange('p (h t) -> p h t', h=8),
                      func=AF.Copy)
                S.dve('tensor_copy', R=[tp[1]], W=[oTr], out=oTr[:],
                      in_=tp[1][0:64, :].rearrange('p (h t) -> p h t', h=8))
                S.dma('pool', dst_dram[:, 0:128, i * 128:(i + 1) * 128].rearrange('h d t -> d h t'), oTn[:], R=[oTn])
                S.dma('pool', dst_dram[:, 128:192, i * 128:(i + 1) * 128].rearrange('h d t -> d h t'), oTr[:], R=[oTr])

            def rope(src, x1, x2, o1, o2):
                sbuf_src, sbuf_dst = src
                shp = list(x1.shape)
                cb = cs[:, :].unsqueeze(1).to_broadcast(shp) if len(shp) == 3 else cs[:, :]
                sb_ = sn[:, :].unsqueeze(1).to_broadcast(shp) if len(shp) == 3 else sn[:, :]
                a1 = tr[:, 0:shp[1], :] if len(shp) == 3 else tr[:, 0, :]
                a2 = tr2[:, 0:shp[1], :] if len(shp) == 3 else tr2[:, 0, :]
                S.dve('tensor_tensor', R=[sbuf_src, cs], W=[tr], out=a1, in0=x1, in1=cb, op=ALU.mult)
                S.dve('tensor_tensor', R=[sbuf_src, sn], W=[tr2], out=a2, in0=x2, in1=sb_, op=ALU.mult)
                S.dve('tensor_tensor', R=[tr, tr2], W=[sbuf_dst], out=o1, in0=a1, in1=a2, op=ALU.subtract)
                S.dve('tensor_tensor', R=[sbuf_src, sn], W=[tr], out=a1, in0=x1, in1=sb_, op=ALU.mult)
                S.dve('tensor_tensor', R=[sbuf_src, cs], W=[tr2], out=a2, in0=x2, in1=cb, op=ALU.mult)
                S.dve('tensor_tensor', R=[tr, tr2], W=[sbuf_dst], out=o2, in0=a1, in1=a2, op=ALU.add)

            for i in range(NT):
                xt = xts[i % 2]
                S.dma('sp', xt[:], io['x'][i * 128:(i + 1) * 128, :], W=[xt])
                S.dma('sp', cs[:], io['cos'][i * 128:(i + 1) * 128, :], W=[cs])
                S.dma('sp', sn[:], io['sin'][i * 128:(i + 1) * 128, :], W=[sn])
                norm_T(S, xt, xnT_all[:, :, i * 128:(i + 1) * 128], xnT_all, wk, c)
                for (c0, c1) in ((0, 512), (512, 832)):
                    for k in range(16):
                        S.pe('matmul', R=[xnT_all, w1], W=[zps], out=zps[:, c0:c1],
                             lhsT=xnT_all[:, k, i * 128:(i + 1) * 128], rhs=w1[:, k, c0:c1],
                             start=(k == 0), stop=(k == 15))
                S.act(R=[zps], W=[zt], out=zt[:, 0:832], in_=zps[:, 0:832], func=AF.Copy)
                S.act(R=[zt], W=[wk['junk'], sm], out=wk['junk'][:, 0:512], in_=zt[:, 0:512], func=AF.Square,
                      accum_out=sm[:, 0:1])
                rstd_calc(S, (sm, sm[:, 0:1]), (sm, sm[:, 1:2]), 1.0 / 512)
                S.dve('tensor_scalar', R=[zt, sm], W=[cn], out=cn[:, 0:512], in0=zt[:, 0:512],
                      scalar1=sm[:, 1:2], scalar2=None, op0=ALU.mult)
                for k in range(4):
                    S.pe('transpose', R=[cn, c['idb']], W=[tp[0]], out=tp[0][:, k * 128:(k + 1) * 128],
                         in_=cn[:, k * 128:(k + 1) * 128], identity=c['idb'][:])
                S.dve('tensor_copy', R=[tp[0]], W=[cT], out=cT[:],
                      in_=tp[0][:, 0:512].rearrange('p (k t) -> p k t', k=4))
                for cb_ in range(3):
                    for k in range(4):
                        S.pe('matmul', R=[cT, wuq], W=[big], out=big[:, cb_ * 512:(cb_ + 1) * 512],
                             lhsT=cT[:, k, :], rhs=wuq[:, k, cb_ * 512:(cb_ + 1) * 512],
                             start=(k == 0), stop=(k == 3))
                S.act(R=[big], W=[sq], out=sq[:, 0:1536], in_=big[:, 0:1536], func=AF.Square)
                S.dve('tensor_reduce', R=[sq], W=[ssh], out=ssh[:],
                      in_=sq[:, 0:1536].rearrange('p (h d) -> p h d', h=8), axis=AX.X, op=ALU.add)
                rstd_calc(S, (ssh, ssh[:]), (rsh, rsh[:]), 1.0 / 192, post=192.0 ** -0.5)
                S.dve('tensor_tensor', R=[big, rsh], W=[qn], out=qn[:],
                      in0=big[:, 0:1536].rearrange('p (h d) -> p h d', h=8),
                      in1=bc(rsh[:, :].unsqueeze(2), [128, 8, 192]), op=ALU.mult)
                S.dve('tensor_tensor', R=[qn, gqh], W=[qn], out=qn[:], in0=qn[:],
                      in1=bc(gqh[:, :].unsqueeze(1), [128, 8, 192]), op=ALU.mult)
                S.act(R=[qn], W=[qf], out=qf[:, :, 0:128], in_=qn[:, :, 0:128], func=AF.Copy)
                rope((qn, qf), qn[:, :, 128:160], qn[:, :, 160:192], qf[:, :, 128:160], qf[:, :, 160:192])
                head_T_out(qf, io['QT'], i)
                S.act(R=[zt], W=[wk['junk'], sm], out=wk['junk'][:, 0:256], in_=zt[:, 512:768], func=AF.Square,
                      accum_out=sm[:, 2:3])
                rstd_calc(S, (sm, sm[:, 2:3]), (sm, sm[:, 3:4]), 1.0 / 256)
                S.dve('tensor_scalar', R=[zt, sm], W=[cn], out=cn[:, 0:256], in0=zt[:, 512:768],
                      scalar1=sm[:, 3:4], scalar2=None, op0=ALU.mult)
                for k in range(2):
                    S.pe('transpose', R=[cn, c['idb']], W=[tp[0]], out=tp[0][:, k * 128:(k + 1) * 128],
                         in_=cn[:, k * 128:(k + 1) * 128], identity=c['idb'][:])
                S.dve('tensor_copy', R=[tp[0]], W=[cT], out=cT[:, 0:2, :],
                      in_=tp[0][:, 0:256].rearrange('p (k t) -> p k t', k=2))
                for cb_ in range(4):
                    for k in range(2):
                        S.pe('matmul', R=[cT, wukv], W=[big], out=big[:, cb_ * 512:(cb_ + 1) * 512],
                             lhsT=cT[:, k, :], rhs=wukv[:, k, cb_ * 512:(cb_ + 1) * 512],
                             start=(k == 0), stop=(k == 1))
                kvv = big[:, :].rearrange('p (h d) -> p h d', h=8)
                S.act(R=[big], W=[sq], out=sq[:, 0:1024].rearrange('p (h d) -> p h d', h=8),
                      in_=kvv[:, :, 0:128], func=AF.Square)
                S.dve('tensor_reduce', R=[sq], W=[ssh], out=ssh[:],
                      in_=sq[:, 0:1024].rearrange('p (h d) -> p h d', h=8), axis=AX.X, op=ALU.add)
                S.act(R=[zt], W=[wk['junk'], sm], out=wk['junk'][:, 0:64], in_=zt[:, 768:832], func=AF.Square,
                      accum_out=sm[:, 4:5])
                S.dve('tensor_scalar', R=[ssh, sm], W=[ssh], out=ssh[:], in0=ssh[:], scalar1=sm[:, 4:5],
                      scalar2=None, op0=ALU.add)
                rstd_calc(S, (ssh, ssh[:]), (rsh, rsh[:]), 1.0 / 192)
                S.dve('tensor_tensor', R=[big, rsh], W=[qn], out=qn[:, :, 0:128], in0=kvv[:, :, 0:128],
                      in1=bc(rsh[:, :].unsqueeze(2), [128, 8, 128]), op=ALU.mult)
                S.dve('tensor_tensor', R=[qn, gkh], W=[qf], out=qf[:, :, 0:128], in0=qn[:, :, 0:128],
                      in1=bc(gkh[:, 0:128].unsqueeze(1), [128, 8, 128]), op=ALU.mult)
                S.dve('tensor_tensor', R=[zt, gkh], W=[kpg], out=kpg[:], in0=zt[:, 768:832], in1=gkh[:, 128:192],
                      op=ALU.mult)
                rope((kpg, kr), kpg[:, 0:32], kpg[:, 32:64], kr[:, 0:32], kr[:, 32:64])
                S.dve('tensor_tensor', R=[kr, rsh], W=[qf], out=qf[:, :, 128:192],
                      in0=bc(kr[:, :].unsqueeze(1), [128, 8, 64]), in1=bc(rsh[:, :].unsqueeze(2), [128, 8, 64]),
                      op=ALU.mult)
                S.act(R=[big], W=[vf], out=vf[:], in_=kvv[:, :, 128:256], func=AF.Copy)
                S.dma('pool', io['V'][:, :, i, :].rearrange('h p d -> p h d'), vf[:], R=[vf])
                head_T_out(qf, io['KT'], i)
        S.barrier()
        with ExitStack() as e2:
            w2 = S.sbuf(e2, 'w2', [128, 16, 2048], BF16)
            wrg = S.sbuf(e2, 'wrg', [128, 8, 128], BF16)
            wig = S.sbuf(e2, 'wig', [128, 8, 128], BF16)
            pv = S.sbuf(e2, 'pv', [128, 8, 8], F32)
            c1 = S.sbuf(e2, 'c1', [128, 8], F32)
            c2 = S.sbuf(e2, 'c2', [128, 8], F32)
            xhT = S.sbuf(e2, 'xhT', [128, 16, 128], BF16)
            g_mix = S.sbuf(e2, 'g_mix2', [128, 16], F32)
            e2a = ExitStack()
            stg = [S.sbuf(e2a, 'stg2', [128, 2048], F32) for _ in range(2)]
            S.dma('sp', g_mix[:], io['g_mix'], W=[g_mix])
            load_w_scaled(S, w2, lambda k: w2[:, k, :], io['w_in'], g_mix, 16, 832, 2880, stg)
            S.dma('sp', stg[0][:, 0:1024].rearrange('c (n d) -> c n d', n=8), io['w_rg'].rearrange('n c d -> c n d'),
                  W=[stg[0]])
            S.dve('tensor_copy', R=[stg[0]], W=[wrg], out=wrg[:], in_=stg[0][:, 0:1024].rearrange('c (n d) -> c n d', n=8))
            S.dma('sp', stg[1][:, 0:1024].rearrange('c (n d) -> c n d', n=8), io['w_ig'].rearrange('n c d -> c n d'),
                  W=[stg[1]])
            S.dve('tensor_copy', R=[stg[1]], W=[wig], out=wig[:], in_=stg[1][:, 0:1024].rearrange('c (n d) -> c n d', n=8))
            S.dma('sp', pv[:], io['lru_vec'], W=[pv])
            S.act(R=[pv], W=[c1], out=c1[:], in_=pv[:, :, 7], func=AF.Exp, scale=-1.0)
            S.act(R=[c1], W=[c2], out=c2[:], in_=c1[:], func=AF.Ln, bias=1.0)
            S.dve('tensor_scalar', R=[c2], W=[c1], out=c1[:], in0=c2[:], scalar1=-8.0, scalar2=None, op0=ALU.mult)
            S.dve('tensor_scalar', R=[c1], W=[c2], out=c2[:], in0=c1[:], scalar1=2.0, scalar2=None, op0=ALU.mult)
            xt = S.sbuf(e2a, 'xt2', [128, D], F32)
            wk = {'junk': S.sbuf(e2a, 'junk2', [128, D], BF16), 'ss': S.sbuf(e2a, 'ss2', [128, 4], F32),
                  'rstd': S.sbuf(e2a, 'rstd2', [128, 4], F32), 'xn': S.sbuf(e2a, 'xn2', [128, D], BF16),
                  'tp': [S.psum(e2, 'tp2', [128, 1024], BF16) for _ in range(2)]}
            S.dma('sp', xt[:], io['xh'], W=[xt])
            norm_T(S, xt, xhT[:, :, :], xhT, wk, c)
            e2a.close()
            S.barrier()
            xrb = S.sbuf(e2, 'xrb', [128, 8, G + 3], F32)
            hc = S.sbuf(e2, 'hc', [128, 8], F32)
            pc = S.sbuf(e2, 'pc', [128, 8], F32)
            zeros = S.sbuf(e2, 'zeros', [128, G], F32)
            S.dve('memset', W=[zeros], ap=zeros[:], constant=0.0)
            S.dve('memset', W=[hc], ap=hc[:], constant=0.0)
            S.dve('memset', W=[pc], ap=pc[:], constant=1.0)
            ps_x = [S.psum(e2, 'ps_x', [128, 512], F32) for _ in range(2)]
            ps_g = [S.psum(e2, 'ps_g', [128, 512], F32) for _ in range(2)]
            ps_r = S.psum(e2, 'ps_r', [128, 512], F32)
            ps_i = S.psum(e2, 'ps_i', [128, 512], F32)

            def mk(n_):
                return [S.sbuf(e2, n_, [128, G], F32) for _ in range(2)]
            xc_, r_, i_, a_, a2_, hl_, pl_, gg_ = (mk('xc'), mk('r'), mk('i'), mk('a'), mk('a2'), mk('hl'),
                                                   mk('pl'), mk('gg'))
            xcb_ = [S.sbuf(e2, 'xcb', [128, G], BF16) for _ in range(2)]
            it = 0
            for gi in range(NG):
                for n in range(8):
                    b = it % 2
                    it += 1
                    xc, r, ig, a, a2, hl, pl, gg, xcb = (xc_[b], r_[b], i_[b], a_[b], a2_[b], hl_[b], pl_[b],
                                                         gg_[b], xcb_[b])
                    px, pg = ps_x[b], ps_g[b]
                    cx = n * 128
                    cg = 1024 + n * 128
                    if gi == 0:
                        for k in range(16):
                            S.pe('matmul', R=[w2, xhT], W=[ps_r], out=ps_r[:, 0:3], lhsT=w2[:, k, cx:cx + 128],
                                 rhs=xhT[:, k, 125:128], start=(k == 0), stop=(k == 15))
                        S.dve('tensor_copy', R=[ps_r], W=[xrb], out=xrb[:, n, 0:3], in_=ps_r[:, 0:3])
                    else:
                        S.dve('tensor_copy', R=[xrb], W=[xrb], out=xrb[:, n, 0:3], in_=xrb[:, n, G:G + 3])
                    for k in range(16):
                        S.pe('matmul', R=[w2, xnT_all], W=[px], out=px[:, 0:G], lhsT=w2[:, k, cx:cx + 128],
                             rhs=xnT_all[:, k, gi * G:(gi + 1) * G], start=(k == 0), stop=(k == 15))
                    for k in range(16):
                        S.pe('matmul', R=[w2, xnT_all], W=[pg], out=pg[:, 0:G], lhsT=w2[:, k, cg:cg + 128],
                             rhs=xnT_all[:, k, gi * G:(gi + 1) * G], start=(k == 0), stop=(k == 15))
                    S.act(R=[px], W=[xrb], out=xrb[:, n, 3:3 + G], in_=px[:, 0:G], func=AF.Copy)
                    S.dve('tensor_scalar', R=[xrb, pv], W=[xc], out=xc[:], in0=xrb[:, n, 0:G], scalar1=pv[:, n, 0:1],
                          scalar2=pv[:, n, 4:5], op0=ALU.mult, op1=ALU.add)
                    for j in (1, 2, 3):
                        S.dve('scalar_tensor_tensor', R=[xrb, pv, xc], W=[xc], out=xc[:], in0=xrb[:, n, j:j + G],
                              scalar=pv[:, n, j:j + 1], in1=xc[:], op0=ALU.mult, op1=ALU.add)
                    S.act(R=[xc], W=[xcb], out=xcb[:], in_=xc[:], func=AF.Copy)
                    S.pe('matmul', R=[wrg, xcb], W=[ps_r], out=ps_r[:, 0:G], lhsT=wrg[:, n, :], rhs=xcb[:],
                         start=True, stop=True)
                    S.pe('matmul', R=[wig, xcb], W=[ps_i], out=ps_i[:, 0:G], lhsT=wig[:, n, :], rhs=xcb[:],
                         start=True, stop=True)
                    S.act(R=[ps_r, pv], W=[r], out=r[:], in_=ps_r[:, 0:G], func=AF.Sigmoid, bias=pv[:, n, 5:6])
                    S.act(R=[ps_i, pv], W=[ig], out=ig[:], in_=ps_i[:, 0:G], func=AF.Sigmoid, bias=pv[:, n, 6:7])
                    S.act(R=[r, c1], W=[a], out=a[:], in_=r[:], func=AF.Exp, scale=c1[:, n:n + 1])
                    S.act(R=[r, c2], W=[a2], out=a2[:], in_=r[:], func=AF.Exp, scale=c2[:, n:n + 1])
                    S.act(R=[pg], W=[gg], out=gg[:], in_=pg[:, 0:G], func=AF.Gelu)
                    S.dve('tensor_scalar', R=[a2], W=[a2], out=a2[:], in0=a2[:], scalar1=-1.0, scalar2=1.0,
                          op0=ALU.mult, op1=ALU.add)
                    S.dve('tensor_scalar', R=[a2], W=[a2], out=a2[:], in0=a2[:], scalar1=0.0, scalar2=None,
                          op0=ALU.max)
                    S.act(R=[a2], W=[a2], out=a2[:], in_=a2[:], func=AF.Sqrt)
                    S.dve('tensor_tensor', R=[ig, xc], W=[ig], out=ig[:], in0=ig[:], in1=xc[:], op=ALU.mult)
                    S.dve('tensor_tensor', R=[ig, a2], W=[ig], out=ig[:], in0=ig[:], in1=a2[:], op=ALU.mult)
                    S.dve('tensor_tensor_scan', R=[a, ig, hc], W=[hl], out=hl[:], data0=a[:], data1=ig[:],
                          initial=hc[:, n:n + 1], op0=ALU.mult, op1=ALU.add)
                    S.dve('tensor_tensor_scan', R=[a, zeros, pc], W=[pl], out=pl[:], data0=a[:], data1=zeros[:],
                          initial=pc[:, n:n + 1], op0=ALU.mult, op1=ALU.add)
                    S.dve('tensor_copy', R=[hl], W=[hc], out=hc[:, n:n + 1], in_=hl[:, G - 1:G])
                    S.dve('tensor_copy', R=[pl], W=[pc], out=pc[:, n:n + 1], in_=pl[:, G - 1:G])
                    S.dve('tensor_tensor', R=[hl, gg], W=[hl], out=hl[:], in0=hl[:], in1=gg[:], op=ALU.mult)
                    S.dve('tensor_tensor', R=[pl, gg], W=[pl], out=pl[:], in0=pl[:], in1=gg[:], op=ALU.mult)
                    S.dma('pool', io['Yl'][n, :, gi * G:(gi + 1) * G], hl[:], R=[hl])
                    S.dma('pool', io['Yp'][n, :, gi * G:(gi + 1) * G], pl[:], R=[pl])
            S.dma('pool', io['carry'][0], pc[:], R=[pc])
            S.dma('pool', io['carry'][1], hc[:], R=[hc])
        S.barrier()


def dram_in(nc, name, shape, dt=F32):
    return nc.dram_tensor(name, list(shape), dt, kind="ExternalInput").ap()


def dram_out(nc, name, shape, dt=F32):
    return nc.dram_tensor(name, list(shape), dt, kind="ExternalOutput").ap()


def build_A(T):
    nc = bass.Bass("TRN2", target_bir_lowering=False)
    io = {}
    for name, shape in (('x', [T, D]), ('xh', [128, D]), ('cos', [T, 32]), ('sin', [T, 32]), ('ident', [128, 128]),
                        ('g_mix', [128, 16]), ('g_q', [128, 4]), ('g_kv', [128, 2]), ('gqh', [192]), ('gkh', [192]),
                        ('w_in', [D, DIN]), ('w_uq', [512, 1536]), ('w_ukv', [256, 2048]),
                        ('w_rg', [8, 128, 128]), ('w_ig', [8, 128, 128]), ('lru_vec', [128, 8, 8])):
        io[name] = dram_in(nc, name, shape)
    io['QT'] = dram_out(nc, 'QT', [NH, 192, T], BF16)
    io['KT'] = dram_out(nc, 'KT', [NH, 192, T], BF16)
    io['V'] = dram_out(nc, 'V', [NH, 128, T // 128, 128], BF16)
    io['Yl'] = dram_out(nc, 'Yl', [8, 128, T])
    io['Yp'] = dram_out(nc, 'Yp', [8, 128, T])
    io['carry'] = dram_out(nc, 'carry', [2, 128, 8])
    with ExitStack() as es:
        S = Sched(nc, es)
        emit_A(S, nc, T, io)
        S.barrier()
    return nc


def col_layout(v, k):
    return np.ascontiguousarray(np.asarray(v, np.float32).reshape(k, 128).T)


def rope_tables(S_len):
    inv = (1.0 / (np.float32(10000.0) ** (np.arange(0, 64, 2, dtype=np.float32) / np.float32(64)))).astype(np.float32)
    ang = np.arange(S_len, dtype=np.float32)[:, None] * inv[None, :]
    return np.cos(ang).astype(np.float32), np.sin(ang).astype(np.float32)


def inputs_A(inp, l, xs, T):
    S_len = CPB * T
    cos, sin = rope_tables(S_len)
    lru_vec = np.stack([col_layout(inp['conv_w'][l][j], 8) for j in range(4)] +
                       [col_layout(inp[n][l], 8) for n in ('conv_b', 'b_rgate', 'b_igate', 'lru_lambda')], axis=2)
    shared = {
        'ident': np.eye(128, dtype=np.float32),
        'g_mix': col_layout(inp['mix_norm_g'][l], 16), 'g_q': col_layout(inp['q_lora_norm_g'][l], 4),
        'g_kv': col_layout(inp['kv_lora_norm_g'][l], 2),
        'gqh': np.ascontiguousarray(inp['att_q_norm_g'][l]), 'gkh': np.ascontiguousarray(inp['att_k_norm_g'][l]),
        'w_in': np.ascontiguousarray(inp['w_in'][l]), 'w_uq': np.ascontiguousarray(inp['w_uq'][l]),
        'w_ukv': np.ascontiguousarray(inp['w_ukv'][l]),
        'w_rg': np.ascontiguousarray(inp['w_rgate'][l]), 'w_ig': np.ascontiguousarray(inp['w_igate'][l]),
        'lru_vec': np.ascontiguousarray(lru_vec),
    }
    maps = []
    for cidx in range(NCORES):
        j = cidx % CPB
        m = dict(shared)
        m['x'] = xs[cidx]
        m['xh'] = xs[cidx - 1][T - 128:T] if j > 0 else np.zeros((128, D), np.float32)
        m['cos'] = np.ascontiguousarray(cos[j * T:(j + 1) * T])
        m['sin'] = np.ascontiguousarray(sin[j * T:(j + 1) * T])
        maps.append(m)
    return maps


def emit_B_attn(S, nc, T, io, c, yattT, ssatt):
    NT = T // 128
    G = min(512, T)
    NG = T // G
    GT = G // 128
    NOTH = CPB - 1
    with ExitStack() as es:
        tri = S.sbuf(es, 'tri', [128, 128], F32)
        kb = S.sbuf(es, 'kb', [128, NOTH], F32)
        onesf = S.sbuf(es, 'onesf', [128, 128], F32)
        S.dma('sp', tri[:], io['tri'], W=[tri])
        S.dma('sp', kb[:], io['kb'], W=[kb])
        S.dve('memset', W=[onesf], ap=onesf[:], constant=1.0)
        hb = []
        for _ in range(2):
            d_ = {
                'qn': S.sbuf(es, 'qn', [128, T], BF16), 'qr': S.sbuf(es, 'qr', [128, T], BF16),
                'kn': S.sbuf(es, 'kn', [128, T], BF16), 'kr': S.sbuf(es, 'kr', [128, T], BF16),
                'v': S.sbuf(es, 'v', [128, NT, 128], BF16),
                'Kn': S.sbuf(es, 'Kn', [128, NOTH, T], BF16), 'Kr': S.sbuf(es, 'Kr', [128, NOTH, T], BF16),
                'V': S.sbuf(es, 'V', [128, NOTH, NT, 128], BF16)}
            for n_ in ('qr', 'kr', 'Kr'):
                S.pool('memset', W=[d_[n_]], ap=d_[n_][:], constant=0.0)
            hb.append(d_)
        ps = [S.psum(es, 'ps', [128, 512], F32) for _ in range(3)]
        po = [S.psum(es, 'po', [128, 512], F32) for _ in range(2)]
        pd = S.psum(es, 'pd', [128, 512], F32)
        pss = S.psum(es, 'pss', [128, 512], F32)
        pT = [S.sbuf(es, 'pT', [128, G], BF16) for _ in range(4)]
        acc = [S.sbuf(es, 'acc', [128, G], F32) for _ in range(2)]
        rec = S.sbuf(es, 'rec', [128, G], F32)
        yf = S.sbuf(es, 'yf', [128, G], F32)
        sq = S.sbuf(es, 'sq', [128, G], F32)
        it = 0
        gi = 0
        for h in range(NH):
            B = hb[h % 2]
            S.dma('sp', B['qn'][:], io['QT'][h, 0:128, :], W=[B['qn']])
            S.dma('sp', B['qr'][0:64, :], io['QT'][h, 128:192, :], W=[B['qr']])
            S.dma('sp', B['kn'][:], io['KT'][h, 0:128, :], W=[B['kn']])
            S.dma('sp', B['kr'][0:64, :], io['KT'][h, 128:192, :], W=[B['kr']])
            S.dma('sp', B['v'][:], io['V'][h], W=[B['v']])
            S.dma('sp', B['Kn'][:], io['KT_oth'][:, h, 0:128, :].rearrange('s d t -> d s t'), W=[B['Kn']])
            S.dma('sp', B['Kr'][0:64], io['KT_oth'][:, h, 128:192, :].rearrange('s d t -> d s t'), W=[B['Kr']])
            for s in range(NOTH):
                S.dma('sp', B['V'][:, s], io['V_oth'][s, h], W=[B['V']])
            for g in range(NG):
                a_ = acc[gi % 2]
                o_ = po[gi % 2]
                gi += 1
                items = []
                for s in range(NOTH):
                    for kt in range(NT):
                        items.append((B['Kn'][:, s, kt * 128:(kt + 1) * 128], B['Kr'][:, s, kt * 128:(kt + 1) * 128],
                                      B['V'][:, s, kt, :], 0, False, kb[:, s:s + 1], ('Kn', 'Kr', 'V')))
                for kt in range((g + 1) * GT):
                    d = max(0, kt - g * GT)
                    items.append((B['kn'][:, kt * 128:(kt + 1) * 128], B['kr'][:, kt * 128:(kt + 1) * 128],
                                  B['v'][:, kt, :], d * 128, kt >= g * GT, None, ('kn', 'kr', 'v')))
                base = it
                it += len(items)

                def qk(idx):
                    kn_ap, kr_ap, v_ap, off, diag, bias, names = items[idx]
                    p_ = ps[(base + idx) % 3]
                    N = G - off
                    q0 = g * G + off
                    S.pe('matmul', R=[B[names[0]], B['qn']], W=[p_], out=p_[:, 0:N], lhsT=kn_ap,
                         rhs=B['qn'][:, q0:q0 + N], start=True, stop=False)
                    S.pe('matmul', R=[B[names[1]], B['qr']], W=[p_], out=p_[:, 0:N], lhsT=kr_ap,
                         rhs=B['qr'][:, q0:q0 + N], start=False, stop=True)

                def rest(idx):
                    kn_ap, kr_ap, v_ap, off, diag, bias, names = items[idx]
                    p_ = ps[(base + idx) % 3]
                    t_ = pT[(base + idx) % 4]
                    N = G - off
                    if bias is None:
                        S.act(R=[p_], W=[t_], out=t_[:, 0:N], in_=p_[:, 0:N], func=AF.Exp)
                    else:
                        S.act(R=[p_, kb], W=[t_], out=t_[:, 0:N], in_=p_[:, 0:N], func=AF.Exp, bias=bias)
                    if diag:
                        S.dve('tensor_tensor', R=[t_, tri], W=[t_], out=t_[:, 0:128], in0=t_[:, 0:128], in1=tri[:],
                              op=ALU.mult)
                    if idx == 0:
                        S.dve('tensor_copy', R=[t_], W=[a_], out=a_[:, 0:G], in_=t_[:, 0:G])
                    else:
                        S.dve('tensor_tensor', R=[t_, a_], W=[a_], out=a_[:, off:G], in0=a_[:, off:G], in1=t_[:, 0:N],
                              op=ALU.add)
                    S.pe('matmul', R=[B[names[2]], t_], W=[o_], out=o_[:, off:G], lhsT=v_ap, rhs=t_[:, 0:N],
                         start=(idx == 0), stop=(idx == len(items) - 1))

                qk(0)
                if len(items) > 1:
                    qk(1)
                for idx in range(len(items)):
                    if idx + 2 < len(items):
                        qk(idx + 2)
                    rest(idx)
                S.pe('matmul', R=[onesf, a_], W=[pd], out=pd[:, 0:G], lhsT=onesf[:], rhs=a_[:, 0:G], start=True, stop=True)
                S.dve('reciprocal', R=[pd], W=[rec], out=rec[:], in_=pd[:, 0:G])
                S.dve('tensor_tensor', R=[o_, rec], W=[yf], out=yf[:], in0=o_[:, 0:G], in1=rec[:], op=ALU.mult)
                S.act(R=[yf], W=[yattT], out=yattT[:, h, g * G:(g + 1) * G], in_=yf[:], func=AF.Copy)
                S.act(R=[yf], W=[sq], out=sq[:], in_=yf[:], func=AF.Square)
                for t in range(GT):
                    S.pe('matmul', R=[sq, onesf], W=[pss], out=pss[:, t:t + 1], lhsT=sq[:, t * 128:(t + 1) * 128],
                         rhs=onesf[:, 0:1], start=True, stop=True)
                if h == 0:
                    S.dve('tensor_copy', R=[pss], W=[ssatt], out=ssatt[:, g * GT:(g + 1) * GT], in_=pss[:, 0:GT])
                else:
                    S.dve('tensor_tensor', R=[pss, ssatt], W=[ssatt], out=ssatt[:, g * GT:(g + 1) * GT],
                          in0=ssatt[:, g * GT:(g + 1) * GT], in1=pss[:, 0:GT], op=ALU.add)
    S.barrier()


def head4_norm(S, wk4, src_ps, gbuf, post, dst):
    sq4, st4, qn4 = wk4
    S.act(R=[src_ps], W=[sq4], out=sq4[:], in_=src_ps[:, 0:512], func=AF.Square)
    S.dve('tensor_reduce', R=[sq4], W=[st4], out=st4[:, 0:4], in_=sq4[:, :].rearrange('p (h d) -> p h d', h=4),
          axis=AX.X, op=ALU.add)
    rstd_calc(S, (st4, st4[:, 0:4]), (st4, st4[:, 4:8]), 1.0 / 128, post=post)
    S.dve('tensor_tensor', R=[src_ps, st4], W=[qn4], out=qn4[:],
          in0=src_ps[:, 0:512].rearrange('p (h d) -> p h d', h=4),
          in1=bc(st4[:, 4:8].unsqueeze(2), [128, 4, 128]), op=ALU.mult)
    S.dve('tensor_tensor', R=[qn4, gbuf], W=[dst], out=dst[:], in0=qn4[:],
          in1=bc(gbuf[:, :].unsqueeze(1), [128, 4, 128]), op=ALU.mult)


def mk_wk(S, es):
    return {'junk': S.sbuf(es, 'junk', [128, D], BF16), 'ss': S.sbuf(es, 'ss', [128, 4], F32),
            'rstd': S.sbuf(es, 'rstd', [128, 4], F32), 'xn': S.sbuf(es, 'xn', [128, D], BF16),
            'tp': [S.psum(es, 'tp', [128, 1024], BF16) for _ in range(2)]}


def mk_wk4(S, es):
    return (S.sbuf(es, 'sq4', [128, 512], F32), S.sbuf(es, 'st4', [128, 8], F32), S.sbuf(es, 'qn4', [128, 4, 128], F32))


def emit_B_mid(S, nc, T, io, c, yattT, ssatt):
    NT = T // 128
    with ExitStack() as es:
        onesf = S.sbuf(es, 'onesf', [128, 128], F32)
        S.dve('memset', W=[onesf], ap=onesf[:], constant=1.0)
        yrT = S.sbuf(es, 'yrT', [128, 8, T], BF16)
        ssr = S.sbuf(es, 'ssr', [128, NT], F32)
        ra = S.sbuf(es, 'ra', [128, NT], F32)
        rr = S.sbuf(es, 'rr', [128, NT], F32)
        g_o = S.sbuf(es, 'g_o', [128, 16], F32)
        S.dma('sp', g_o[:], io['g_out'], W=[g_o])
        pA = S.psum(es, 'pA', [128, 512], F32)
        pB = S.psum(es, 'pB', [128, 512], F32)
        pC = S.psum(es, 'pC', [128, 512], F32)
        with ExitStack() as e1:
            car = S.sbuf(e1, 'car', [128, CPB, 2, 8], F32)
            cf = S.sbuf(e1, 'cf', [128, CPB], F32)
            h0 = S.sbuf(e1, 'h0', [128, 8], F32)
            tmp = S.sbuf(e1, 'tmp8', [128, 8], F32)
            S.dma('sp', car[:], io['carry_all'].rearrange('s a p n -> p s a n'), W=[car])
            S.dma('sp', cf[:], io['cf'], W=[cf])
            S.dve('memset', W=[h0], ap=h0[:], constant=0.0)
            for s_ in range(CPB):
                S.dve('tensor_tensor', R=[car, h0], W=[tmp], out=tmp[:], in0=car[:, s_, 0, :], in1=h0[:], op=ALU.mult)
                S.dve('tensor_tensor', R=[car, tmp], W=[tmp], out=tmp[:], in0=tmp[:], in1=car[:, s_, 1, :], op=ALU.add)
                S.dve('tensor_tensor', R=[tmp, h0], W=[tmp], out=tmp[:], in0=tmp[:], in1=h0[:], op=ALU.subtract)
                S.dve('scalar_tensor_tensor', R=[tmp, cf, h0], W=[h0], out=h0[:], in0=tmp[:], scalar=cf[:, s_:s_ + 1],
                      in1=h0[:], op0=ALU.mult, op1=ALU.add)
            yl = [S.sbuf(e1, 'yl', [128, T], F32) for _ in range(2)]
            yp = [S.sbuf(e1, 'yp', [128, T], F32) for _ in range(2)]
            for n in range(8):
                a, b = yl[n % 2], yp[n % 2]
                S.dma('sp', a[:], io['Yl'][n], W=[a])
                S.dma('sp', b[:], io['Yp'][n], W=[b])
                S.dve('scalar_tensor_tensor', R=[a, b, h0], W=[a], out=a[:], in0=b[:], scalar=h0[:, n:n + 1], in1=a[:],
                      op0=ALU.mult, op1=ALU.add)
                S.act(R=[a], W=[yrT], out=yrT[:, n, :], in_=a[:], func=AF.Copy)
                S.act(R=[a], W=[b], out=b[:], in_=a[:], func=AF.Square)
                for t in range(NT):
                    S.pe('matmul', R=[b, onesf], W=[pC], out=pC[:, t:t + 1], lhsT=b[:, t * 128:(t + 1) * 128],
                         rhs=onesf[:, 0:1], start=True, stop=True)
                if n == 0:
                    S.dve('tensor_copy', R=[pC], W=[ssr], out=ssr[:], in_=pC[:, 0:NT])
                else:
                    S.dve('tensor_tensor', R=[pC, ssr], W=[ssr], out=ssr[:], in0=ssr[:], in1=pC[:, 0:NT], op=ALU.add)
            rstd_calc(S, (ssr, ssr[:]), (rr, rr[:]), 1.0 / 1024)
            rstd_calc(S, (ssatt, ssatt[:]), (ra, ra[:]), 1.0 / 1024)
        S.barrier()
        wout = S.sbuf(es, 'wout', [128, 16, D], BF16)
        xts = [S.sbuf(es, 'xt', [128, D], F32) for _ in range(2)]
        x1s = [S.sbuf(es, 'x1', [128, D], F32) for _ in range(2)]
        load_w_scaled(S, wout, lambda k: wout[:, k, :], io['w_out'], g_o, 16, 0, D, x1s)
        for i in range(NT):
            xt, x1 = xts[i % 2], x1s[i % 2]
            tok = slice(i * 128, (i + 1) * 128)
            S.dma('sp', xt[:], io['x'][tok, :], W=[xt])
            for cb in range(4):
                cs_ = slice(cb * 512, (cb + 1) * 512)
                for k in range(8):
                    S.pe('matmul', R=[yattT, wout], W=[pA], out=pA[:, 0:512], lhsT=yattT[:, k, tok], rhs=wout[:, k, cs_],
                         start=(k == 0), stop=(k == 7))
                for k in range(8):
                    S.pe('matmul', R=[yrT, wout], W=[pB], out=pB[:, 0:512], lhsT=yrT[:, k, tok], rhs=wout[:, 8 + k, cs_],
                         start=(k == 0), stop=(k == 7))
                S.dve('scalar_tensor_tensor', R=[pA, ra, xt], W=[x1], out=x1[:, cs_], in0=pA[:, 0:512],
                      scalar=ra[:, i:i + 1], in1=xt[:, cs_], op0=ALU.mult, op1=ALU.add)
                S.dve('scalar_tensor_tensor', R=[pB, rr, x1], W=[x1], out=x1[:, cs_], in0=pB[:, 0:512],
                      scalar=rr[:, i:i + 1], in1=x1[:, cs_], op0=ALU.mult, op1=ALU.add)
            S.dma('pool', io['x2'][tok, :], x1[:], R=[x1])
    S.barrier()
    with ExitStack() as es:
        onesb = S.sbuf(es, 'onesb', [128, 128], BF16)
        S.dve('memset', W=[onesb], ap=onesb[:], constant=1.0)
        wmq = S.sbuf(es, 'wmq', [128, 16, 512], BF16)
        wmo = S.sbuf(es, 'wmo', [128, 4, D], BF16)
        kmT = S.sbuf(es, 'kmT', [128, 4, NMEM], BF16)
        vm = S.sbuf(es, 'vm', [128, 2, 512], BF16)
        gmq = S.sbuf(es, 'gmq', [128, 128], F32)
        gmk = S.sbuf(es, 'gmk', [128, 128], F32)
        g_x = S.sbuf(es, 'g_x', [128, 16], F32)
        g_m = S.sbuf(es, 'g_m', [128, 16], F32)
        one4 = S.sbuf(es, 'one4', [128, 4], F32)
        wk = mk_wk(S, es)
        wk4 = mk_wk4(S, es)
        qb4 = S.sbuf(es, 'qb4', [128, 4, 128], BF16)
        tp = wk['tp']
        pA = S.psum(es, 'pA', [128, 512], F32)
        pB = S.psum(es, 'pB', [128, 512], F32)
        pC = S.psum(es, 'pC', [128, 512], F32)
        pD = S.psum(es, 'pD', [128, 512], F32)
        xts = [S.sbuf(es, 'xt', [128, D], F32) for _ in range(2)]
        S.dve('memset', W=[one4], ap=one4[:], constant=1.0)
        S.dma('sp', gmq[:], io['g_memq'].partition_broadcast(128), W=[gmq])
        S.dma('sp', gmk[:], io['g_memk'].partition_broadcast(128), W=[gmk])
        S.dma('sp', g_x[:], io['g_xattn'], W=[g_x])
        S.dma('sp', g_m[:], io['g_mem'], W=[g_m])
        load_w_scaled(S, wmq, lambda k: wmq[:, k, :], io['w_mq'], g_x, 16, 0, 512, xts)
        load_w_scaled(S, wmo, lambda k: wmo[:, k, :], io['w_mo'], one4, 4, 0, D, xts)
        with ExitStack() as e1:
            wmk = S.sbuf(e1, 'wmk', [128, 16, 512], BF16)
            wmv = S.sbuf(e1, 'wmv', [128, 16, 512], BF16)
            mT = S.sbuf(e1, 'mT', [128, 16, 128], BF16)
            load_w_scaled(S, wmk, lambda k: wmk[:, k, :], io['w_mk'], g_m, 16, 0, 512, xts)
            load_w_scaled(S, wmv, lambda k: wmv[:, k, :], io['w_mv'], g_m, 16, 0, 512, xts)
            for mt in range(2):
                xt = xts[mt]
                S.dma('sp', xt[:], io['mem'][mt * 128:(mt + 1) * 128, :], W=[xt])
                norm_T(S, xt, mT[:, :, :], mT, wk, c)
                for k in range(16):
                    S.pe('matmul', R=[mT, wmk], W=[pA], out=pA[:, 0:512], lhsT=mT[:, k, :], rhs=wmk[:, k, :],
                         start=(k == 0), stop=(k == 15))
                for k in range(16):
                    S.pe('matmul', R=[mT, wmv], W=[pB], out=pB[:, 0:512], lhsT=mT[:, k, :], rhs=wmv[:, k, :],
                         start=(k == 0), stop=(k == 15))
                S.act(R=[pB], W=[vm], out=vm[:, mt, :], in_=pB[:, 0:512], func=AF.Copy)
                head4_norm(S, wk4, pA, gmk, 1.0, qb4)
                for h in range(4):
                    S.pe('transpose', R=[qb4, c['idb']], W=[tp[0]], out=tp[0][:, h * 128:(h + 1) * 128],
                         in_=qb4[:, h, :], identity=c['idb'][:])
                S.dve('tensor_copy', R=[tp[0]], W=[kmT], out=kmT[:, :, mt * 128:(mt + 1) * 128],
                      in_=tp[0][:, 0:512].rearrange('p (h t) -> p h t', h=4))
        S.barrier()
        xnT = S.sbuf(es, 'xnT', [128, 16, 128], BF16)
        qmT = S.sbuf(es, 'qmT', [128, 4, 128], BF16)
        pm = [S.sbuf(es, 'pm', [128, 512], BF16) for _ in range(2)]
        recm = S.sbuf(es, 'recm', [128, 512], F32)
        oT = S.sbuf(es, 'oT', [128, 4, 128], BF16)
        for i in range(NT):
            x1 = xts[i % 2]
            tok = slice(i * 128, (i + 1) * 128)
            S.dma('sp', x1[:], io['x2'][tok, :], W=[x1])
            norm_T(S, x1, xnT[:, :, :], xnT, wk, c)
            for k in range(16):
                S.pe('matmul', R=[xnT, wmq], W=[pC], out=pC[:, 0:512], lhsT=xnT[:, k, :], rhs=wmq[:, k, :],
                     start=(k == 0), stop=(k == 15))
            head4_norm(S, wk4, pC, gmq, 128.0 ** -0.5, qb4)
            for h in range(4):
                S.pe('transpose', R=[qb4, c['idb']], W=[tp[0]], out=tp[0][:, h * 128:(h + 1) * 128],
                     in_=qb4[:, h, :], identity=c['idb'][:])
            S.dve('tensor_copy', R=[tp[0]], W=[qmT], out=qmT[:], in_=tp[0][:, 0:512].rearrange('p (h t) -> p h t', h=4))
            for mt in range(2):
                for h in range(4):
                    S.pe('matmul', R=[kmT, qmT], W=[pD], out=pD[:, h * 128:(h + 1) * 128],
                         lhsT=kmT[:, h, mt * 128:(mt + 1) * 128], rhs=qmT[:, h, :], start=True, stop=True)
                S.act(R=[pD], W=[pm[mt]], out=pm[mt][:], in_=pD[:, 0:512], func=AF.Exp)
            for h in range(4):
                for mt in range(2):
                    S.pe('matmul', R=[vm, pm[mt]], W=[pA], out=pA[:, h * 128:(h + 1) * 128],
                         lhsT=vm[:, mt, h * 128:(h + 1) * 128], rhs=pm[mt][:, h * 128:(h + 1) * 128],
                         start=(mt == 0), stop=(mt == 1))
            for mt in range(2):
                S.pe('matmul', R=[onesb, pm[mt]], W=[pB], out=pB[:, 0:512], lhsT=onesb[:], rhs=pm[mt][:],
                     start=(mt == 0), stop=(mt == 1))
            S.dve('reciprocal', R=[pB], W=[recm], out=recm[:], in_=pB[:, 0:512])
            S.dve('tensor_tensor', R=[pA, recm], W=[oT], out=oT[:], in0=pA[:, 0:512].rearrange('p (h t) -> p h t', h=4),
                  in1=recm[:, :].rearrange('p (h t) -> p h t', h=4), op=ALU.mult)
            for cb in range(4):
                cs_ = slice(cb * 512, (cb + 1) * 512)
                pX = pC if cb % 2 == 0 else pD
                for h in range(4):
                    S.pe('matmul', R=[oT, wmo], W=[pX], out=pX[:, 0:512], lhsT=oT[:, h, :], rhs=wmo[:, h, cs_],
                         start=(h == 0), stop=(h == 3))
                S.dve('tensor_tensor', R=[pX, x1], W=[x1], out=x1[:, cs_], in0=x1[:, cs_], in1=pX[:, 0:512], op=ALU.add)
            S.dma('pool', io['x2'][tok, :], x1[:], R=[x1])
    S.barrier()


def emit_B_moe(S, nc, T, io, c):
    NT = T // 128
    G = min(512, T)
    NG = T // G
    GT = G // 128
    with ExitStack() as es:
        gm = S.sbuf(es, 'gm', [128, D], F32)
        wr = S.sbuf(es, 'wr', [128, 16, 36], F32)
        br = S.sbuf(es, 'br', [128, 36], F32)
        S.dma('sp', gm[:], io['g_moe'].partition_broadcast(128), W=[gm])
        S.dma('sp', wr[:], io['w_r'].rearrange('(k p) n -> p k n', p=128), W=[wr])
        S.dma('sp', br[:], io['b_r'].partition_broadcast(128), W=[br])
        wb = [{'g': S.sbuf(es, 'weg', [128, 16, DEXP], BF16), 'u': S.sbuf(es, 'weu', [128, 16, DEXP], BF16),
               'd': S.sbuf(es, 'wed', [128, 4, D], BF16)} for _ in range(2)]
        hT = S.sbuf(es, 'hT', [128, 16, G], BF16)
        aT = [S.sbuf(es, 'aT', [128, 4, G], BF16) for _ in range(2)]
        yacc = [S.sbuf(es, 'yacc', [128, D], F32) for _ in range(GT)]
        cw = S.sbuf(es, 'cw', [128, GT, 32], F32)
        hf = S.sbuf(es, 'hf', [128, D], F32)
        hb = S.sbuf(es, 'hb', [128, D], BF16)
        hT32 = S.sbuf(es, 'hT32', [128, 16, 128], F32)
        junk = S.sbuf(es, 'junk', [128, D], BF16)
        st = S.sbuf(es, 'st', [128, 16], F32)
        lg = S.sbuf(es, 'lg', [128, 36], F32)
        oh = S.sbuf(es, 'oh', [128, 4], F32)
        sel = S.sbuf(es, 'sel', [128, 8], F32)
        p8 = S.sbuf(es, 'p8', [128, 8], F32)
        p8b = S.sbuf(es, 'p8b', [128, 8], F32)
        mk1 = S.sbuf(es, 'mk1', [128, 8], F32)
        mk2 = S.sbuf(es, 'mk2', [128, 8], F32)
        sgt = [S.sbuf(es, 'sgt', [128, G], F32) for _ in range(2)]
        tpb = S.psum(es, 'tpb', [128, 1024], BF16)
        tpf = S.psum(es, 'tpf', [128, 512], F32)
        pg = [S.psum(es, 'pg', [128, 512], F32) for _ in range(2)]
        pu = [S.psum(es, 'pu', [128, 512], F32) for _ in range(2)]
        py = [S.psum(es, 'py', [128, 512], F32) for _ in range(2)]
        for g in range(NG):
            for t in range(GT):
                i = g * GT + t
                tok = slice(i * 128, (i + 1) * 128)
                ya = yacc[t]
                S.dma('sp', ya[:], io['x2'][tok, :], W=[ya])
                S.act(R=[ya], W=[junk, st], out=junk[:], in_=ya[:], func=AF.Square, accum_out=st[:, 0:1])
                rstd_calc(S, (st, st[:, 0:1]), (st, st[:, 1:2]), 1.0 / D)
                S.dve('scalar_tensor_tensor', R=[ya, st, gm], W=[hf], out=hf[:], in0=ya[:], scalar=st[:, 1:2], in1=gm[:],
                      op0=ALU.mult, op1=ALU.mult)
                S.act(R=[hf], W=[hb], out=hb[:], in_=hf[:], func=AF.Copy)
                for half in range(2):
                    for k in range(8):
                        kk = half * 8 + k
                        S.pe('transpose', R=[hb, c['idb']], W=[tpb], out=tpb[:, k * 128:(k + 1) * 128],
                             in_=hb[:, kk * 128:(kk + 1) * 128], identity=c['idb'][:])
                    S.act(R=[tpb], W=[hT], out=hT[:, half * 8:(half + 1) * 8, t * 128:(t + 1) * 128],
                          in_=tpb[:, :].rearrange('p (k t) -> p k t', k=8), func=AF.Copy)
                for q4 in range(4):
                    for k in range(4):
                        kk = q4 * 4 + k
                        S.pe('transpose', R=[hf, c['idf']], W=[tpf], out=tpf[:, k * 128:(k + 1) * 128],
                             in_=hf[:, kk * 128:(kk + 1) * 128], identity=c['idf'][:])
                    S.dve('tensor_copy', R=[tpf], W=[hT32], out=hT32[:, q4 * 4:(q4 + 1) * 4, :],
                          in_=tpf[:, :].rearrange('p (k t) -> p k t', k=4))
                pl = py[0]
                for k in range(16):
                    S.pe('matmul', R=[hT32, wr], W=[pl], out=pl[:, 0:36], lhsT=hT32[:, k, :], rhs=wr[:, k, :],
                         start=(k == 0), stop=(k == 15))
                S.dve('tensor_tensor', R=[pl, br], W=[lg], out=lg[:], in0=pl[:, 0:36], in1=br[:], op=ALU.add)
                S.dve('tensor_reduce', R=[lg], W=[st], out=st[:, 2:3], in_=lg[:, 0:4], axis=AX.X, op=ALU.max)
                S.dve('tensor_scalar', R=[st], W=[st], out=st[:, 3:4], in0=st[:, 2:3], scalar1=-1.0, scalar2=None,
                      op0=ALU.mult)
                S.act(R=[lg, st], W=[p8b, st], out=p8b[:, 0:4], in_=lg[:, 0:4], func=AF.Exp, bias=st[:, 3:4],
                      accum_out=st[:, 4:5])
                S.dve('reciprocal', R=[st], W=[st], out=st[:, 5:6], in_=st[:, 4:5])
                S.dve('tensor_scalar', R=[lg, st], W=[oh], out=oh[:], in0=lg[:, 0:4], scalar1=st[:, 2:3], scalar2=None,
                      op0=ALU.is_equal)
                S.dve('tensor_scalar', R=[lg, oh], W=[sel], out=sel[:], in0=lg[:, 4:12], scalar1=oh[:, 0:1], scalar2=None,
                      op0=ALU.mult)
                for gg_ in (1, 2, 3):
                    S.dve('scalar_tensor_tensor', R=[lg, oh, sel], W=[sel], out=sel[:], in0=lg[:, 4 + 8 * gg_:12 + 8 * gg_],
                          scalar=oh[:, gg_:gg_ + 1], in1=sel[:], op0=ALU.mult, op1=ALU.add)
                S.dve('tensor_reduce', R=[sel], W=[st], out=st[:, 6:7], in_=sel[:], axis=AX.X, op=ALU.max)
                S.dve('tensor_scalar', R=[st], W=[st], out=st[:, 7:8], in0=st[:, 6:7], scalar1=-1.0, scalar2=None,
                      op0=ALU.mult)
                S.act(R=[sel, st], W=[p8], out=p8[:], in_=sel[:], func=AF.Exp, bias=st[:, 7:8])
                S.dve('tensor_reduce', R=[p8], W=[st], out=st[:, 8:9], in_=p8[:], axis=AX.X, op=ALU.max)
                S.dve('tensor_scalar', R=[p8, st], W=[mk1], out=mk1[:], in0=p8[:], scalar1=st[:, 8:9], scalar2=None,
                      op0=ALU.is_equal)
                S.dve('scalar_tensor_tensor', R=[mk1, p8], W=[p8b], out=p8b[:], in0=mk1[:], scalar=-2.0, in1=p8[:],
                      op0=ALU.mult, op1=ALU.add)
                S.dve('tensor_reduce', R=[p8b], W=[st], out=st[:, 9:10], in_=p8b[:], axis=AX.X, op=ALU.max)
                S.dve('tensor_scalar', R=[p8b, st], W=[mk2], out=mk2[:], in0=p8b[:], scalar1=st[:, 9:10], scalar2=None,
                      op0=ALU.is_equal)
                S.dve('tensor_tensor', R=[mk1, mk2], W=[mk1], out=mk1[:], in0=mk1[:], in1=mk2[:], op=ALU.add)
                S.dve('tensor_tensor', R=[st], W=[st], out=st[:, 10:11], in0=st[:, 8:9], in1=st[:, 9:10], op=ALU.add)
                S.dve('reciprocal', R=[st], W=[st], out=st[:, 11:12], in_=st[:, 10:11])
                S.dve('tensor_tensor', R=[st], W=[st], out=st[:, 12:13], in0=st[:, 11:12], in1=st[:, 5:6], op=ALU.mult)
                S.dve('scalar_tensor_tensor', R=[p8, st, mk1], W=[p8b], out=p8b[:], in0=p8[:], scalar=st[:, 12:13],
                      in1=mk1[:], op0=ALU.mult, op1=ALU.mult)
                S.dve('tensor_tensor', R=[oh, p8b], W=[cw], out=cw[:, t, :].rearrange('p (a b) -> p a b', a=4),
                      in0=bc(oh[:, :].unsqueeze(2), [128, 4, 8]), in1=bc(p8b[:, :].unsqueeze(1), [128, 4, 8]),
                      op=ALU.mult)
            for e in range(NEXP):
                W_ = wb[e % 2]
                S.dma('pool', W_['g'][:], io['w_eg'][e].rearrange('(k p) f -> p k f', p=128), W=[W_['g']])
                S.dma('pool', W_['u'][:], io['w_eu'][e].rearrange('(k p) f -> p k f', p=128), W=[W_['u']])
                S.dma('pool', W_['d'][:], io['w_ed'][e].rearrange('(k p) n -> p k n', p=128), W=[W_['d']])
                a_ = aT[e % 2]
                for fc in range(4):
                    pg_, pu_, sg_ = pg[fc % 2], pu[fc % 2], sgt[fc % 2]
                    fs = slice(fc * 128, (fc + 1) * 128)
                    for k in range(16):
                        S.pe('matmul', R=[W_['g'], hT], W=[pg_], out=pg_[:, 0:G], lhsT=W_['g'][:, k, fs], rhs=hT[:, k, :],
                             start=(k == 0), stop=(k == 15))
                    for k in range(16):
                        S.pe('matmul', R=[W_['u'], hT], W=[pu_], out=pu_[:, 0:G], lhsT=W_['u'][:, k, fs], rhs=hT[:, k, :],
                             start=(k == 0), stop=(k == 15))
                    S.act(R=[pg_], W=[sg_], out=sg_[:], in_=pg_[:, 0:G], func=AF.Silu)
                    S.dve('tensor_tensor', R=[sg_, pu_], W=[a_], out=a_[:, fc, :], in0=sg_[:], in1=pu_[:, 0:G], op=ALU.mult)
                for t in range(GT):
                    for cb in range(4):
                        py_ = py[(t * 4 + cb) % 2]
                        cs_ = slice(cb * 512, (cb + 1) * 512)
                        for fc in range(4):
                            S.pe('matmul', R=[a_, W_['d']], W=[py_], out=py_[:, 0:512], lhsT=a_[:, fc, t * 128:(t + 1) * 128],
                                 rhs=W_['d'][:, fc, cs_], start=(fc == 0), stop=(fc == 3))
                        S.dve('scalar_tensor_tensor', R=[py_, cw, yacc[t]], W=[yacc[t]], out=yacc[t][:, cs_], in0=py_[:, 0:512],
                              scalar=cw[:, t, e:e + 1], in1=yacc[t][:, cs_], op0=ALU.mult, op1=ALU.add)
            for t in range(GT):
                i = g * GT + t
                S.dma('sp', io['xo'][i * 128:(i + 1) * 128, :], yacc[t][:], R=[yacc[t]])
    S.barrier()


def emit_B(S, nc, T, io, sparse=True):
    with ExitStack() as es:
        c = load_consts(S, es, io)
        with ExitStack() as e1:
            yattT = S.sbuf(e1, 'yattT', [128, NH, T], BF16)
            ssatt = S.sbuf(e1, 'ssatt', [128, T // 128], F32)
            emit_B_attn(S, nc, T, io, c, yattT, ssatt)
            emit_B_mid(S, nc, T, io, c, yattT, ssatt)
        S.barrier()
        if sparse:
            emit_B_moe_sparse(S, nc, T, io, c)
        else:
            emit_B_moe(S, nc, T, io, c)


def build_B(T, sparse=True):
    nc = bass.Bass("TRN2", target_bir_lowering=False)
    io = {}
    NT = T // 128
    for name, shape, dt in (
            ('x', [T, D], F32), ('ident', [128, 128], F32), ('tri', [128, 128], F32), ('kb', [128, CPB - 1], F32),
            ('QT', [NH, 192, T], BF16), ('KT', [NH, 192, T], BF16), ('V', [NH, 128, NT, 128], BF16),
            ('KT_oth', [CPB - 1, NH, 192, T], BF16), ('V_oth', [CPB - 1, NH, 128, NT, 128], BF16),
            ('Yl', [8, 128, T], F32), ('Yp', [8, 128, T], F32), ('carry_all', [CPB, 2, 128, 8], F32),
            ('cf', [128, CPB], F32), ('g_out', [128, 16], F32), ('w_out', [D, D], F32),
            ('g_xattn', [128, 16], F32), ('g_mem', [128, 16], F32), ('g_memq', [128], F32), ('g_memk', [128], F32),
            ('w_mq', [D, 512], F32), ('w_mk', [D, 512], F32), ('w_mv', [D, 512], F32), ('w_mo', [512, D], F32),
            ('mem', [NMEM, D], F32), ('g_moe', [D], F32), ('w_r', [D, 36], F32), ('b_r', [36], F32),
            ('w_eg', [NEXP, D, DEXP], F32), ('w_eu', [NEXP, D, DEXP], F32), ('w_ed', [NEXP, DEXP, D], F32)):
        io[name] = dram_in(nc, name, shape, dt)
    io['x2'] = dram_out(nc, 'x2', [T, D])
    io['xo'] = dram_out(nc, 'xo', [T, D])
    if sparse:
        for name, shape in (('ecap', [32]), ('ustrict', [128, 128]), ('tok2', [128, NT, 2])):
            io[name] = dram_in(nc, name, shape)
        io['cnt'] = dram_out(nc, 'cnt', [1, 32])
        io['Xs'] = nc.dram_tensor('Xs', [NEXP * CAP, D], BF16, kind="Internal").ap()
        io['meta'] = nc.dram_tensor('meta', [NEXP * CAP, 16], F32, kind="Internal").ap()
        io['Yt'] = [nc.dram_tensor('Yt%d' % hh, [2 * T, 1024], F32, kind="Internal").ap() for hh in range(2)]
    with ExitStack() as es:
        S = Sched(nc, es)
        emit_B(S, nc, T, io, sparse)
        S.barrier()
    return nc


def inputs_B(inp, l, xs, resA, T):
    g = lambda n: np.ascontiguousarray(inp[n][l])
    tri = np.triu(np.ones((128, 128), np.float32))
    shared = {
        'ident': np.eye(128, dtype=np.float32), 'tri': tri,
        'g_out': col_layout(np.concatenate([inp['att_out_norm_g'][l], inp['rnn_out_norm_g'][l]]), 16),
        'w_out': g('w_out'), 'g_xattn': col_layout(inp['xattn_norm_g'][l], 16), 'g_mem': col_layout(inp['mem_norm_g'][l], 16),
        'g_memq': g('mem_q_norm_g'), 'g_memk': g('mem_k_norm_g'),
        'w_mq': g('w_mq'), 'w_mk': g('w_mk'), 'w_mv': g('w_mv'), 'w_mo': g('w_mo'),
        'g_moe': g('moe_norm_g'),
        'w_r': np.ascontiguousarray(np.concatenate([inp['w_router_group'][l], inp['w_router_expert'][l]], axis=1)),
        'b_r': np.ascontiguousarray(np.concatenate([inp['b_router_group'][l], inp['b_router_expert'][l]])),
        'w_eg': g('w_exp_gate'), 'w_eu': g('w_exp_up'), 'w_ed': g('w_exp_down'),
    }
    NT = T // 128
    t_idx = (np.arange(NT)[None, :] * 128 + np.arange(128)[:, None]).astype(np.float32)
    shared['ecap'] = (np.arange(32) * CAP).astype(np.float32)
    shared['ustrict'] = np.triu(np.ones((128, 128), np.float32), 1)
    shared['tok2'] = np.ascontiguousarray(np.stack([2 * t_idx, 2 * t_idx + 1], axis=2))
    maps = []
    for cidx in range(NCORES):
        b, j = cidx // CPB, cidx % CPB
        grp = range(b * CPB, (b + 1) * CPB)
        m = dict(shared)
        m['x'] = xs[cidx]
        m['mem'] = np.ascontiguousarray(inp['mem'][b])
        for n in ('QT', 'KT', 'V', 'Yl', 'Yp'):
            m[n] = resA[cidx][n]
        oth = [k for k in grp if k != cidx]
        m['KT_oth'] = np.stack([resA[k]['KT'] for k in oth])
        m['V_oth'] = np.stack([resA[k]['V'] for k in oth])
        m['carry_all'] = np.stack([resA[k]['carry'] for k in grp])
        kb = np.zeros((128, CPB - 1), np.float32)
        for si, k in enumerate(oth):
            if k > cidx:
                kb[:, si] = -30000.0
        m['kb'] = kb
        cf = np.zeros((128, CPB), np.float32)
        cf[:, :j] = 1.0
        m['cf'] = cf
        maps.append(m)
    return maps


_NC_CACHE = {}


def _get_nc(kind, T):
    key = (kind, T)
    if key not in _NC_CACHE:
        _NC_CACHE[key] = build_A(T) if kind == 'A' else build_B(T)
    return _NC_CACHE[key]


def run_layers(inp, T):
    B = inp['x'].shape[0]
    x = np.asarray(inp['x'], np.float32)
    xs = [np.ascontiguousarray(x[c // CPB, (c % CPB) * T:(c % CPB + 1) * T]) for c in range(NCORES)]
    inp = {k: np.asarray(v) for k, v in inp.items()}
    for l in range(2):
        ra = run_bass_kernel_spmd(build_A(T), inputs_A(inp, l, xs, T), core_ids=list(range(NCORES))).results
        mb = inputs_B(inp, l, xs, ra, T)
        rb = run_bass_kernel_spmd(build_B(T, True), mb, core_ids=list(range(NCORES))).results
        if max(float(np.max(r['cnt'])) for r in rb) > CAP:
            for m in mb:
                for n in ('ecap', 'ustrict', 'tok2'):
                    m.pop(n)
            rb = run_bass_kernel_spmd(build_B(T, False), mb, core_ids=list(range(NCORES))).results
        xs = [np.ascontiguousarray(rb[c]['xo']) for c in range(NCORES)]
    out = np.empty((B, CPB * T, D), np.float32)
    for c in range(NCORES):
        out[c // CPB, (c % CPB) * T:(c % CPB + 1) * T] = xs[c]
    return out


def kernel(**inputs):
    return run_layers(inputs, 2048)


def emit_B_moe_sparse(S, nc, T, io, c):
    NT = T // 128
    NB = CAP // 128
    NSLOT = NEXP * CAP
    with ExitStack() as es:
        onesb = S.sbuf(es, 'onesb', [128, 128], BF16)
        S.dve('memset', W=[onesb], ap=onesb[:], constant=1.0)
        breg_slot = nc.gpsimd.to_reg(NSLOT - 1)
        breg_tok = nc.gpsimd.to_reg(2 * T - 1)
        with ExitStack() as e1:
            gm = S.sbuf(e1, 'gm', [128, D], F32)
            wr = S.sbuf(e1, 'wr', [128, 16, 36], F32)
            br = S.sbuf(e1, 'br', [128, 36], F32)
            ecap = S.sbuf(e1, 'ecap', [128, 32], F32)
            ustr = S.sbuf(e1, 'ustr', [128, 128], F32)
            ustb = S.sbuf(e1, 'ustb', [128, 128], BF16)
            tok2 = S.sbuf(e1, 'tok2', [128, NT, 2], F32)
            S.dma('sp', gm[:], io['g_moe'].partition_broadcast(128), W=[gm])
            S.dma('sp', wr[:], io['w_r'].rearrange('(k p) n -> p k n', p=128), W=[wr])
            S.dma('sp', br[:], io['b_r'].partition_broadcast(128), W=[br])
            S.dma('sp', ecap[:], io['ecap'].partition_broadcast(128), W=[ecap])
            S.dma('sp', ustr[:], io['ustrict'], W=[ustr])
            S.dma('sp', tok2[:], io['tok2'], W=[tok2])
            S.dve('tensor_copy', R=[ustr], W=[ustb], out=ustb[:], in_=ustr[:])
            mi = S.sbuf(e1, 'mi', [128, NSLOT // 128, 16], F32)
            S.dve('memset', W=[mi], ap=mi[:], constant=0.0)
            S.dve('memset', W=[mi], ap=mi[:, :, 0:1], constant=1.0e6)
            S.dma('sp', io['meta'].rearrange('(p a) m -> p a m', p=128), mi[:], R=[mi])
            Mall = S.sbuf(e1, 'Mall', [128, NT, 32], BF16)
            ya = S.sbuf(e1, 'ya', [128, D], F32)
            hf = S.sbuf(e1, 'hf', [128, D], F32)
            hbs = [S.sbuf(e1, 'hb', [128, D], BF16) for _ in range(2)]
            hT32 = S.sbuf(e1, 'hT32', [128, 16, 128], F32)
            junk = S.sbuf(e1, 'junk', [128, D], BF16)
            st = S.sbuf(e1, 'st', [128, 16], F32)
            lg = S.sbuf(e1, 'lg', [128, 36], F32)
            oh = S.sbuf(e1, 'oh', [128, 4], F32)
            sel = S.sbuf(e1, 'sel', [128, 8], F32)
            p8 = S.sbuf(e1, 'p8', [128, 8], F32)
            p8b = S.sbuf(e1, 'p8b', [128, 8], F32)
            mk1 = S.sbuf(e1, 'mk1', [128, 8], F32)
            mk2 = S.sbuf(e1, 'mk2', [128, 8], F32)
            M1 = S.sbuf(e1, 'M1', [128, 32], F32)
            M2 = S.sbuf(e1, 'M2', [128, 32], F32)
            Mt = S.sbuf(e1, 'Mt', [128, 32], F32)
            psl = S.sbuf(e1, 'psl', [128, 32], F32)
            pen = S.sbuf(e1, 'pen', [128, 32], F32)
            sk = S.sbuf(e1, 'sk', [128, 2], F32)
            metas = [[S.sbuf(e1, 'mrow', [128, 16], F32) for _ in range(2)] for _ in range(2)]
            skis = [[S.sbuf(e1, 'ski', [128, 1], I32) for _ in range(2)] for _ in range(2)]
            tpf = S.psum(e1, 'tpf', [128, 512], F32)
            plg = S.psum(e1, 'plg', [128, 512], F32)
            ppos = S.psum(e1, 'ppos', [128, 512], F32)
            for i in range(NT):
                tok = slice(i * 128, (i + 1) * 128)
                hb = hbs[i % 2]
                S.dma('sp', ya[:], io['x2'][tok, :], W=[ya])
                S.act(R=[ya], W=[junk, st], out=junk[:], in_=ya[:], func=AF.Square, accum_out=st[:, 0:1])
                rstd_calc(S, (st, st[:, 0:1]), (st, st[:, 1:2]), 1.0 / D)
                S.dve('scalar_tensor_tensor', R=[ya, st, gm], W=[hf], out=hf[:], in0=ya[:], scalar=st[:, 1:2], in1=gm[:],
                      op0=ALU.mult, op1=ALU.mult)
                S.act(R=[hf], W=[hb], out=hb[:], in_=hf[:], func=AF.Copy)
                for q4 in range(4):
                    for k in range(4):
                        kk = q4 * 4 + k
                        S.pe('transpose', R=[hf, c['idf']], W=[tpf], out=tpf[:, k * 128:(k + 1) * 128],
                             in_=hf[:, kk * 128:(kk + 1) * 128], identity=c['idf'][:])
                    S.dve('tensor_copy', R=[tpf], W=[hT32], out=hT32[:, q4 * 4:(q4 + 1) * 4, :],
                          in_=tpf[:, :].rearrange('p (k t) -> p k t', k=4))
                for k in range(16):
                    S.pe('matmul', R=[hT32, wr], W=[plg], out=plg[:, 0:36], lhsT=hT32[:, k, :], rhs=wr[:, k, :],
                         start=(k == 0), stop=(k == 15))
                S.dve('tensor_tensor', R=[plg, br], W=[lg], out=lg[:], in0=plg[:, 0:36], in1=br[:], op=ALU.add)
                S.dve('tensor_reduce', R=[lg], W=[st], out=st[:, 2:3], in_=lg[:, 0:4], axis=AX.X, op=ALU.max)
                S.dve('tensor_scalar', R=[st], W=[st], out=st[:, 3:4], in0=st[:, 2:3], scalar1=-1.0, scalar2=None,
                      op0=ALU.mult)
                S.act(R=[lg, st], W=[p8b, st], out=p8b[:, 0:4], in_=lg[:, 0:4], func=AF.Exp, bias=st[:, 3:4],
                      accum_out=st[:, 4:5])
                S.dve('reciprocal', R=[st], W=[st], out=st[:, 5:6], in_=st[:, 4:5])
                S.dve('tensor_scalar', R=[lg, st], W=[oh], out=oh[:], in0=lg[:, 0:4], scalar1=st[:, 2:3], scalar2=None,
                      op0=ALU.is_equal)
                S.dve('tensor_scalar', R=[lg, oh], W=[sel], out=sel[:], in0=lg[:, 4:12], scalar1=oh[:, 0:1], scalar2=None,
                      op0=ALU.mult)
                for gg_ in (1, 2, 3):
                    S.dve('scalar_tensor_tensor', R=[lg, oh, sel], W=[sel], out=sel[:], in0=lg[:, 4 + 8 * gg_:12 + 8 * gg_],
                          scalar=oh[:, gg_:gg_ + 1], in1=sel[:], op0=ALU.mult, op1=ALU.add)
                S.dve('tensor_reduce', R=[sel], W=[st], out=st[:, 6:7], in_=sel[:], axis=AX.X, op=ALU.max)
                S.dve('tensor_scalar', R=[st], W=[st], out=st[:, 7:8], in0=st[:, 6:7], scalar1=-1.0, scalar2=None,
                      op0=ALU.mult)
                S.act(R=[sel, st], W=[p8], out=p8[:], in_=sel[:], func=AF.Exp, bias=st[:, 7:8])
                S.dve('tensor_reduce', R=[p8], W=[st], out=st[:, 8:9], in_=p8[:], axis=AX.X, op=ALU.max)
                S.dve('tensor_scalar', R=[p8, st], W=[mk1], out=mk1[:], in0=p8[:], scalar1=st[:, 8:9], scalar2=None,
                      op0=ALU.is_equal)
                S.dve('scalar_tensor_tensor', R=[mk1, p8], W=[p8b], out=p8b[:], in0=mk1[:], scalar=-2.0, in1=p8[:],
                      op0=ALU.mult, op1=ALU.add)
                S.dve('tensor_reduce', R=[p8b], W=[st], out=st[:, 9:10], in_=p8b[:], axis=AX.X, op=ALU.max)
                S.dve('tensor_scalar', R=[p8b, st], W=[mk2], out=mk2[:], in0=p8b[:], scalar1=st[:, 9:10], scalar2=None,
                      op0=ALU.is_equal)
                S.dve('tensor_tensor', R=[st], W=[st], out=st[:, 10:11], in0=st[:, 8:9], in1=st[:, 9:10], op=ALU.add)
                S.dve('reciprocal', R=[st], W=[st], out=st[:, 11:12], in_=st[:, 10:11])
                S.dve('tensor_tensor', R=[st], W=[st], out=st[:, 12:13], in0=st[:, 11:12], in1=st[:, 5:6], op=ALU.mult)
                m1r, m2r = metas[i % 2]
                S.dve('memset', W=[m1r], ap=m1r[:], constant=0.0)
                S.dve('memset', W=[m2r], ap=m2r[:], constant=0.0)
                S.dve('tensor_copy', R=[tok2], W=[m1r], out=m1r[:, 0:1], in_=tok2[:, i, 0:1])
                S.dve('tensor_copy', R=[tok2], W=[m2r], out=m2r[:, 0:1], in_=tok2[:, i, 1:2])
                S.dve('tensor_tensor', R=[st], W=[m1r], out=m1r[:, 1:2], in0=st[:, 8:9], in1=st[:, 12:13], op=ALU.mult)
                S.dve('tensor_tensor', R=[st], W=[m2r], out=m2r[:, 1:2], in0=st[:, 9:10], in1=st[:, 12:13], op=ALU.mult)
                S.dve('tensor_tensor', R=[oh, mk1], W=[M1], out=M1[:, :].rearrange('p (a b) -> p a b', a=4),
                      in0=bc(oh[:, :].unsqueeze(2), [128, 4, 8]), in1=bc(mk1[:, :].unsqueeze(1), [128, 4, 8]), op=ALU.mult)
                S.dve('tensor_tensor', R=[oh, mk2], W=[M2], out=M2[:, :].rearrange('p (a b) -> p a b', a=4),
                      in0=bc(oh[:, :].unsqueeze(2), [128, 4, 8]), in1=bc(mk2[:, :].unsqueeze(1), [128, 4, 8]), op=ALU.mult)
                S.dve('tensor_tensor', R=[M1, M2], W=[Mt], out=Mt[:], in0=M1[:], in1=M2[:], op=ALU.add)
                S.dve('tensor_copy', R=[Mt], W=[Mall], out=Mall[:, i, :], in_=Mt[:])
                S.pe('matmul', R=[ustb, Mall], W=[ppos], out=ppos[:, 0:32], lhsT=ustb[:], rhs=Mall[:, i, :],
                     start=True, stop=(i == 0))
                for i2 in range(i):
                    S.pe('matmul', R=[onesb, Mall], W=[ppos], out=ppos[:, 0:32], lhsT=onesb[:], rhs=Mall[:, i2, :],
                         start=False, stop=(i2 == i - 1))
                S.dve('tensor_scalar', R=[ppos], W=[pen], out=pen[:], in0=ppos[:, 0:32], scalar1=float(CAP), scalar2=1.0e6,
                      op0=ALU.is_ge, op1=ALU.mult)
                S.dve('tensor_tensor', R=[ppos, ecap], W=[psl], out=psl[:], in0=ppos[:, 0:32], in1=ecap[:], op=ALU.add)
                S.dve('tensor_tensor', R=[psl, pen], W=[psl], out=psl[:], in0=psl[:], in1=pen[:], op=ALU.add)
                S.dve('tensor_tensor', R=[psl, M1], W=[pen], out=pen[:], in0=psl[:], in1=M1[:], op=ALU.mult)
                S.dve('tensor_reduce', R=[pen], W=[sk], out=sk[:, 0:1], in_=pen[:], axis=AX.X, op=ALU.add)
                S.dve('tensor_tensor', R=[psl, M2], W=[pen], out=pen[:], in0=psl[:], in1=M2[:], op=ALU.mult)
                S.dve('tensor_reduce', R=[pen], W=[sk], out=sk[:, 1:2], in_=pen[:], axis=AX.X, op=ALU.add)
                for kk in range(2):
                    ski = skis[i % 2][kk]
                    mr = metas[i % 2][kk]
                    S.dve('tensor_copy', R=[sk], W=[ski], out=ski[:], in_=sk[:, kk:kk + 1])
                    S.indirect(R=[hb, ski], out=io['Xs'], out_offset=bass.IndirectOffsetOnAxis(ap=ski[:, :], axis=0),
                               in_=hb[:, :], in_offset=None, bounds_check=breg_slot, oob_is_err=False)
                    S.indirect(R=[mr, ski], out=io['meta'], out_offset=bass.IndirectOffsetOnAxis(ap=ski[:, :], axis=0),
                               in_=mr[:, :], in_offset=None, bounds_check=breg_slot, oob_is_err=False)
            for i2 in range(NT):
                S.pe('matmul', R=[onesb, Mall], W=[ppos], out=ppos[:, 0:32], lhsT=onesb[:], rhs=Mall[:, i2, :],
                     start=(i2 == 0), stop=(i2 == NT - 1))
            S.dve('tensor_copy', R=[ppos], W=[psl], out=psl[:], in_=ppos[:, 0:32])
            S.dma('sp', io['cnt'], psl[0:1, :], R=[psl])
        S.barrier()
        with ExitStack() as e2:
            wb = [{'g': S.sbuf(e2, 'weg', [128, 16, DEXP], BF16), 'u': S.sbuf(e2, 'weu', [128, 16, DEXP], BF16),
                   'd': S.sbuf(e2, 'wed', [128, 4, D], BF16)} for _ in range(2)]
            xbs = [S.sbuf(e2, 'xb', [128, D], BF16) for _ in range(2)]
            mts = [S.sbuf(e2, 'mt', [128, 16], F32) for _ in range(2)]
            idxs = [S.sbuf(e2, 'idx', [128, 1], I32) for _ in range(2)]
            xTs = [S.sbuf(e2, 'xT', [128, 16, 128], BF16) for _ in range(2)]
            sgs = [S.sbuf(e2, 'sg', [128, DEXP], F32) for _ in range(2)]
            abs_ = [S.sbuf(e2, 'ab', [128, DEXP], BF16) for _ in range(2)]
            aTs = [S.sbuf(e2, 'aT', [128, 4, 128], BF16) for _ in range(2)]
            ybs = [S.sbuf(e2, 'yb', [128, D], F32) for _ in range(2)]
            tpb = [S.psum(e2, 'tpb', [128, 1024], BF16) for _ in range(2)]
            pg = S.psum(e2, 'pg', [128, 512], F32)
            pu = S.psum(e2, 'pu', [128, 512], F32)
            py = [S.psum(e2, 'py', [128, 512], F32) for _ in range(2)]

            def load_w(e):
                W_ = wb[e % 2]
                S.dma('pool', W_['g'][:], io['w_eg'][e].rearrange('(k p) f -> p k f', p=128), W=[W_['g']])
                S.dma('pool', W_['u'][:], io['w_eu'][e].rearrange('(k p) f -> p k f', p=128), W=[W_['u']])
                S.dma('pool', W_['d'][:], io['w_ed'][e].rearrange('(k p) n -> p k n', p=128), W=[W_['d']])
            tpa = S.psum(e2, 'tpa', [128, 512], BF16)
            blocks = [(e, r) for e in range(NEXP) for r in range(NB)]

            def bufs(bi):
                b = bi % 2
                return xbs[b], mts[b], idxs[b], xTs[b], sgs[b], abs_[b], aTs[b], ybs[b]

            def stage_T(bi):
                e, r = blocks[bi]
                xb, mt, idx, xT, sg, ab, aT, yb = bufs(bi)
                r0 = e * CAP + r * 128
                S.dma('sp', xb[:], io['Xs'][r0:r0 + 128, :], W=[xb])
                S.dma('sp', mt[:], io['meta'][r0:r0 + 128, :], W=[mt])
                for half in range(2):
                    for k in range(8):
                        kk = half * 8 + k
                        S.pe('transpose', R=[xb, c['idb']], W=[tpb[half]], out=tpb[half][:, k * 128:(k + 1) * 128],
                             in_=xb[:, kk * 128:(kk + 1) * 128], identity=c['idb'][:])
                    src = tpb[half][:, :].rearrange('p (k t) -> p k t', k=8)
                    if half == 0:
                        S.act(R=[tpb[half]], W=[xT], out=xT[:, 0:8, :], in_=src, func=AF.Copy)
                    else:
                        S.dve('tensor_copy', R=[tpb[half]], W=[xT], out=xT[:, 8:16, :], in_=src)

            def stage_GU(bi):
                e, r = blocks[bi]
                W_ = wb[e % 2]
                xb, mt, idx, xT, sg, ab, aT, yb = bufs(bi)
                for k in range(16):
                    S.pe('matmul', R=[xT, W_['g']], W=[pg], out=pg[:, 0:DEXP], lhsT=xT[:, k, :], rhs=W_['g'][:, k, :],
                         start=(k == 0), stop=(k == 15))
                for k in range(16):
                    S.pe('matmul', R=[xT, W_['u']], W=[pu], out=pu[:, 0:DEXP], lhsT=xT[:, k, :], rhs=W_['u'][:, k, :],
                         start=(k == 0), stop=(k == 15))
                S.act(R=[pg], W=[sg], out=sg[:], in_=pg[:, 0:DEXP], func=AF.Silu)
                S.dve('tensor_tensor', R=[sg, pu], W=[ab], out=ab[:], in0=sg[:], in1=pu[:, 0:DEXP], op=ALU.mult)

            def stage_DN(bi):
                e, r = blocks[bi]
                W_ = wb[e % 2]
                xb, mt, idx, xT, sg, ab, aT, yb = bufs(bi)
                for fc in range(4):
                    S.pe('transpose', R=[ab, c['idb']], W=[tpa], out=tpa[:, fc * 128:(fc + 1) * 128],
                         in_=ab[:, fc * 128:(fc + 1) * 128], identity=c['idb'][:])
                S.dve('tensor_copy', R=[tpa], W=[aT], out=aT[:], in_=tpa[:, 0:512].rearrange('p (k t) -> p k t', k=4))
                for cb in range(4):
                    py_ = py[cb % 2]
                    cs_ = slice(cb * 512, (cb + 1) * 512)
                    for fc in range(4):
                        S.pe('matmul', R=[aT, W_['d']], W=[py_], out=py_[:, 0:512], lhsT=aT[:, fc, :], rhs=W_['d'][:, fc, cs_],
                             start=(fc == 0), stop=(fc == 3))
                    if cb % 2 == 0:
                        S.act(R=[py_, mt], W=[yb], out=yb[:, cs_], in_=py_[:, 0:512], func=AF.Copy, scale=mt[:, 1:2])
                    else:
                        S.dve('tensor_scalar', R=[py_, mt], W=[yb], out=yb[:, cs_], in0=py_[:, 0:512], scalar1=mt[:, 1:2],
                              scalar2=None, op0=ALU.mult)
                S.dve('tensor_copy', R=[mt], W=[idx], out=idx[:], in_=mt[:, 0:1])
                for hh in range(2):
                    S.indirect(R=[yb, idx], out=io['Yt'][hh], out_offset=bass.IndirectOffsetOnAxis(ap=idx[:, :], axis=0),
                               in_=yb[:, hh * 1024:(hh + 1) * 1024], in_offset=None, bounds_check=breg_tok,
                               oob_is_err=False)

            load_w(0)
            stage_T(0)
            for bi, (e, r) in enumerate(blocks):
                if r == 0 and e + 1 < NEXP:
                    load_w(e + 1)
                stage_GU(bi)
                if bi + 1 < len(blocks):
                    stage_T(bi + 1)
                stage_DN(bi)
        S.barrier()
        with ExitStack() as e3:
            yts = [S.sbuf(e3, 'yt', [128, 2, D], F32) for _ in range(2)]
            xs_ = [S.sbuf(e3, 'xc', [128, D], F32) for _ in range(2)]
            for i in range(NT):
                yt, xc = yts[i % 2], xs_[i % 2]
                for hh in range(2):
                    S.dma('sp', yt[:, :, hh * 1024:(hh + 1) * 1024],
                          io['Yt'][hh][i * 256:(i + 1) * 256, :].rearrange('(p k) d -> p k d', k=2), W=[yt])
                S.dma('sp', xc[:], io['x2'][i * 128:(i + 1) * 128, :], W=[xc])
                S.dve('tensor_tensor', R=[xc, yt], W=[xc], out=xc[:], in0=xc[:], in1=yt[:, 0, :], op=ALU.add)
                S.pool('tensor_tensor', R=[xc, yt], W=[xc], out=xc[:], in0=xc[:], in1=yt[:, 1, :], op=ALU.add)
                S.dma('sp', io['xo'][i * 128:(i + 1) * 128, :], xc[:], R=[xc])
    S.barrier()
```

```python
import numpy as np
from contextlib import ExitStack
import concourse.bass as bass
import concourse.mybir as mybir
from concourse.bass_utils import run_bass_kernel_spmd

F32 = mybir.dt.float32
BF16 = mybir.dt.bfloat16
AF = mybir.ActivationFunctionType
ALU = mybir.AluOpType
AX = mybir.AxisListType
I32 = mybir.dt.int32
CAP = 640

EPS = 1e-6
D = 2048
NCORES = 8
CPB = 4
DIN = 2880
NH = 8
NEXP = 32
DEXP = 512
NMEM = 256


class Buf:
    def __init__(self, t):
        self.t = t
        self.w = None
        self.r = []

    def __getitem__(self, k):
        return self.t[k]


class Sched:
    SAME_ENGINE_SYNC = True
    NDMA = 16

    def __init__(self, nc, es):
        self.nc = nc
        self.es = es
        self.eng = {'pe': nc.tensor, 'act': nc.scalar, 'dve': nc.vector,
                    'pool': nc.gpsimd, 'sp': nc.sync}
        self.sem = {}
        self.cnt = {}
        for e in ('pe', 'act', 'dve', 'pool'):
            self.sem[e] = es.enter_context(nc.semaphore('s_' + e))
            self.cnt[e] = 0
        self.dq = {}
        for q in ('sp', 'pool', 'act'):
            lst = []
            for i in range(self.NDMA):
                k = ('d', q, i)
                self.sem[k] = es.enter_context(nc.semaphore('d_%s_%d' % (q, i)))
                self.cnt[k] = 0
                lst.append(k)
            self.dq[q] = [lst, 0]
        self.seen = {e: {} for e in self.eng}
        self.n_ins = 0
        self.uid = 0

    def sbuf(self, es, name, shape, dt):
        self.uid += 1
        return Buf(es.enter_context(self.nc.sbuf_tensor('%s_%d' % (name, self.uid), list(shape), dt)))

    def psum(self, es, name, shape, dt=F32):
        self.uid += 1
        return Buf(es.enter_context(self.nc.psum_tensor('%s_%d' % (name, self.uid), list(shape), dt)))

    def _deps(self, e, reads, writes):
        deps = {}

        def add(tok):
            if tok is None:
                return
            k, v = tok
            if deps.get(k, 0) < v:
                deps[k] = v
        for b in reads:
            add(b.w)
        for b in writes:
            add(b.w)
            for t in b.r:
                add(t)
        eng = self.eng[e]
        for k, v in deps.items():
            if k == e and (e == 'pe' or not self.SAME_ENGINE_SYNC):
                continue
            if self.seen[e].get(k, 0) >= v:
                continue
            eng.wait_ge(self.sem[k], v)
            self.seen[e][k] = v

    def _post(self, tok, reads, writes):
        for b in reads:
            b.r.append(tok)
            if len(b.r) > 48:
                d = {}
                for k, v in b.r:
                    if d.get(k, 0) < v:
                        d[k] = v
                b.r = list(d.items())
        for b in writes:
            b.w = tok
            b.r = []

    def op(self, e, name, R=(), W=(), **kw):
        self._deps(e, R, W)
        ins = getattr(self.eng[e], name)(**kw)
        self.cnt[e] += 1
        ins.then_inc(self.sem[e], 1)
        tok = (e, self.cnt[e])
        self._post(tok, R, W)
        self.n_ins += 1
        return tok

    def dve(self, name, R=(), W=(), **kw):
        return self.op('dve', name, R, W, **kw)

    def act(self, R=(), W=(), **kw):
        return self.op('act', 'activation', R, W, **kw)

    def pe(self, name, R=(), W=(), **kw):
        return self.op('pe', name, R, W, **kw)

    def pool(self, name, R=(), W=(), **kw):
        return self.op('pool', name, R, W, **kw)

    def _drain_sem(self, q, k):
        v = self.cnt[k]
        if v and self.seen[q].get(k, 0) < v:
            self.eng[q].wait_ge(self.sem[k], v)
            self.seen[q][k] = v

    def dma(self, q, out, in_, R=(), W=(), **kw):
        self._deps(q, R, W)
        lst, i = self.dq[q]
        k = lst[i % len(lst)]
        self.dq[q][1] = i + 1
        self._drain_sem(q, k)
        ins = self.eng[q].dma_start(out=out, in_=in_, **kw)
        self.cnt[k] += 16
        ins.then_inc(self.sem[k], 16)
        tok = (k, self.cnt[k])
        self._post(tok, R, W)
        self.n_ins += 1
        return tok

    def indirect(self, R=(), W=(), **kw):
        q = 'pool'
        self._deps(q, R, W)
        lst, i = self.dq[q]
        k = lst[i % len(lst)]
        self.dq[q][1] = i + 1
        self._drain_sem(q, k)
        ins = self.eng[q].indirect_dma_start(**kw)
        self.cnt[k] += 16
        ins.then_inc(self.sem[k], 16)
        tok = (k, self.cnt[k])
        self._post(tok, R, W)
        self.n_ins += 1
        return tok

    def barrier(self):
        for e, eng in self.eng.items():
            for k, v in self.cnt.items():
                if v == 0 or k == e and e == 'pe':
                    continue
                if self.seen[e].get(k, 0) >= v:
                    continue
                eng.wait_ge(self.sem[k], v)
                self.seen[e][k] = v


def bc(ap, shape):
    return ap.to_broadcast(list(shape))


def rstd_calc(S, ss, out, inv_n, post=1.0):
    sb, sap = ss
    ob, oap = out
    S.dve('tensor_scalar', R=[sb], W=[ob], out=oap, in0=sap, scalar1=inv_n, scalar2=EPS,
          op0=ALU.mult, op1=ALU.add)
    S.act(R=[ob], W=[ob], out=oap, in_=oap, func=AF.Sqrt)
    S.dve('reciprocal', R=[ob], W=[ob], out=oap, in_=oap)
    if post != 1.0:
        S.dve('tensor_scalar', R=[ob], W=[ob], out=oap, in0=oap, scalar1=post, scalar2=None, op0=ALU.mult)


def load_consts(S, es, io):
    c = {}
    c['idf'] = S.sbuf(es, 'idf', [128, 128], F32)
    c['idb'] = S.sbuf(es, 'idb', [128, 128], BF16)
    S.dma('sp', c['idf'][:], io['ident'], W=[c['idf']])
    S.dve('tensor_copy', R=[c['idf']], W=[c['idb']], out=c['idb'][:], in_=c['idf'][:])
    return c


def norm_T(S, xt, xnT_dst, dstbuf, wk, c):
    junk, ss, rstd, xn, tp = wk['junk'], wk['ss'], wk['rstd'], wk['xn'], wk['tp']
    S.act(R=[xt], W=[junk, ss], out=junk[:, 0:D], in_=xt[:], func=AF.Square, accum_out=ss[:, 0:1])
    rstd_calc(S, (ss, ss[:, 0:1]), (rstd, rstd[:, 0:1]), 1.0 / D)
    S.act(R=[xt, rstd], W=[xn], out=xn[:, 0:1024], in_=xt[:, 0:1024], func=AF.Copy, scale=rstd[:, 0:1])
    S.dve('tensor_scalar', R=[xt, rstd], W=[xn], out=xn[:, 1024:2048], in0=xt[:, 1024:2048],
          scalar1=rstd[:, 0:1], scalar2=None, op0=ALU.mult)
    for half in range(2):
        t = tp[half]
        for k in range(8):
            kk = half * 8 + k
            S.pe('transpose', R=[xn, c['idb']], W=[t], out=t[:, k * 128:(k + 1) * 128],
                 in_=xn[:, kk * 128:(kk + 1) * 128], identity=c['idb'][:])
        src = t[:, :].rearrange('p (k t) -> p k t', k=8)
        if half == 0:
            S.act(R=[t], W=[dstbuf], out=xnT_dst[:, 0:8, :], in_=src, func=AF.Copy)
        else:
            S.dve('tensor_copy', R=[t], W=[dstbuf], out=xnT_dst[:, 8:16, :], in_=src)


def load_w_scaled(S, dst, dst_ap_fn, w_dram, g, nk, c0, c1, stg, q='sp'):
    for k in range(nk):
        st = stg[k % len(stg)]
        n = c1 - c0
        S.dma(q, st[:, 0:n], w_dram[k * 128:(k + 1) * 128, c0:c1], W=[st])
        if k % 2 == 0:
            S.dve('tensor_scalar', R=[st, g], W=[dst], out=dst_ap_fn(k), in0=st[:, 0:n],
                  scalar1=g[:, k:k + 1], scalar2=None, op0=ALU.mult)
        else:
            S.act(R=[st, g], W=[dst], out=dst_ap_fn(k), in_=st[:, 0:n], func=AF.Copy, scale=g[:, k:k + 1])


def emit_A(S, nc, T, io):
    NT = T // 128
    G = min(512, T)
    NG = T // G
    with ExitStack() as es:
        c = load_consts(S, es, io)
        xnT_all = S.sbuf(es, 'xnT_all', [128, 16, T], BF16)
        with ExitStack() as e1:
            w1 = S.sbuf(e1, 'w1', [128, 16, 832], BF16)
            wuq = S.sbuf(e1, 'wuq', [128, 4, 1536], BF16)
            wukv = S.sbuf(e1, 'wukv', [128, 2, 2048], BF16)
            stg = [S.sbuf(e1, 'stg', [128, 2048], F32) for _ in range(2)]
            g_mix = S.sbuf(e1, 'g_mix', [128, 16], F32)
            g_q = S.sbuf(e1, 'g_q', [128, 4], F32)
            g_kv = S.sbuf(e1, 'g_kv', [128, 2], F32)
            gqh = S.sbuf(e1, 'gqh', [128, 192], F32)
            gkh = S.sbuf(e1, 'gkh', [128, 192], F32)
            S.dma('sp', g_mix[:], io['g_mix'], W=[g_mix])
            S.dma('sp', g_q[:], io['g_q'], W=[g_q])
            S.dma('sp', g_kv[:], io['g_kv'], W=[g_kv])
            S.dma('sp', gqh[:], io['gqh'].partition_broadcast(128), W=[gqh])
            S.dma('sp', gkh[:], io['gkh'].partition_broadcast(128), W=[gkh])
            load_w_scaled(S, w1, lambda k: w1[:, k, :], io['w_in'], g_mix, 16, 0, 832, stg)
            load_w_scaled(S, wuq, lambda k: wuq[:, k, :], io['w_uq'], g_q, 4, 0, 1536, stg)
            load_w_scaled(S, wukv, lambda k: wukv[:, k, :], io['w_ukv'], g_kv, 2, 0, 2048, stg)
            xts = [S.sbuf(e1, 'xt', [128, D], F32) for _ in range(2)]
            wk = {'junk': S.sbuf(e1, 'junk', [128, D], BF16), 'ss': S.sbuf(e1, 'ss', [128, 4], F32),
                  'rstd': S.sbuf(e1, 'rstd', [128, 4], F32), 'xn': S.sbuf(e1, 'xn', [128, D], BF16),
                  'tp': [S.psum(e1, 'tp', [128, 1024], BF16) for _ in range(2)]}
            zps = S.psum(e1, 'zps', [128, 1024], F32)
            big = S.psum(e1, 'big', [128, 2048], F32)
            zt = S.sbuf(e1, 'zt', [128, 832], F32)
            cs = S.sbuf(e1, 'cs', [128, 32], F32)
            sn = S.sbuf(e1, 'sn', [128, 32], F32)
            sm = S.sbuf(e1, 'sm', [128, 16], F32)
            cn = S.sbuf(e1, 'cn', [128, 512], BF16)
            cT = S.sbuf(e1, 'cT', [128, 4, 128], BF16)
            sq = S.sbuf(e1, 'sq', [128, 1536], F32)
            ssh = S.sbuf(e1, 'ssh', [128, 8], F32)
            rsh = S.sbuf(e1, 'rsh', [128, 8], F32)
            qn = S.sbuf(e1, 'qn', [128, 8, 192], F32)
            qf = S.sbuf(e1, 'qf', [128, 8, 192], BF16)
            tr = S.sbuf(e1, 'tr', [128, 8, 32], F32)
            tr2 = S.sbuf(e1, 'tr2', [128, 8, 32], F32)
            kr = S.sbuf(e1, 'kr', [128, 64], F32)
            kpg = S.sbuf(e1, 'kpg', [128, 64], F32)
            vf = S.sbuf(e1, 'vf', [128, 8, 128], BF16)
            oTn = S.sbuf(e1, 'oTn', [128, 8, 128], BF16)
            oTr = S.sbuf(e1, 'oTr', [64, 8, 128], BF16)
            tp = wk['tp']

            def head_T_out(src, dst_dram, i):
                for h in range(8):
                    S.pe('transpose', R=[src, c['idb']], W=[tp[0]], out=tp[0][:, h * 128:(h + 1) * 128],
                         in_=src[:, h, 0:128], identity=c['idb'][:])
                for h in range(8):
                    S.pe('transpose', R=[src, c['idb']], W=[tp[1]], out=tp[1][0:64, h * 128:(h + 1) * 128],
                         in_=src[:, h, 128:192], identity=c['idb'][:])
                S.act(R=[tp[0]], W=[oTn], out=oTn[:], in_=tp[0][:, :].rearrange('p (h t) -> p h t', h=8),
                      func=AF.Copy)
                S.dve('tensor_copy', R=[tp[1]], W=[oTr], out=oTr[:],
                      in_=tp[1][0:64, :].rearrange('p (h t) -> p h t', h=8))
                S.dma('pool', dst_dram[:, 0:128, i * 128:(i + 1) * 128].rearrange('h d t -> d h t'), oTn[:], R=[oTn])
                S.dma('pool', dst_dram[:, 128:192, i * 128:(i + 1) * 128].rearrange('h d t -> d h t'), oTr[:], R=[oTr])

            def rope(src, x1, x2, o1, o2):
                sbuf_src, sbuf_dst = src
                shp = list(x1.shape)
                cb = cs[:, :].unsqueeze(1).to_broadcast(shp) if len(shp) == 3 else cs[:, :]
                sb_ = sn[:, :].unsqueeze(1).to_broadcast(shp) if len(shp) == 3 else sn[:, :]
                a1 = tr[:, 0:shp[1], :] if len(shp) == 3 else tr[:, 0, :]
                a2 = tr2[:, 0:shp[1], :] if len(shp) == 3 else tr2[:, 0, :]
                S.dve('tensor_tensor', R=[sbuf_src, cs], W=[tr], out=a1, in0=x1, in1=cb, op=ALU.mult)
                S.dve('tensor_tensor', R=[sbuf_src, sn], W=[tr2], out=a2, in0=x2, in1=sb_, op=ALU.mult)
                S.dve('tensor_tensor', R=[tr, tr2], W=[sbuf_dst], out=o1, in0=a1, in1=a2, op=ALU.subtract)
                S.dve('tensor_tensor', R=[sbuf_src, sn], W=[tr], out=a1, in0=x1, in1=sb_, op=ALU.mult)
                S.dve('tensor_tensor', R=[sbuf_src, cs], W=[tr2], out=a2, in0=x2, in1=cb, op=ALU.mult)
                S.dve('tensor_tensor', R=[tr, tr2], W=[sbuf_dst], out=o2, in0=a1, in1=a2, op=ALU.add)

            for i in range(NT):
                xt = xts[i % 2]
                S.dma('sp', xt[:], io['x'][i * 128:(i + 1) * 128, :], W=[xt])
                S.dma('sp', cs[:], io['cos'][i * 128:(i + 1) * 128, :], W=[cs])
                S.dma('sp', sn[:], io['sin'][i * 128:(i + 1) * 128, :], W=[sn])
                norm_T(S, xt, xnT_all[:, :, i * 128:(i + 1) * 128], xnT_all, wk, c)
                for (c0, c1) in ((0, 512), (512, 832)):
                    for k in range(16):
                        S.pe('matmul', R=[xnT_all, w1], W=[zps], out=zps[:, c0:c1],
                             lhsT=xnT_all[:, k, i * 128:(i + 1) * 128], rhs=w1[:, k, c0:c1],
                             start=(k == 0), stop=(k == 15))
                S.act(R=[zps], W=[zt], out=zt[:, 0:832], in_=zps[:, 0:832], func=AF.Copy)
                S.act(R=[zt], W=[wk['junk'], sm], out=wk['junk'][:, 0:512], in_=zt[:, 0:512], func=AF.Square,
                      accum_out=sm[:, 0:1])
                rstd_calc(S, (sm, sm[:, 0:1]), (sm, sm[:, 1:2]), 1.0 / 512)
                S.dve('tensor_scalar', R=[zt, sm], W=[cn], out=cn[:, 0:512], in0=zt[:, 0:512],
                      scalar1=sm[:, 1:2], scalar2=None, op0=ALU.mult)
                for k in range(4):
                    S.pe('transpose', R=[cn, c['idb']], W=[tp[0]], out=tp[0][:, k * 128:(k + 1) * 128],
                         in_=cn[:, k * 128:(k + 1) * 128], identity=c['idb'][:])
                S.dve('tensor_copy', R=[tp[0]], W=[cT], out=cT[:],
                      in_=tp[0][:, 0:512].rearrange('p (k t) -> p k t', k=4))
                for cb_ in range(3):
                    for k in range(4):
                        S.pe('matmul', R=[cT, wuq], W=[big], out=big[:, cb_ * 512:(cb_ + 1) * 512],
                             lhsT=cT[:, k, :], rhs=wuq[:, k, cb_ * 512:(cb_ + 1) * 512],
                             start=(k == 0), stop=(k == 3))
                S.act(R=[big], W=[sq], out=sq[:, 0:1536], in_=big[:, 0:1536], func=AF.Square)
                S.dve('tensor_reduce', R=[sq], W=[ssh], out=ssh[:],
                      in_=sq[:, 0:1536].rearrange('p (h d) -> p h d', h=8), axis=AX.X, op=ALU.add)
                rstd_calc(S, (ssh, ssh[:]), (rsh, rsh[:]), 1.0 / 192, post=192.0 ** -0.5)
                S.dve('tensor_tensor', R=[big, rsh], W=[qn], out=qn[:],
                      in0=big[:, 0:1536].rearrange('p (h d) -> p h d', h=8),
                      in1=bc(rsh[:, :].unsqueeze(2), [128, 8, 192]), op=ALU.mult)
                S.dve('tensor_tensor', R=[qn, gqh], W=[qn], out=qn[:], in0=qn[:],
                      in1=bc(gqh[:, :].unsqueeze(1), [128, 8, 192]), op=ALU.mult)
                S.act(R=[qn], W=[qf], out=qf[:, :, 0:128], in_=qn[:, :, 0:128], func=AF.Copy)
                rope((qn, qf), qn[:, :, 128:160], qn[:, :, 160:192], qf[:, :, 128:160], qf[:, :, 160:192])
                head_T_out(qf, io['QT'], i)
                S.act(R=[zt], W=[wk['junk'], sm], out=wk['junk'][:, 0:256], in_=zt[:, 512:768], func=AF.Square,
                      accum_out=sm[:, 2:3])
                rstd_calc(S, (sm, sm[:, 2:3]), (sm, sm[:, 3:4]), 1.0 / 256)
                S.dve('tensor_scalar', R=[zt, sm], W=[cn], out=cn[:, 0:256], in0=zt[:, 512:768],
                      scalar1=sm[:, 3:4], scalar2=None, op0=ALU.mult)
                for k in range(2):
                    S.pe('transpose', R=[cn, c['idb']], W=[tp[0]], out=tp[0][:, k * 128:(k + 1) * 128],
                         in_=cn[:, k * 128:(k + 1) * 128], identity=c['idb'][:])
                S.dve('tensor_copy', R=[tp[0]], W=[cT], out=cT[:, 0:2, :],
                      in_=tp[0][:, 0:256].rearrange('p (k t) -> p k t', k=2))
                for cb_ in range(4):
                    for k in range(2):
                        S.pe('matmul', R=[cT, wukv], W=[big], out=big[:, cb_ * 512:(cb_ + 1) * 512],
                             lhsT=cT[:, k, :], rhs=wukv[:, k, cb_ * 512:(cb_ + 1) * 512],
                             start=(k == 0), stop=(k == 1))
                kvv = big[:, :].rearrange('p (h d) -> p h d', h=8)
                S.act(R=[big], W=[sq], out=sq[:, 0:1024].rearrange('p (h d) -> p h d', h=8),
                      in_=kvv[:, :, 0:128], func=AF.Square)
                S.dve('tensor_reduce', R=[sq], W=[ssh], out=ssh[:],
                      in_=sq[:, 0:1024].rearrange('p (h d) -> p h d', h=8), axis=AX.X, op=ALU.add)
                S.act(R=[zt], W=[wk['junk'], sm], out=wk['junk'][:, 0:64], in_=zt[:, 768:832], func=AF.Square,
                      accum_out=sm[:, 4:5])
                S.dve('tensor_scalar', R=[ssh, sm], W=[ssh], out=ssh[:], in0=ssh[:], scalar1=sm[:, 4:5],
                      scalar2=None, op0=ALU.add)
                rstd_calc(S, (ssh, ssh[:]), (rsh, rsh[:]), 1.0 / 192)
                S.dve('tensor_tensor', R=[big, rsh], W=[qn], out=qn[:, :, 0:128], in0=kvv[:, :, 0:128],
                      in1=bc(rsh[:, :].unsqueeze(2), [128, 8, 128]), op=ALU.mult)
                S.dve('tensor_tensor', R=[qn, gkh], W=[qf], out=qf[:, :, 0:128], in0=qn[:, :, 0:128],
                      in1=bc(gkh[:, 0:128].unsqueeze(1), [128, 8, 128]), op=ALU.mult)
                S.dve('tensor_tensor', R=[zt, gkh], W=[kpg], out=kpg[:], in0=zt[:, 768:832], in1=gkh[:, 128:192],
                      op=ALU.mult)
                rope((kpg, kr), kpg[:, 0:32], kpg[:, 32:64], kr[:, 0:32], kr[:, 32:64])
                S.dve('tensor_tensor', R=[kr, rsh], W=[qf], out=qf[:, :, 128:192],
                      in0=bc(kr[:, :].unsqueeze(1), [128, 8, 64]), in1=bc(rsh[:, :].unsqueeze(2), [128, 8, 64]),
                      op=ALU.mult)
                S.act(R=[big], W=[vf], out=vf[:], in_=kvv[:, :, 128:256], func=AF.Copy)
                S.dma('pool', io['V'][:, :, i, :].rearrange('h p d -> p h d'), vf[:], R=[vf])
                head_T_out(qf, io['KT'], i)
        S.barrier()
        with ExitStack() as e2:
            w2 = S.sbuf(e2, 'w2', [128, 16, 2048], BF16)
            wrg = S.sbuf(e2, 'wrg', [128, 8, 128], BF16)
            wig = S.sbuf(e2, 'wig', [128, 8, 128], BF16)
            pv = S.sbuf(e2, 'pv', [128, 8, 8], F32)
            c1 = S.sbuf(e2, 'c1', [128, 8], F32)
            c2 = S.sbuf(e2, 'c2', [128, 8], F32)
            xhT = S.sbuf(e2, 'xhT', [128, 16, 128], BF16)
            g_mix = S.sbuf(e2, 'g_mix2', [128, 16], F32)
            e2a = ExitStack()
            stg = [S.sbuf(e2a, 'stg2', [128, 2048], F32) for _ in range(2)]
            S.dma('sp', g_mix[:], io['g_mix'], W=[g_mix])
            load_w_scaled(S, w2, lambda k: w2[:, k, :], io['w_in'], g_mix, 16, 832, 2880, stg)
            S.dma('sp', stg[0][:, 0:1024].rearrange('c (n d) -> c n d', n=8), io['w_rg'].rearrange('n c d -> c n d'),
                  W=[stg[0]])
            S.dve('tensor_copy', R=[stg[0]], W=[wrg], out=wrg[:], in_=stg[0][:, 0:1024].rearrange('c (n d) -> c n d', n=8))
            S.dma('sp', stg[1][:, 0:1024].rearrange('c (n d) -> c n d', n=8), io['w_ig'].rearrange('n c d -> c n d'),
                  W=[stg[1]])
            S.dve('tensor_copy', R=[stg[1]], W=[wig], out=wig[:], in_=stg[1][:, 0:1024].rearrange('c (n d) -> c n d', n=8))
            S.dma('sp', pv[:], io['lru_vec'], W=[pv])
            S.act(R=[pv], W=[c1], out=c1[:], in_=pv[:, :, 7], func=AF.Exp, scale=-1.0)
            S.act(R=[c1], W=[c2], out=c2[:], in_=c1[:], func=AF.Ln, bias=1.0)
            S.dve('tensor_scalar', R=[c2], W=[c1], out=c1[:], in0=c2[:], scalar1=-8.0, scalar2=None, op0=ALU.mult)
            S.dve('tensor_scalar', R=[c1], W=[c2], out=c2[:], in0=c1[:], scalar1=2.0, scalar2=None, op0=ALU.mult)
            xt = S.sbuf(e2a, 'xt2', [128, D], F32)
            wk = {'junk': S.sbuf(e2a, 'junk2', [128, D], BF16), 'ss': S.sbuf(e2a, 'ss2', [128, 4], F32),
                  'rstd': S.sbuf(e2a, 'rstd2', [128, 4], F32), 'xn': S.sbuf(e2a, 'xn2', [128, D], BF16),
                  'tp': [S.psum(e2, 'tp2', [128, 1024], BF16) for _ in range(2)]}
            S.dma('sp', xt[:], io['xh'], W=[xt])
            norm_T(S, xt, xhT[:, :, :], xhT, wk, c)
            e2a.close()
            S.barrier()
            xrb = S.sbuf(e2, 'xrb', [128, 8, G + 3], F32)
            hc = S.sbuf(e2, 'hc', [128, 8], F32)
            pc = S.sbuf(e2, 'pc', [128, 8], F32)
            zeros = S.sbuf(e2, 'zeros', [128, G], F32)
            S.dve('memset', W=[zeros], ap=zeros[:], constant=0.0)
            S.dve('memset', W=[hc], ap=hc[:], constant=0.0)
            S.dve('memset', W=[pc], ap=pc[:], constant=1.0)
            ps_x = [S.psum(e2, 'ps_x', [128, 512], F32) for _ in range(2)]
            ps_g = [S.psum(e2, 'ps_g', [128, 512], F32) for _ in range(2)]
            ps_r = S.psum(e2, 'ps_r', [128, 512], F32)
            ps_i = S.psum(e2, 'ps_i', [128, 512], F32)

            def mk(n_):
                return [S.sbuf(e2, n_, [128, G], F32) for _ in range(2)]
            xc_, r_, i_, a_, a2_, hl_, pl_, gg_ = (mk('xc'), mk('r'), mk('i'), mk('a'), mk('a2'), mk('hl'),
                                                   mk('pl'), mk('gg'))
            xcb_ = [S.sbuf(e2, 'xcb', [128, G], BF16) for _ in range(2)]
            it = 0
            for gi in range(NG):
                for n in range(8):
                    b = it % 2
                    it += 1
                    xc, r, ig, a, a2, hl, pl, gg, xcb = (xc_[b], r_[b], i_[b], a_[b], a2_[b], hl_[b], pl_[b],
                                                         gg_[b], xcb_[b])
                    px, pg = ps_x[b], ps_g[b]
                    cx = n * 128
                    cg = 1024 + n * 128
                    if gi == 0:
                        for k in range(16):
                            S.pe('matmul', R=[w2, xhT], W=[ps_r], out=ps_r[:, 0:3], lhsT=w2[:, k, cx:cx + 128],
                                 rhs=xhT[:, k, 125:128], start=(k == 0), stop=(k == 15))
                        S.dve('tensor_copy', R=[ps_r], W=[xrb], out=xrb[:, n, 0:3], in_=ps_r[:, 0:3])
                    else:
                        S.dve('tensor_copy', R=[xrb], W=[xrb], out=xrb[:, n, 0:3], in_=xrb[:, n, G:G + 3])
                    for k in range(16):
                        S.pe('matmul', R=[w2, xnT_all], W=[px], out=px[:, 0:G], lhsT=w2[:, k, cx:cx + 128],
                             rhs=xnT_all[:, k, gi * G:(gi + 1) * G], start=(k == 0), stop=(k == 15))
                    for k in range(16):
                        S.pe('matmul', R=[w2, xnT_all], W=[pg], out=pg[:, 0:G], lhsT=w2[:, k, cg:cg + 128],
                             rhs=xnT_all[:, k, gi * G:(gi + 1) * G], start=(k == 0), stop=(k == 15))
                    S.act(R=[px], W=[xrb], out=xrb[:, n, 3:3 + G], in_=px[:, 0:G], func=AF.Copy)
                    S.dve('tensor_scalar', R=[xrb, pv], W=[xc], out=xc[:], in0=xrb[:, n, 0:G], scalar1=pv[:, n, 0:1],
                          scalar2=pv[:, n, 4:5], op0=ALU.mult, op1=ALU.add)
                    for j in (1, 2, 3):
                        S.dve('scalar_tensor_tensor', R=[xrb, pv, xc], W=[xc], out=xc[:], in0=xrb[:, n, j:j + G],
                              scalar=pv[:, n, j:j + 1], in1=xc[:], op0=ALU.mult, op1=ALU.add)
                    S.act(R=[xc], W=[xcb], out=xcb[:], in_=xc[:], func=AF.Copy)
                    S.pe('matmul', R=[wrg, xcb], W=[ps_r], out=ps_r[:, 0:G], lhsT=wrg[:, n, :], rhs=xcb[:],
                         start=True, stop=True)
                    S.pe('matmul', R=[wig, xcb], W=[ps_i], out=ps_i[:, 0:G], lhsT=wig[:, n, :], rhs=xcb[:],
                         start=True, stop=True)
                    S.act(R=[ps_r, pv], W=[r], out=r[:], in_=ps_r[:, 0:G], func=AF.Sigmoid, bias=pv[:, n, 5:6])
                    S.act(R=[ps_i, pv], W=[ig], out=ig[:], in_=ps_i[:, 0:G], func=AF.Sigmoid, bias=pv[:, n, 6:7])
                    S.act(R=[r, c1], W=[a], out=a[:], in_=r[:], func=AF.Exp, scale=c1[:, n:n + 1])
                    S.act(R=[r, c2], W=[a2], out=a2[:], in_=r[:], func=AF.Exp, scale=c2[:, n:n + 1])
                    S.act(R=[pg], W=[gg], out=gg[:], in_=pg[:, 0:G], func=AF.Gelu)
                    S.dve('tensor_scalar', R=[a2], W=[a2], out=a2[:], in0=a2[:], scalar1=-1.0, scalar2=1.0,
                          op0=ALU.mult, op1=ALU.add)
                    S.dve('tensor_scalar', R=[a2], W=[a2], out=a2[:], in0=a2[:], scalar1=0.0, scalar2=None,
                          op0=ALU.max)
                    S.act(R=[a2], W=[a2], out=a2[:], in_=a2[:], func=AF.Sqrt)
                    S.dve('tensor_tensor', R=[ig, xc], W=[ig], out=ig[:], in0=ig[:], in1=xc[:], op=ALU.mult)
                    S.dve('tensor_tensor', R=[ig, a2], W=[ig], out=ig[:], in0=ig[:], in1=a2[:], op=ALU.mult)
                    S.dve('tensor_tensor_scan', R=[a, ig, hc], W=[hl], out=hl[:], data0=a[:], data1=ig[:],
                          initial=hc[:, n:n + 1], op0=ALU.mult, op1=ALU.add)
                    S.dve('tensor_tensor_scan', R=[a, zeros, pc], W=[pl], out=pl[:], data0=a[:], data1=zeros[:],
                          initial=pc[:, n:n + 1], op0=ALU.mult, op1=ALU.add)
                    S.dve('tensor_copy', R=[hl], W=[hc], out=hc[:, n:n + 1], in_=hl[:, G - 1:G])
                    S.dve('tensor_copy', R=[pl], W=[pc], out=pc[:, n:n + 1], in_=pl[:, G - 1:G])
                    S.dve('tensor_tensor', R=[hl, gg], W=[hl], out=hl[:], in0=hl[:], in1=gg[:], op=ALU.mult)
                    S.dve('tensor_tensor', R=[pl, gg], W=[pl], out=pl[:], in0=pl[:], in1=gg[:], op=ALU.mult)
                    S.dma('pool', io['Yl'][n, :, gi * G:(gi + 1) * G], hl[:], R=[hl])
                    S.dma('pool', io['Yp'][n, :, gi * G:(gi + 1) * G], pl[:], R=[pl])
            S.dma('pool', io['carry'][0], pc[:], R=[pc])
            S.dma('pool', io['carry'][1], hc[:], R=[hc])
        S.barrier()


def dram_in(nc, name, shape, dt=F32):
    return nc.dram_tensor(name, list(shape), dt, kind="ExternalInput").ap()


def dram_out(nc, name, shape, dt=F32):
    return nc.dram_tensor(name, list(shape), dt, kind="ExternalOutput").ap()


def build_A(T):
    nc = bass.Bass("TRN2", target_bir_lowering=False)
    io = {}
    for name, shape in (('x', [T, D]), ('xh', [128, D]), ('cos', [T, 32]), ('sin', [T, 32]), ('ident', [128, 128]),
                        ('g_mix', [128, 16]), ('g_q', [128, 4]), ('g_kv', [128, 2]), ('gqh', [192]), ('gkh', [192]),
                        ('w_in', [D, DIN]), ('w_uq', [512, 1536]), ('w_ukv', [256, 2048]),
                        ('w_rg', [8, 128, 128]), ('w_ig', [8, 128, 128]), ('lru_vec', [128, 8, 8])):
        io[name] = dram_in(nc, name, shape)
    io['QT'] = dram_out(nc, 'QT', [NH, 192, T], BF16)
    io['KT'] = dram_out(nc, 'KT', [NH, 192, T], BF16)
    io['V'] = dram_out(nc, 'V', [NH, 128, T // 128, 128], BF16)
    io['Yl'] = dram_out(nc, 'Yl', [8, 128, T])
    io['Yp'] = dram_out(nc, 'Yp', [8, 128, T])
    io['carry'] = dram_out(nc, 'carry', [2, 128, 8])
    with ExitStack() as es:
        S = Sched(nc, es)
        emit_A(S, nc, T, io)
        S.barrier()
    return nc


def col_layout(v, k):
    return np.ascontiguousarray(np.asarray(v, np.float32).reshape(k, 128).T)


def rope_tables(S_len):
    inv = (1.0 / (np.float32(10000.0) ** (np.arange(0, 64, 2, dtype=np.float32) / np.float32(64)))).astype(np.float32)
    ang = np.arange(S_len, dtype=np.float32)[:, None] * inv[None, :]
    return np.cos(ang).astype(np.float32), np.sin(ang).astype(np.float32)


def inputs_A(inp, l, xs, T):
    S_len = CPB * T
    cos, sin = rope_tables(S_len)
    lru_vec = np.stack([col_layout(inp['conv_w'][l][j], 8) for j in range(4)] +
                       [col_layout(inp[n][l], 8) for n in ('conv_b', 'b_rgate', 'b_igate', 'lru_lambda')], axis=2)
    shared = {
        'ident': np.eye(128, dtype=np.float32),
        'g_mix': col_layout(inp['mix_norm_g'][l], 16), 'g_q': col_layout(inp['q_lora_norm_g'][l], 4),
        'g_kv': col_layout(inp['kv_lora_norm_g'][l], 2),
        'gqh': np.ascontiguousarray(inp['att_q_norm_g'][l]), 'gkh': np.ascontiguousarray(inp['att_k_norm_g'][l]),
        'w_in': np.ascontiguousarray(inp['w_in'][l]), 'w_uq': np.ascontiguousarray(inp['w_uq'][l]),
        'w_ukv': np.ascontiguousarray(inp['w_ukv'][l]),
        'w_rg': np.ascontiguousarray(inp['w_rgate'][l]), 'w_ig': np.ascontiguousarray(inp['w_igate'][l]),
        'lru_vec': np.ascontiguousarray(lru_vec),
    }
    maps = []
    for cidx in range(NCORES):
        j = cidx % CPB
        m = dict(shared)
        m['x'] = xs[cidx]
        m['xh'] = xs[cidx - 1][T - 128:T] if j > 0 else np.zeros((128, D), np.float32)
        m['cos'] = np.ascontiguousarray(cos[j * T:(j + 1) * T])
        m['sin'] = np.ascontiguousarray(sin[j * T:(j + 1) * T])
        maps.append(m)
    return maps


def emit_B_attn(S, nc, T, io, c, yattT, ssatt):
    NT = T // 128
    G = min(512, T)
    NG = T // G
    GT = G // 128
    NOTH = CPB - 1
    with ExitStack() as es:
        tri = S.sbuf(es, 'tri', [128, 128], F32)
        kb = S.sbuf(es, 'kb', [128, NOTH], F32)
        onesf = S.sbuf(es, 'onesf', [128, 128], F32)
        S.dma('sp', tri[:], io['tri'], W=[tri])
        S.dma('sp', kb[:], io['kb'], W=[kb])
        S.dve('memset', W=[onesf], ap=onesf[:], constant=1.0)
        hb = []
        for _ in range(2):
            d_ = {
                'qn': S.sbuf(es, 'qn', [128, T], BF16), 'qr': S.sbuf(es, 'qr', [128, T], BF16),
                'kn': S.sbuf(es, 'kn', [128, T], BF16), 'kr': S.sbuf(es, 'kr', [128, T], BF16),
                'v': S.sbuf(es, 'v', [128, NT, 128], BF16),
                'Kn': S.sbuf(es, 'Kn', [128, NOTH, T], BF16), 'Kr': S.sbuf(es, 'Kr', [128, NOTH, T], BF16),
                'V': S.sbuf(es, 'V', [128, NOTH, NT, 128], BF16)}
            for n_ in ('qr', 'kr', 'Kr'):
                S.pool('memset', W=[d_[n_]], ap=d_[n_][:], constant=0.0)
            hb.append(d_)
        ps = [S.psum(es, 'ps', [128, 512], F32) for _ in range(3)]
        po = [S.psum(es, 'po', [128, 512], F32) for _ in range(2)]
        pd = S.psum(es, 'pd', [128, 512], F32)
        pss = S.psum(es, 'pss', [128, 512], F32)
        pT = [S.sbuf(es, 'pT', [128, G], BF16) for _ in range(4)]
        acc = [S.sbuf(es, 'acc', [128, G], F32) for _ in range(2)]
        rec = S.sbuf(es, 'rec', [128, G], F32)
        yf = S.sbuf(es, 'yf', [128, G], F32)
        sq = S.sbuf(es, 'sq', [128, G], F32)
        it = 0
        gi = 0
        for h in range(NH):
            B = hb[h % 2]
            S.dma('sp', B['qn'][:], io['QT'][h, 0:128, :], W=[B['qn']])
            S.dma('sp', B['qr'][0:64, :], io['QT'][h, 128:192, :], W=[B['qr']])
            S.dma('sp', B['kn'][:], io['KT'][h, 0:128, :], W=[B['kn']])
            S.dma('sp', B['kr'][0:64, :], io['KT'][h, 128:192, :], W=[B['kr']])
            S.dma('sp', B['v'][:], io['V'][h], W=[B['v']])
            S.dma('sp', B['Kn'][:], io['KT_oth'][:, h, 0:128, :].rearrange('s d t -> d s t'), W=[B['Kn']])
            S.dma('sp', B['Kr'][0:64], io['KT_oth'][:, h, 128:192, :].rearrange('s d t -> d s t'), W=[B['Kr']])
            for s in range(NOTH):
                S.dma('sp', B['V'][:, s], io['V_oth'][s, h], W=[B['V']])
            for g in range(NG):
                a_ = acc[gi % 2]
                o_ = po[gi % 2]
                gi += 1
                items = []
                for s in range(NOTH):
                    for kt in range(NT):
                        items.append((B['Kn'][:, s, kt * 128:(kt + 1) * 128], B['Kr'][:, s, kt * 128:(kt + 1) * 128],
                                      B['V'][:, s, kt, :], 0, False, kb[:, s:s + 1], ('Kn', 'Kr', 'V')))
                for kt in range((g + 1) * GT):
                    d = max(0, kt - g * GT)
                    items.append((B['kn'][:, kt * 128:(kt + 1) * 128], B['kr'][:, kt * 128:(kt + 1) * 128],
                                  B['v'][:, kt, :], d * 128, kt >= g * GT, None, ('kn', 'kr', 'v')))
                base = it
                it += len(items)

                def qk(idx):
                    kn_ap, kr_ap, v_ap, off, diag, bias, names = items[idx]
                    p_ = ps[(base + idx) % 3]
                    N = G - off
                    q0 = g * G + off
                    S.pe('matmul', R=[B[names[0]], B['qn']], W=[p_], out=p_[:, 0:N], lhsT=kn_ap,
                         rhs=B['qn'][:, q0:q0 + N], start=True, stop=False)
                    S.pe('matmul', R=[B[names[1]], B['qr']], W=[p_], out=p_[:, 0:N], lhsT=kr_ap,
                         rhs=B['qr'][:, q0:q0 + N], start=False, stop=True)

                def rest(idx):
                    kn_ap, kr_ap, v_ap, off, diag, bias, names = items[idx]
                    p_ = ps[(base + idx) % 3]
                    t_ = pT[(base + idx) % 4]
                    N = G - off
                    if bias is None:
                        S.act(R=[p_], W=[t_], out=t_[:, 0:N], in_=p_[:, 0:N], func=AF.Exp)
                    else:
                        S.act(R=[p_, kb], W=[t_], out=t_[:, 0:N], in_=p_[:, 0:N], func=AF.Exp, bias=bias)
                    if diag:
                        S.dve('tensor_tensor', R=[t_, tri], W=[t_], out=t_[:, 0:128], in0=t_[:, 0:128], in1=tri[:],
                              op=ALU.mult)
                    if idx == 0:
                        S.dve('tensor_copy', R=[t_], W=[a_], out=a_[:, 0:G], in_=t_[:, 0:G])
                    else:
                        S.dve('tensor_tensor', R=[t_, a_], W=[a_], out=a_[:, off:G], in0=a_[:, off:G], in1=t_[:, 0:N],
                              op=ALU.add)
                    S.pe('matmul', R=[B[names[2]], t_], W=[o_], out=o_[:, off:G], lhsT=v_ap, rhs=t_[:, 0:N],
                         start=(idx == 0), stop=(idx == len(items) - 1))

                qk(0)
                if len(items) > 1:
                    qk(1)
                for idx in range(len(items)):
                    if idx + 2 < len(items):
                        qk(idx + 2)
                    rest(idx)
                S.pe('matmul', R=[onesf, a_], W=[pd], out=pd[:, 0:G], lhsT=onesf[:], rhs=a_[:, 0:G], start=True, stop=True)
                S.dve('reciprocal', R=[pd], W=[rec], out=rec[:], in_=pd[:, 0:G])
                S.dve('tensor_tensor', R=[o_, rec], W=[yf], out=yf[:], in0=o_[:, 0:G], in1=rec[:], op=ALU.mult)
                S.act(R=[yf], W=[yattT], out=yattT[:, h, g * G:(g + 1) * G], in_=yf[:], func=AF.Copy)
                S.act(R=[yf], W=[sq], out=sq[:], in_=yf[:], func=AF.Square)
                for t in range(GT):
                    S.pe('matmul', R=[sq, onesf], W=[pss], out=pss[:, t:t + 1], lhsT=sq[:, t * 128:(t + 1) * 128],
                         rhs=onesf[:, 0:1], start=True, stop=True)
                if h == 0:
                    S.dve('tensor_copy', R=[pss], W=[ssatt], out=ssatt[:, g * GT:(g + 1) * GT], in_=pss[:, 0:GT])
                else:
                    S.dve('tensor_tensor', R=[pss, ssatt], W=[ssatt], out=ssatt[:, g * GT:(g + 1) * GT],
                          in0=ssatt[:, g * GT:(g + 1) * GT], in1=pss[:, 0:GT], op=ALU.add)
    S.barrier()


def head4_norm(S, wk4, src_ps, gbuf, post, dst):
    sq4, st4, qn4 = wk4
    S.act(R=[src_ps], W=[sq4], out=sq4[:], in_=src_ps[:, 0:512], func=AF.Square)
    S.dve('tensor_reduce', R=[sq4], W=[st4], out=st4[:, 0:4], in_=sq4[:, :].rearrange('p (h d) -> p h d', h=4),
          axis=AX.X, op=ALU.add)
    rstd_calc(S, (st4, st4[:, 0:4]), (st4, st4[:, 4:8]), 1.0 / 128, post=post)
    S.dve('tensor_tensor', R=[src_ps, st4], W=[qn4], out=qn4[:],
          in0=src_ps[:, 0:512].rearrange('p (h d) -> p h d', h=4),
          in1=bc(st4[:, 4:8].unsqueeze(2), [128, 4, 128]), op=ALU.mult)
    S.dve('tensor_tensor', R=[qn4, gbuf], W=[dst], out=dst[:], in0=qn4[:],
          in1=bc(gbuf[:, :].unsqueeze(1), [128, 4, 128]), op=ALU.mult)


def mk_wk(S, es):
    return {'junk': S.sbuf(es, 'junk', [128, D], BF16), 'ss': S.sbuf(es, 'ss', [128, 4], F32),
            'rstd': S.sbuf(es, 'rstd', [128, 4], F32), 'xn': S.sbuf(es, 'xn', [128, D], BF16),
            'tp': [S.psum(es, 'tp', [128, 1024], BF16) for _ in range(2)]}


def mk_wk4(S, es):
    return (S.sbuf(es, 'sq4', [128, 512], F32), S.sbuf(es, 'st4', [128, 8], F32), S.sbuf(es, 'qn4', [128, 4, 128], F32))


def emit_B_mid(S, nc, T, io, c, yattT, ssatt):
    NT = T // 128
    with ExitStack() as es:
        onesf = S.sbuf(es, 'onesf', [128, 128], F32)
        S.dve('memset', W=[onesf], ap=onesf[:], constant=1.0)
        yrT = S.sbuf(es, 'yrT', [128, 8, T], BF16)
        ssr = S.sbuf(es, 'ssr', [128, NT], F32)
        ra = S.sbuf(es, 'ra', [128, NT], F32)
        rr = S.sbuf(es, 'rr', [128, NT], F32)
        g_o = S.sbuf(es, 'g_o', [128, 16], F32)
        S.dma('sp', g_o[:], io['g_out'], W=[g_o])
        pA = S.psum(es, 'pA', [128, 512], F32)
        pB = S.psum(es, 'pB', [128, 512], F32)
        pC = S.psum(es, 'pC', [128, 512], F32)
        with ExitStack() as e1:
            car = S.sbuf(e1, 'car', [128, CPB, 2, 8], F32)
            cf = S.sbuf(e1, 'cf', [128, CPB], F32)
            h0 = S.sbuf(e1, 'h0', [128, 8], F32)
            tmp = S.sbuf(e1, 'tmp8', [128, 8], F32)
            S.dma('sp', car[:], io['carry_all'].rearrange('s a p n -> p s a n'), W=[car])
            S.dma('sp', cf[:], io['cf'], W=[cf])
            S.dve('memset', W=[h0], ap=h0[:], constant=0.0)
            for s_ in range(CPB):
                S.dve('tensor_tensor', R=[car, h0], W=[tmp], out=tmp[:], in0=car[:, s_, 0, :], in1=h0[:], op=ALU.mult)
                S.dve('tensor_tensor', R=[car, tmp], W=[tmp], out=tmp[:], in0=tmp[:], in1=car[:, s_, 1, :], op=ALU.add)
                S.dve('tensor_tensor', R=[tmp, h0], W=[tmp], out=tmp[:], in0=tmp[:], in1=h0[:], op=ALU.subtract)
                S.dve('scalar_tensor_tensor', R=[tmp, cf, h0], W=[h0], out=h0[:], in0=tmp[:], scalar=cf[:, s_:s_ + 1],
                      in1=h0[:], op0=ALU.mult, op1=ALU.add)
            yl = [S.sbuf(e1, 'yl', [128, T], F32) for _ in range(2)]
            yp = [S.sbuf(e1, 'yp', [128, T], F32) for _ in range(2)]
            for n in range(8):
                a, b = yl[n % 2], yp[n % 2]
                S.dma('sp', a[:], io['Yl'][n], W=[a])
                S.dma('sp', b[:], io['Yp'][n], W=[b])
                S.dve('scalar_tensor_tensor', R=[a, b, h0], W=[a], out=a[:], in0=b[:], scalar=h0[:, n:n + 1], in1=a[:],
                      op0=ALU.mult, op1=ALU.add)
                S.act(R=[a], W=[yrT], out=yrT[:, n, :], in_=a[:], func=AF.Copy)
                S.act(R=[a], W=[b], out=b[:], in_=a[:], func=AF.Square)
                for t in range(NT):
                    S.pe('matmul', R=[b, onesf], W=[pC], out=pC[:, t:t + 1], lhsT=b[:, t * 128:(t + 1) * 128],
                         rhs=onesf[:, 0:1], start=True, stop=True)
                if n == 0:
                    S.dve('tensor_copy', R=[pC], W=[ssr], out=ssr[:], in_=pC[:, 0:NT])
                else:
                    S.dve('tensor_tensor', R=[pC, ssr], W=[ssr], out=ssr[:], in0=ssr[:], in1=pC[:, 0:NT], op=ALU.add)
            rstd_calc(S, (ssr, ssr[:]), (rr, rr[:]), 1.0 / 1024)
            rstd_calc(S, (ssatt, ssatt[:]), (ra, ra[:]), 1.0 / 1024)
        S.barrier()
        wout = S.sbuf(es, 'wout', [128, 16, D], BF16)
        xts = [S.sbuf(es, 'xt', [128, D], F32) for _ in range(2)]
        x1s = [S.sbuf(es, 'x1', [128, D], F32) for _ in range(2)]
        load_w_scaled(S, wout, lambda k: wout[:, k, :], io['w_out'], g_o, 16, 0, D, x1s)
        for i in range(NT):
            xt, x1 = xts[i % 2], x1s[i % 2]
            tok = slice(i * 128, (i + 1) * 128)
            S.dma('sp', xt[:], io['x'][tok, :], W=[xt])
            for cb in range(4):
                cs_ = slice(cb * 512, (cb + 1) * 512)
                for k in range(8):
                    S.pe('matmul', R=[yattT, wout], W=[pA], out=pA[:, 0:512], lhsT=yattT[:, k, tok], rhs=wout[:, k, cs_],
                         start=(k == 0), stop=(k == 7))
                for k in range(8):
                    S.pe('matmul', R=[yrT, wout], W=[pB], out=pB[:, 0:512], lhsT=yrT[:, k, tok], rhs=wout[:, 8 + k, cs_],
                         start=(k == 0), stop=(k == 7))
                S.dve('scalar_tensor_tensor', R=[pA, ra, xt], W=[x1], out=x1[:, cs_], in0=pA[:, 0:512],
                      scalar=ra[:, i:i + 1], in1=xt[:, cs_], op0=ALU.mult, op1=ALU.add)
                S.dve('scalar_tensor_tensor', R=[pB, rr, x1], W=[x1], out=x1[:, cs_], in0=pB[:, 0:512],
                      scalar=rr[:, i:i + 1], in1=x1[:, cs_], op0=ALU.mult, op1=ALU.add)
            S.dma('pool', io['x2'][tok, :], x1[:], R=[x1])
    S.barrier()
    with ExitStack() as es:
        onesb = S.sbuf(es, 'onesb', [128, 128], BF16)
        S.dve('memset', W=[onesb], ap=onesb[:], constant=1.0)
        wmq = S.sbuf(es, 'wmq', [128, 16, 512], BF16)
        wmo = S.sbuf(es, 'wmo', [128, 4, D], BF16)
        kmT = S.sbuf(es, 'kmT', [128, 4, NMEM], BF16)
        vm = S.sbuf(es, 'vm', [128, 2, 512], BF16)
        gmq = S.sbuf(es, 'gmq', [128, 128], F32)
        gmk = S.sbuf(es, 'gmk', [128, 128], F32)
        g_x = S.sbuf(es, 'g_x', [128, 16], F32)
        g_m = S.sbuf(es, 'g_m', [128, 16], F32)
        one4 = S.sbuf(es, 'one4', [128, 4], F32)
        wk = mk_wk(S, es)
        wk4 = mk_wk4(S, es)
        qb4 = S.sbuf(es, 'qb4', [128, 4, 128], BF16)
        tp = wk['tp']
        pA = S.psum(es, 'pA', [128, 512], F32)
        pB = S.psum(es, 'pB', [128, 512], F32)
        pC = S.psum(es, 'pC', [128, 512], F32)
        pD = S.psum(es, 'pD', [128, 512], F32)
        xts = [S.sbuf(es, 'xt', [128, D], F32) for _ in range(2)]
        S.dve('memset', W=[one4], ap=one4[:], constant=1.0)
        S.dma('sp', gmq[:], io['g_memq'].partition_broadcast(128), W=[gmq])
        S.dma('sp', gmk[:], io['g_memk'].partition_broadcast(128), W=[gmk])
        S.dma('sp', g_x[:], io['g_xattn'], W=[g_x])
        S.dma('sp', g_m[:], io['g_mem'], W=[g_m])
        load_w_scaled(S, wmq, lambda k: wmq[:, k, :], io['w_mq'], g_x, 16, 0, 512, xts)
        load_w_scaled(S, wmo, lambda k: wmo[:, k, :], io['w_mo'], one4, 4, 0, D, xts)
        with ExitStack() as e1:
            wmk = S.sbuf(e1, 'wmk', [128, 16, 512], BF16)
            wmv = S.sbuf(e1, 'wmv', [128, 16, 512], BF16)
            mT = S.sbuf(e1, 'mT', [128, 16, 128], BF16)
            load_w_scaled(S, wmk, lambda k: wmk[:, k, :], io['w_mk'], g_m, 16, 0, 512, xts)
            load_w_scaled(S, wmv, lambda k: wmv[:, k, :], io['w_mv'], g_m, 16, 0, 512, xts)
            for mt in range(2):
                xt = xts[mt]
                S.dma('sp', xt[:], io['mem'][mt * 128:(mt + 1) * 128, :], W=[xt])
                norm_T(S, xt, mT[:, :, :], mT, wk, c)
                for k in range(16):
                    S.pe('matmul', R=[mT, wmk], W=[pA], out=pA[:, 0:512], lhsT=mT[:, k, :], rhs=wmk[:, k, :],
                         start=(k == 0), stop=(k == 15))
                for k in range(16):
                    S.pe('matmul', R=[mT, wmv], W=[pB], out=pB[:, 0:512], lhsT=mT[:, k, :], rhs=wmv[:, k, :],
                         start=(k == 0), stop=(k == 15))
                S.act(R=[pB], W=[vm], out=vm[:, mt, :], in_=pB[:, 0:512], func=AF.Copy)
                head4_norm(S, wk4, pA, gmk, 1.0, qb4)
                for h in range(4):
                    S.pe('transpose', R=[qb4, c['idb']], W=[tp[0]], out=tp[0][:, h * 128:(h + 1) * 128],
                         in_=qb4[:, h, :], identity=c['idb'][:])
                S.dve('tensor_copy', R=[tp[0]], W=[kmT], out=kmT[:, :, mt * 128:(mt + 1) * 128],
                      in_=tp[0][:, 0:512].rearrange('p (h t) -> p h t', h=4))
        S.barrier()
        xnT = S.sbuf(es, 'xnT', [128, 16, 128], BF16)
        qmT = S.sbuf(es, 'qmT', [128, 4, 128], BF16)
        pm = [S.sbuf(es, 'pm', [128, 512], BF16) for _ in range(2)]
        recm = S.sbuf(es, 'recm', [128, 512], F32)
        oT = S.sbuf(es, 'oT', [128, 4, 128], BF16)
        for i in range(NT):
            x1 = xts[i % 2]
            tok = slice(i * 128, (i + 1) * 128)
            S.dma('sp', x1[:], io['x2'][tok, :], W=[x1])
            norm_T(S, x1, xnT[:, :, :], xnT, wk, c)
            for k in range(16):
                S.pe('matmul', R=[xnT, wmq], W=[pC], out=pC[:, 0:512], lhsT=xnT[:, k, :], rhs=wmq[:, k, :],
                     start=(k == 0), stop=(k == 15))
            head4_norm(S, wk4, pC, gmq, 128.0 ** -0.5, qb4)
            for h in range(4):
                S.pe('transpose', R=[qb4, c['idb']], W=[tp[0]], out=tp[0][:, h * 128:(h + 1) * 128],
                     in_=qb4[:, h, :], identity=c['idb'][:])
            S.dve('tensor_copy', R=[tp[0]], W=[qmT], out=qmT[:], in_=tp[0][:, 0:512].rearrange('p (h t) -> p h t', h=4))
            for mt in range(2):
                for h in range(4):
                    S.pe('matmul', R=[kmT, qmT], W=[pD], out=pD[:, h * 128:(h + 1) * 128],
                         lhsT=kmT[:, h, mt * 128:(mt + 1) * 128], rhs=qmT[:, h, :], start=True, stop=True)
                S.act(R=[pD], W=[pm[mt]], out=pm[mt][:], in_=pD[:, 0:512], func=AF.Exp)
            for h in range(4):
                for mt in range(2):
                    S.pe('matmul', R=[vm, pm[mt]], W=[pA], out=pA[:, h * 128:(h + 1) * 128],
                         lhsT=vm[:, mt, h * 128:(h + 1) * 128], rhs=pm[mt][:, h * 128:(h + 1) * 128],
                         start=(mt == 0), stop=(mt == 1))
            for mt in range(2):
                S.pe('matmul', R=[onesb, pm[mt]], W=[pB], out=pB[:, 0:512], lhsT=onesb[:], rhs=pm[mt][:],
                     start=(mt == 0), stop=(mt == 1))
            S.dve('reciprocal', R=[pB], W=[recm], out=recm[:], in_=pB[:, 0:512])
            S.dve('tensor_tensor', R=[pA, recm], W=[oT], out=oT[:], in0=pA[:, 0:512].rearrange('p (h t) -> p h t', h=4),
                  in1=recm[:, :].rearrange('p (h t) -> p h t', h=4), op=ALU.mult)
            for cb in range(4):
                cs_ = slice(cb * 512, (cb + 1) * 512)
                pX = pC if cb % 2 == 0 else pD
                for h in range(4):
                    S.pe('matmul', R=[oT, wmo], W=[pX], out=pX[:, 0:512], lhsT=oT[:, h, :], rhs=wmo[:, h, cs_],
                         start=(h == 0), stop=(h == 3))
                S.dve('tensor_tensor', R=[pX, x1], W=[x1], out=x1[:, cs_], in0=x1[:, cs_], in1=pX[:, 0:512], op=ALU.add)
            S.dma('pool', io['x2'][tok, :], x1[:], R=[x1])
    S.barrier()


def emit_B_moe(S, nc, T, io, c):
    NT = T // 128
    G = min(512, T)
    NG = T // G
    GT = G // 128
    with ExitStack() as es:
        gm = S.sbuf(es, 'gm', [128, D], F32)
        wr = S.sbuf(es, 'wr', [128, 16, 36], F32)
        br = S.sbuf(es, 'br', [128, 36], F32)
        S.dma('sp', gm[:], io['g_moe'].partition_broadcast(128), W=[gm])
        S.dma('sp', wr[:], io['w_r'].rearrange('(k p) n -> p k n', p=128), W=[wr])
        S.dma('sp', br[:], io['b_r'].partition_broadcast(128), W=[br])
        wb = [{'g': S.sbuf(es, 'weg', [128, 16, DEXP], BF16), 'u': S.sbuf(es, 'weu', [128, 16, DEXP], BF16),
               'd': S.sbuf(es, 'wed', [128, 4, D], BF16)} for _ in range(2)]
        hT = S.sbuf(es, 'hT', [128, 16, G], BF16)
        aT = [S.sbuf(es, 'aT', [128, 4, G], BF16) for _ in range(2)]
        yacc = [S.sbuf(es, 'yacc', [128, D], F32) for _ in range(GT)]
        cw = S.sbuf(es, 'cw', [128, GT, 32], F32)
        hf = S.sbuf(es, 'hf', [128, D], F32)
        hb = S.sbuf(es, 'hb', [128, D], BF16)
        hT32 = S.sbuf(es, 'hT32', [128, 16, 128], F32)
        junk = S.sbuf(es, 'junk', [128, D], BF16)
        st = S.sbuf(es, 'st', [128, 16], F32)
        lg = S.sbuf(es, 'lg', [128, 36], F32)
        oh = S.sbuf(es, 'oh', [128, 4], F32)
        sel = S.sbuf(es, 'sel', [128, 8], F32)
        p8 = S.sbuf(es, 'p8', [128, 8], F32)
        p8b = S.sbuf(es, 'p8b', [128, 8], F32)
        mk1 = S.sbuf(es, 'mk1', [128, 8], F32)
        mk2 = S.sbuf(es, 'mk2', [128, 8], F32)
        sgt = [S.sbuf(es, 'sgt', [128, G], F32) for _ in range(2)]
        tpb = S.psum(es, 'tpb', [128, 1024], BF16)
        tpf = S.psum(es, 'tpf', [128, 512], F32)
        pg = [S.psum(es, 'pg', [128, 512], F32) for _ in range(2)]
        pu = [S.psum(es, 'pu', [128, 512], F32) for _ in range(2)]
        py = [S.psum(es, 'py', [128, 512], F32) for _ in range(2)]
        for g in range(NG):
            for t in range(GT):
                i = g * GT + t
                tok = slice(i * 128, (i + 1) * 128)
                ya = yacc[t]
                S.dma('sp', ya[:], io['x2'][tok, :], W=[ya])
                S.act(R=[ya], W=[junk, st], out=junk[:], in_=ya[:], func=AF.Square, accum_out=st[:, 0:1])
                rstd_calc(S, (st, st[:, 0:1]), (st, st[:, 1:2]), 1.0 / D)
                S.dve('scalar_tensor_tensor', R=[ya, st, gm], W=[hf], out=hf[:], in0=ya[:], scalar=st[:, 1:2], in1=gm[:],
                      op0=ALU.mult, op1=ALU.mult)
                S.act(R=[hf], W=[hb], out=hb[:], in_=hf[:], func=AF.Copy)
                for half in range(2):
                    for k in range(8):
                        kk = half * 8 + k
                        S.pe('transpose', R=[hb, c['idb']], W=[tpb], out=tpb[:, k * 128:(k + 1) * 128],
                             in_=hb[:, kk * 128:(kk + 1) * 128], identity=c['idb'][:])
                    S.act(R=[tpb], W=[hT], out=hT[:, half * 8:(half + 1) * 8, t * 128:(t + 1) * 128],
                          in_=tpb[:, :].rearrange('p (k t) -> p k t', k=8), func=AF.Copy)
                for q4 in range(4):
                    for k in range(4):
                        kk = q4 * 4 + k
                        S.pe('transpose', R=[hf, c['idf']], W=[tpf], out=tpf[:, k * 128:(k + 1) * 128],
                             in_=hf[:, kk * 128:(kk + 1) * 128], identity=c['idf'][:])
                    S.dve('tensor_copy', R=[tpf], W=[hT32], out=hT32[:, q4 * 4:(q4 + 1) * 4, :],
                          in_=tpf[:, :].rearrange('p (k t) -> p k t', k=4))
                pl = py[0]
                for k in range(16):
                    S.pe('matmul', R=[hT32, wr], W=[pl], out=pl[:, 0:36], lhsT=hT32[:, k, :], rhs=wr[:, k, :],
                         start=(k == 0), stop=(k == 15))
                S.dve('tensor_tensor', R=[pl, br], W=[lg], out=lg[:], in0=pl[:, 0:36], in1=br[:], op=ALU.add)
                S.dve('tensor_reduce', R=[lg], W=[st], out=st[:, 2:3], in_=lg[:, 0:4], axis=AX.X, op=ALU.max)
                S.dve('tensor_scalar', R=[st], W=[st], out=st[:, 3:4], in0=st[:, 2:3], scalar1=-1.0, scalar2=None,
                      op0=ALU.mult)
                S.act(R=[lg, st], W=[p8b, st], out=p8b[:, 0:4], in_=lg[:, 0:4], func=AF.Exp, bias=st[:, 3:4],
                      accum_out=st[:, 4:5])
                S.dve('reciprocal', R=[st], W=[st], out=st[:, 5:6], in_=st[:, 4:5])
                S.dve('tensor_scalar', R=[lg, st], W=[oh], out=oh[:], in0=lg[:, 0:4], scalar1=st[:, 2:3], scalar2=None,
                      op0=ALU.is_equal)
                S.dve('tensor_scalar', R=[lg, oh], W=[sel], out=sel[:], in0=lg[:, 4:12], scalar1=oh[:, 0:1], scalar2=None,
                      op0=ALU.mult)
                for gg_ in (1, 2, 3):
                    S.dve('scalar_tensor_tensor', R=[lg, oh, sel], W=[sel], out=sel[:], in0=lg[:, 4 + 8 * gg_:12 + 8 * gg_],
                          scalar=oh[:, gg_:gg_ + 1], in1=sel[:], op0=ALU.mult, op1=ALU.add)
                S.dve('tensor_reduce', R=[sel], W=[st], out=st[:, 6:7], in_=sel[:], axis=AX.X, op=ALU.max)
                S.dve('tensor_scalar', R=[st], W=[st], out=st[:, 7:8], in0=st[:, 6:7], scalar1=-1.0, scalar2=None,
                      op0=ALU.mult)
                S.act(R=[sel, st], W=[p8], out=p8[:], in_=sel[:], func=AF.Exp, bias=st[:, 7:8])
                S.dve('tensor_reduce', R=[p8], W=[st], out=st[:, 8:9], in_=p8[:], axis=AX.X, op=ALU.max)
                S.dve('tensor_scalar', R=[p8, st], W=[mk1], out=mk1[:], in0=p8[:], scalar1=st[:, 8:9], scalar2=None,
                      op0=ALU.is_equal)
                S.dve('scalar_tensor_tensor', R=[mk1, p8], W=[p8b], out=p8b[:], in0=mk1[:], scalar=-2.0, in1=p8[:],
                      op0=ALU.mult, op1=ALU.add)
                S.dve('tensor_reduce', R=[p8b], W=[st], out=st[:, 9:10], in_=p8b[:], axis=AX.X, op=ALU.max)
                S.dve('tensor_scalar', R=[p8b, st], W=[mk2], out=mk2[:], in0=p8b[:], scalar1=st[:, 9:10], scalar2=None,
                      op0=ALU.is_equal)
                S.dve('tensor_tensor', R=[mk1, mk2], W=[mk1], out=mk1[:], in0=mk1[:], in1=mk2[:], op=ALU.add)
                S.dve('tensor_tensor', R=[st], W=[st], out=st[:, 10:11], in0=st[:, 8:9], in1=st[:, 9:10], op=ALU.add)
                S.dve('reciprocal', R=[st], W=[st], out=st[:, 11:12], in_=st[:, 10:11])
                S.dve('tensor_tensor', R=[st], W=[st], out=st[:, 12:13], in0=st[:, 11:12], in1=st[:, 5:6], op=ALU.mult)
                S.dve('scalar_tensor_tensor', R=[p8, st, mk1], W=[p8b], out=p8b[:], in0=p8[:], scalar=st[:, 12:13],
                      in1=mk1[:], op0=ALU.mult, op1=ALU.mult)
                S.dve('tensor_tensor', R=[oh, p8b], W=[cw], out=cw[:, t, :].rearrange('p (a b) -> p a b', a=4),
                      in0=bc(oh[:, :].unsqueeze(2), [128, 4, 8]), in1=bc(p8b[:, :].unsqueeze(1), [128, 4, 8]),
                      op=ALU.mult)
            for e in range(NEXP):
                W_ = wb[e % 2]
                S.dma('pool', W_['g'][:], io['w_eg'][e].rearrange('(k p) f -> p k f', p=128), W=[W_['g']])
                S.dma('pool', W_['u'][:], io['w_eu'][e].rearrange('(k p) f -> p k f', p=128), W=[W_['u']])
                S.dma('pool', W_['d'][:], io['w_ed'][e].rearrange('(k p) n -> p k n', p=128), W=[W_['d']])
                a_ = aT[e % 2]
                for fc in range(4):
                    pg_, pu_, sg_ = pg[fc % 2], pu[fc % 2], sgt[fc % 2]
                    fs = slice(fc * 128, (fc + 1) * 128)
                    for k in range(16):
                        S.pe('matmul', R=[W_['g'], hT], W=[pg_], out=pg_[:, 0:G], lhsT=W_['g'][:, k, fs], rhs=hT[:, k, :],
                             start=(k == 0), stop=(k == 15))
                    for k in range(16):
                        S.pe('matmul', R=[W_['u'], hT], W=[pu_], out=pu_[:, 0:G], lhsT=W_['u'][:, k, fs], rhs=hT[:, k, :],
                             start=(k == 0), stop=(k == 15))
                    S.act(R=[pg_], W=[sg_], out=sg_[:], in_=pg_[:, 0:G], func=AF.Silu)
                    S.dve('tensor_tensor', R=[sg_, pu_], W=[a_], out=a_[:, fc, :], in0=sg_[:], in1=pu_[:, 0:G], op=ALU.mult)
                for t in range(GT):
                    for cb in range(4):
                        py_ = py[(t * 4 + cb) % 2]
                        cs_ = slice(cb * 512, (cb + 1) * 512)
                        for fc in range(4):
                            S.pe('matmul', R=[a_, W_['d']], W=[py_], out=py_[:, 0:512], lhsT=a_[:, fc, t * 128:(t + 1) * 128],
                                 rhs=W_['d'][:, fc, cs_], start=(fc == 0), stop=(fc == 3))
                        S.dve('scalar_tensor_tensor', R=[py_, cw, yacc[t]], W=[yacc[t]], out=yacc[t][:, cs_], in0=py_[:, 0:512],
                              scalar=cw[:, t, e:e + 1], in1=yacc[t][:, cs_], op0=ALU.mult, op1=ALU.add)
            for t in range(GT):
                i = g * GT + t
                S.dma('sp', io['xo'][i * 128:(i + 1) * 128, :], yacc[t][:], R=[yacc[t]])
    S.barrier()


def emit_B(S, nc, T, io, sparse=True):
    with ExitStack() as es:
        c = load_consts(S, es, io)
        with ExitStack() as e1:
            yattT = S.sbuf(e1, 'yattT', [128, NH, T], BF16)
            ssatt = S.sbuf(e1, 'ssatt', [128, T // 128], F32)
            emit_B_attn(S, nc, T, io, c, yattT, ssatt)
            emit_B_mid(S, nc, T, io, c, yattT, ssatt)
        S.barrier()
        if sparse:
            emit_B_moe_sparse(S, nc, T, io, c)
        else:
            emit_B_moe(S, nc, T, io, c)


def build_B(T, sparse=True):
    nc = bass.Bass("TRN2", target_bir_lowering=False)
    io = {}
    NT = T // 128
    for name, shape, dt in (
            ('x', [T, D], F32), ('ident', [128, 128], F32), ('tri', [128, 128], F32), ('kb', [128, CPB - 1], F32),
            ('QT', [NH, 192, T], BF16), ('KT', [NH, 192, T], BF16), ('V', [NH, 128, NT, 128], BF16),
            ('KT_oth', [CPB - 1, NH, 192, T], BF16), ('V_oth', [CPB - 1, NH, 128, NT, 128], BF16),
            ('Yl', [8, 128, T], F32), ('Yp', [8, 128, T], F32), ('carry_all', [CPB, 2, 128, 8], F32),
            ('cf', [128, CPB], F32), ('g_out', [128, 16], F32), ('w_out', [D, D], F32),
            ('g_xattn', [128, 16], F32), ('g_mem', [128, 16], F32), ('g_memq', [128], F32), ('g_memk', [128], F32),
            ('w_mq', [D, 512], F32), ('w_mk', [D, 512], F32), ('w_mv', [D, 512], F32), ('w_mo', [512, D], F32),
            ('mem', [NMEM, D], F32), ('g_moe', [D], F32), ('w_r', [D, 36], F32), ('b_r', [36], F32),
            ('w_eg', [NEXP, D, DEXP], F32), ('w_eu', [NEXP, D, DEXP], F32), ('w_ed', [NEXP, DEXP, D], F32)):
        io[name] = dram_in(nc, name, shape, dt)
    io['x2'] = dram_out(nc, 'x2', [T, D])
    io['xo'] = dram_out(nc, 'xo', [T, D])
    if sparse:
        for name, shape in (('ecap', [32]), ('ustrict', [128, 128]), ('tok2', [128, NT, 2])):
            io[name] = dram_in(nc, name, shape)
        io['cnt'] = dram_out(nc, 'cnt', [1, 32])
        io['Xs'] = nc.dram_tensor('Xs', [NEXP * CAP, D], BF16, kind="Internal").ap()
        io['meta'] = nc.dram_tensor('meta', [NEXP * CAP, 16], F32, kind="Internal").ap()
        io['Yt'] = [nc.dram_tensor('Yt%d' % hh, [2 * T, 1024], F32, kind="Internal").ap() for hh in range(2)]
    with ExitStack() as es:
        S = Sched(nc, es)
        emit_B(S, nc, T, io, sparse)
        S.barrier()
    return nc


def inputs_B(inp, l, xs, resA, T):
    g = lambda n: np.ascontiguousarray(inp[n][l])
    tri = np.triu(np.ones((128, 128), np.float32))
    shared = {
        'ident': np.eye(128, dtype=np.float32), 'tri': tri,
        'g_out': col_layout(np.concatenate([inp['att_out_norm_g'][l], inp['rnn_out_norm_g'][l]]), 16),
        'w_out': g('w_out'), 'g_xattn': col_layout(inp['xattn_norm_g'][l], 16), 'g_mem': col_layout(inp['mem_norm_g'][l], 16),
        'g_memq': g('mem_q_norm_g'), 'g_memk': g('mem_k_norm_g'),
        'w_mq': g('w_mq'), 'w_mk': g('w_mk'), 'w_mv': g('w_mv'), 'w_mo': g('w_mo'),
        'g_moe': g('moe_norm_g'),
        'w_r': np.ascontiguousarray(np.concatenate([inp['w_router_group'][l], inp['w_router_expert'][l]], axis=1)),
        'b_r': np.ascontiguousarray(np.concatenate([inp['b_router_group'][l], inp['b_router_expert'][l]])),
        'w_eg': g('w_exp_gate'), 'w_eu': g('w_exp_up'), 'w_ed': g('w_exp_down'),
    }
    NT = T // 128
    t_idx = (np.arange(NT)[None, :] * 128 + np.arange(128)[:, None]).astype(np.float32)
    shared['ecap'] = (np.arange(32) * CAP).astype(np.float32)
    shared['ustrict'] = np.triu(np.ones((128, 128), np.float32), 1)
    shared['tok2'] = np.ascontiguousarray(np.stack([2 * t_idx, 2 * t_idx + 1], axis=2))
    maps = []
    for cidx in range(NCORES):
        b, j = cidx // CPB, cidx % CPB
        grp = range(b * CPB, (b + 1) * CPB)
        m = dict(shared)
        m['x'] = xs[cidx]
        m['mem'] = np.ascontiguousarray(inp['mem'][b])
        for n in ('QT', 'KT', 'V', 'Yl', 'Yp'):
            m[n] = resA[cidx][n]
        oth = [k for k in grp if k != cidx]
        m['KT_oth'] = np.stack([resA[k]['KT'] for k in oth])
        m['V_oth'] = np.stack([resA[k]['V'] for k in oth])
        m['carry_all'] = np.stack([resA[k]['carry'] for k in grp])
        kb = np.zeros((128, CPB - 1), np.float32)
        for si, k in enumerate(oth):
            if k > cidx:
                kb[:, si] = -30000.0
        m['kb'] = kb
        cf = np.zeros((128, CPB), np.float32)
        cf[:, :j] = 1.0
        m['cf'] = cf
        maps.append(m)
    return maps


_NC_CACHE = {}


def _get_nc(kind, T):
    key = (kind, T)
    if key not in _NC_CACHE:
        _NC_CACHE[key] = build_A(T) if kind == 'A' else build_B(T)
    return _NC_CACHE[key]


def run_layers(inp, T):
    B = inp['x'].shape[0]
    x = np.asarray(inp['x'], np.float32)
    xs = [np.ascontiguousarray(x[c // CPB, (c % CPB) * T:(c % CPB + 1) * T]) for c in range(NCORES)]
    inp = {k: np.asarray(v) for k, v in inp.items()}
    for l in range(2):
        ra = run_bass_kernel_spmd(build_A(T), inputs_A(inp, l, xs, T), core_ids=list(range(NCORES))).results
        mb = inputs_B(inp, l, xs, ra, T)
        rb = run_bass_kernel_spmd(build_B(T, True), mb, core_ids=list(range(NCORES))).results
        if max(float(np.max(r['cnt'])) for r in rb) > CAP:
            for m in mb:
                for n in ('ecap', 'ustrict', 'tok2'):
                    m.pop(n)
            rb = run_bass_kernel_spmd(build_B(T, False), mb, core_ids=list(range(NCORES))).results
        xs = [np.ascontiguousarray(rb[c]['xo']) for c in range(NCORES)]
    out = np.empty((B, CPB * T, D), np.float32)
    for c in range(NCORES):
        out[c // CPB, (c % CPB) * T:(c % CPB + 1) * T] = xs[c]
    return out


def kernel(**inputs):
    return run_layers(inputs, 2048)


def emit_B_moe_sparse(S, nc, T, io, c):
    NT = T // 128
    NB = CAP // 128
    NSLOT = NEXP * CAP
    with ExitStack() as es:
        onesb = S.sbuf(es, 'onesb', [128, 128], BF16)
        S.dve('memset', W=[onesb], ap=onesb[:], constant=1.0)
        breg_slot = nc.gpsimd.to_reg(NSLOT - 1)
        breg_tok = nc.gpsimd.to_reg(2 * T - 1)
        with ExitStack() as e1:
            gm = S.sbuf(e1, 'gm', [128, D], F32)
            wr = S.sbuf(e1, 'wr', [128, 16, 36], F32)
            br = S.sbuf(e1, 'br', [128, 36], F32)
            ecap = S.sbuf(e1, 'ecap', [128, 32], F32)
            ustr = S.sbuf(e1, 'ustr', [128, 128], F32)
            ustb = S.sbuf(e1, 'ustb', [128, 128], BF16)
            tok2 = S.sbuf(e1, 'tok2', [128, NT, 2], F32)
            S.dma('sp', gm[:], io['g_moe'].partition_broadcast(128), W=[gm])
            S.dma('sp', wr[:], io['w_r'].rearrange('(k p) n -> p k n', p=128), W=[wr])
            S.dma('sp', br[:], io['b_r'].partition_broadcast(128), W=[br])
            S.dma('sp', ecap[:], io['ecap'].partition_broadcast(128), W=[ecap])
            S.dma('sp', ustr[:], io['ustrict'], W=[ustr])
            S.dma('sp', tok2[:], io['tok2'], W=[tok2])
            S.dve('tensor_copy', R=[ustr], W=[ustb], out=ustb[:], in_=ustr[:])
            mi = S.sbuf(e1, 'mi', [128, NSLOT // 128, 16], F32)
            S.dve('memset', W=[mi], ap=mi[:], constant=0.0)
            S.dve('memset', W=[mi], ap=mi[:, :, 0:1], constant=1.0e6)
            S.dma('sp', io['meta'].rearrange('(p a) m -> p a m', p=128), mi[:], R=[mi])
            Mall = S.sbuf(e1, 'Mall', [128, NT, 32], BF16)
            ya = S.sbuf(e1, 'ya', [128, D], F32)
            hf = S.sbuf(e1, 'hf', [128, D], F32)
            hbs = [S.sbuf(e1, 'hb', [128, D], BF16) for _ in range(2)]
            hT32 = S.sbuf(e1, 'hT32', [128, 16, 128], F32)
            junk = S.sbuf(e1, 'junk', [128, D], BF16)
            st = S.sbuf(e1, 'st', [128, 16], F32)
            lg = S.sbuf(e1, 'lg', [128, 36], F32)
            oh = S.sbuf(e1, 'oh', [128, 4], F32)
            sel = S.sbuf(e1, 'sel', [128, 8], F32)
            p8 = S.sbuf(e1, 'p8', [128, 8], F32)
            p8b = S.sbuf(e1, 'p8b', [128, 8], F32)
            mk1 = S.sbuf(e1, 'mk1', [128, 8], F32)
            mk2 = S.sbuf(e1, 'mk2', [128, 8], F32)
            M1 = S.sbuf(e1, 'M1', [128, 32], F32)
            M2 = S.sbuf(e1, 'M2', [128, 32], F32)
            Mt = S.sbuf(e1, 'Mt', [128, 32], F32)
            psl = S.sbuf(e1, 'psl', [128, 32], F32)
            pen = S.sbuf(e1, 'pen', [128, 32], F32)
            sk = S.sbuf(e1, 'sk', [128, 2], F32)
            metas = [[S.sbuf(e1, 'mrow', [128, 16], F32) for _ in range(2)] for _ in range(2)]
            skis = [[S.sbuf(e1, 'ski', [128, 1], I32) for _ in range(2)] for _ in range(2)]
            tpf = S.psum(e1, 'tpf', [128, 512], F32)
            plg = S.psum(e1, 'plg', [128, 512], F32)
            ppos = S.psum(e1, 'ppos', [128, 512], F32)
            for i in range(NT):
                tok = slice(i * 128, (i + 1) * 128)
                hb = hbs[i % 2]
                S.dma('sp', ya[:], io['x2'][tok, :], W=[ya])
                S.act(R=[ya], W=[junk, st], out=junk[:], in_=ya[:], func=AF.Square, accum_out=st[:, 0:1])
                rstd_calc(S, (st, st[:, 0:1]), (st, st[:, 1:2]), 1.0 / D)
                S.dve('scalar_tensor_tensor', R=[ya, st, gm], W=[hf], out=hf[:], in0=ya[:], scalar=st[:, 1:2], in1=gm[:],
                      op0=ALU.mult, op1=ALU.mult)
                S.act(R=[hf], W=[hb], out=hb[:], in_=hf[:], func=AF.Copy)
                for q4 in range(4):
                    for k in range(4):
                        kk = q4 * 4 + k
                        S.pe('transpose', R=[hf, c['idf']], W=[tpf], out=tpf[:, k * 128:(k + 1) * 128],
                             in_=hf[:, kk * 128:(kk + 1) * 128], identity=c['idf'][:])
                    S.dve('tensor_copy', R=[tpf], W=[hT32], out=hT32[:, q4 * 4:(q4 + 1) * 4, :],
                          in_=tpf[:, :].rearrange('p (k t) -> p k t', k=4))
                for k in range(16):
                    S.pe('matmul', R=[hT32, wr], W=[plg], out=plg[:, 0:36], lhsT=hT32[:, k, :], rhs=wr[:, k, :],
                         start=(k == 0), stop=(k == 15))
                S.dve('tensor_tensor', R=[plg, br], W=[lg], out=lg[:], in0=plg[:, 0:36], in1=br[:], op=ALU.add)
                S.dve('tensor_reduce', R=[lg], W=[st], out=st[:, 2:3], in_=lg[:, 0:4], axis=AX.X, op=ALU.max)
                S.dve('tensor_scalar', R=[st], W=[st], out=st[:, 3:4], in0=st[:, 2:3], scalar1=-1.0, scalar2=None,
                      op0=ALU.mult)
                S.act(R=[lg, st], W=[p8b, st], out=p8b[:, 0:4], in_=lg[:, 0:4], func=AF.Exp, bias=st[:, 3:4],
                      accum_out=st[:, 4:5])
                S.dve('reciprocal', R=[st], W=[st], out=st[:, 5:6], in_=st[:, 4:5])
                S.dve('tensor_scalar', R=[lg, st], W=[oh], out=oh[:], in0=lg[:, 0:4], scalar1=st[:, 2:3], scalar2=None,
                      op0=ALU.is_equal)
                S.dve('tensor_scalar', R=[lg, oh], W=[sel], out=sel[:], in0=lg[:, 4:12], scalar1=oh[:, 0:1], scalar2=None,
                      op0=ALU.mult)
                for gg_ in (1, 2, 3):
                    S.dve('scalar_tensor_tensor', R=[lg, oh, sel], W=[sel], out=sel[:], in0=lg[:, 4 + 8 * gg_:12 + 8 * gg_],
                          scalar=oh[:, gg_:gg_ + 1], in1=sel[:], op0=ALU.mult, op1=ALU.add)
                S.dve('tensor_reduce', R=[sel], W=[st], out=st[:, 6:7], in_=sel[:], axis=AX.X, op=ALU.max)
                S.dve('tensor_scalar', R=[st], W=[st], out=st[:, 7:8], in0=st[:, 6:7], scalar1=-1.0, scalar2=None,
                      op0=ALU.mult)
                S.act(R=[sel, st], W=[p8], out=p8[:], in_=sel[:], func=AF.Exp, bias=st[:, 7:8])
                S.dve('tensor_reduce', R=[p8], W=[st], out=st[:, 8:9], in_=p8[:], axis=AX.X, op=ALU.max)
                S.dve('tensor_scalar', R=[p8, st], W=[mk1], out=mk1[:], in0=p8[:], scalar1=st[:, 8:9], scalar2=None,
                      op0=ALU.is_equal)
                S.dve('scalar_tensor_tensor', R=[mk1, p8], W=[p8b], out=p8b[:], in0=mk1[:], scalar=-2.0, in1=p8[:],
                      op0=ALU.mult, op1=ALU.add)
                S.dve('tensor_reduce', R=[p8b], W=[st], out=st[:, 9:10], in_=p8b[:], axis=AX.X, op=ALU.max)
                S.dve('tensor_scalar', R=[p8b, st], W=[mk2], out=mk2[:], in0=p8b[:], scalar1=st[:, 9:10], scalar2=None,
                      op0=ALU.is_equal)
                S.dve('tensor_tensor', R=[st], W=[st], out=st[:, 10:11], in0=st[:, 8:9], in1=st[:, 9:10], op=ALU.add)
                S.dve('reciprocal', R=[st], W=[st], out=st[:, 11:12], in_=st[:, 10:11])
                S.dve('tensor_tensor', R=[st], W=[st], out=st[:, 12:13], in0=st[:, 11:12], in1=st[:, 5:6], op=ALU.mult)
                m1r, m2r = metas[i % 2]
                S.dve('memset', W=[m1r], ap=m1r[:], constant=0.0)
                S.dve('memset', W=[m2r], ap=m2r[:], constant=0.0)
                S.dve('tensor_copy', R=[tok2], W=[m1r], out=m1r[:, 0:1], in_=tok2[:, i, 0:1])
                S.dve('tensor_copy', R=[tok2], W=[m2r], out=m2r[:, 0:1], in_=tok2[:, i, 1:2])
                S.dve('tensor_tensor', R=[st], W=[m1r], out=m1r[:, 1:2], in0=st[:, 8:9], in1=st[:, 12:13], op=ALU.mult)
                S.dve('tensor_tensor', R=[st], W=[m2r], out=m2r[:, 1:2], in0=st[:, 9:10], in1=st[:, 12:13], op=ALU.mult)
                S.dve('tensor_tensor', R=[oh, mk1], W=[M1], out=M1[:, :].rearrange('p (a b) -> p a b', a=4),
                      in0=bc(oh[:, :].unsqueeze(2), [128, 4, 8]), in1=bc(mk1[:, :].unsqueeze(1), [128, 4, 8]), op=ALU.mult)
                S.dve('tensor_tensor', R=[oh, mk2], W=[M2], out=M2[:, :].rearrange('p (a b) -> p a b', a=4),
                      in0=bc(oh[:, :].unsqueeze(2), [128, 4, 8]), in1=bc(mk2[:, :].unsqueeze(1), [128, 4, 8]), op=ALU.mult)
                S.dve('tensor_tensor', R=[M1, M2], W=[Mt], out=Mt[:], in0=M1[:], in1=M2[:], op=ALU.add)
                S.dve('tensor_copy', R=[Mt], W=[Mall], out=Mall[:, i, :], in_=Mt[:])
                S.pe('matmul', R=[ustb, Mall], W=[ppos], out=ppos[:, 0:32], lhsT=ustb[:], rhs=Mall[:, i, :],
                     start=True, stop=(i == 0))
                for i2 in range(i):
                    S.pe('matmul', R=[onesb, Mall], W=[ppos], out=ppos[:, 0:32], lhsT=onesb[:], rhs=Mall[:, i2, :],
                         start=False, stop=(i2 == i - 1))
                S.dve('tensor_scalar', R=[ppos], W=[pen], out=pen[:], in0=ppos[:, 0:32], scalar1=float(CAP), scalar2=1.0e6,
                      op0=ALU.is_ge, op1=ALU.mult)
                S.dve('tensor_tensor', R=[ppos, ecap], W=[psl], out=psl[:], in0=ppos[:, 0:32], in1=ecap[:], op=ALU.add)
                S.dve('tensor_tensor', R=[psl, pen], W=[psl], out=psl[:], in0=psl[:], in1=pen[:], op=ALU.add)
                S.dve('tensor_tensor', R=[psl, M1], W=[pen], out=pen[:], in0=psl[:], in1=M1[:], op=ALU.mult)
                S.dve('tensor_reduce', R=[pen], W=[sk], out=sk[:, 0:1], in_=pen[:], axis=AX.X, op=ALU.add)
                S.dve('tensor_tensor', R=[psl, M2], W=[pen], out=pen[:], in0=psl[:], in1=M2[:], op=ALU.mult)
                S.dve('tensor_reduce', R=[pen], W=[sk], out=sk[:, 1:2], in_=pen[:], axis=AX.X, op=ALU.add)
                for kk in range(2):
                    ski = skis[i % 2][kk]
                    mr = metas[i % 2][kk]
                    S.dve('tensor_copy', R=[sk], W=[ski], out=ski[:], in_=sk[:, kk:kk + 1])
                    S.indirect(R=[hb, ski], out=io['Xs'], out_offset=bass.IndirectOffsetOnAxis(ap=ski[:, :], axis=0),
                               in_=hb[:, :], in_offset=None, bounds_check=breg_slot, oob_is_err=False)
                    S.indirect(R=[mr, ski], out=io['meta'], out_offset=bass.IndirectOffsetOnAxis(ap=ski[:, :], axis=0),
                               in_=mr[:, :], in_offset=None, bounds_check=breg_slot, oob_is_err=False)
            for i2 in range(NT):
                S.pe('matmul', R=[onesb, Mall], W=[ppos], out=ppos[:, 0:32], lhsT=onesb[:], rhs=Mall[:, i2, :],
                     start=(i2 == 0), stop=(i2 == NT - 1))
            S.dve('tensor_copy', R=[ppos], W=[psl], out=psl[:], in_=ppos[:, 0:32])
            S.dma('sp', io['cnt'], psl[0:1, :], R=[psl])
        S.barrier()
        with ExitStack() as e2:
            wb = [{'g': S.sbuf(e2, 'weg', [128, 16, DEXP], BF16), 'u': S.sbuf(e2, 'weu', [128, 16, DEXP], BF16),
                   'd': S.sbuf(e2, 'wed', [128, 4, D], BF16)} for _ in range(2)]
            xbs = [S.sbuf(e2, 'xb', [128, D], BF16) for _ in range(2)]
            mts = [S.sbuf(e2, 'mt', [128, 16], F32) for _ in range(2)]
            idxs = [S.sbuf(e2, 'idx', [128, 1], I32) for _ in range(2)]
            xTs = [S.sbuf(e2, 'xT', [128, 16, 128], BF16) for _ in range(2)]
            sgs = [S.sbuf(e2, 'sg', [128, DEXP], F32) for _ in range(2)]
            abs_ = [S.sbuf(e2, 'ab', [128, DEXP], BF16) for _ in range(2)]
            aTs = [S.sbuf(e2, 'aT', [128, 4, 128], BF16) for _ in range(2)]
            ybs = [S.sbuf(e2, 'yb', [128, D], F32) for _ in range(2)]
            tpb = [S.psum(e2, 'tpb', [128, 1024], BF16) for _ in range(2)]
            pg = S.psum(e2, 'pg', [128, 512], F32)
            pu = S.psum(e2, 'pu', [128, 512], F32)
            py = [S.psum(e2, 'py', [128, 512], F32) for _ in range(2)]

            def load_w(e):
                W_ = wb[e % 2]
                S.dma('pool', W_['g'][:], io['w_eg'][e].rearrange('(k p) f -> p k f', p=128), W=[W_['g']])
                S.dma('pool', W_['u'][:], io['w_eu'][e].rearrange('(k p) f -> p k f', p=128), W=[W_['u']])
                S.dma('pool', W_['d'][:], io['w_ed'][e].rearrange('(k p) n -> p k n', p=128), W=[W_['d']])
            tpa = S.psum(e2, 'tpa', [128, 512], BF16)
            blocks = [(e, r) for e in range(NEXP) for r in range(NB)]

            def bufs(bi):
                b = bi % 2
                return xbs[b], mts[b], idxs[b], xTs[b], sgs[b], abs_[b], aTs[b], ybs[b]

            def stage_T(bi):
                e, r = blocks[bi]
                xb, mt, idx, xT, sg, ab, aT, yb = bufs(bi)
                r0 = e * CAP + r * 128
                S.dma('sp', xb[:], io['Xs'][r0:r0 + 128, :], W=[xb])
                S.dma('sp', mt[:], io['meta'][r0:r0 + 128, :], W=[mt])
                for half in range(2):
                    for k in range(8):
                        kk = half * 8 + k
                        S.pe('transpose', R=[xb, c['idb']], W=[tpb[half]], out=tpb[half][:, k * 128:(k + 1) * 128],
                             in_=xb[:, kk * 128:(kk + 1) * 128], identity=c['idb'][:])
                    src = tpb[half][:, :].rearrange('p (k t) -> p k t', k=8)
                    if half == 0:
                        S.act(R=[tpb[half]], W=[xT], out=xT[:, 0:8, :], in_=src, func=AF.Copy)
                    else:
                        S.dve('tensor_copy', R=[tpb[half]], W=[xT], out=xT[:, 8:16, :], in_=src)

            def stage_GU(bi):
                e, r = blocks[bi]
                W_ = wb[e % 2]
                xb, mt, idx, xT, sg, ab, aT, yb = bufs(bi)
                for k in range(16):
                    S.pe('matmul', R=[xT, W_['g']], W=[pg], out=pg[:, 0:DEXP], lhsT=xT[:, k, :], rhs=W_['g'][:, k, :],
                         start=(k == 0), stop=(k == 15))
                for k in range(16):
                    S.pe('matmul', R=[xT, W_['u']], W=[pu], out=pu[:, 0:DEXP], lhsT=xT[:, k, :], rhs=W_['u'][:, k, :],
                         start=(k == 0), stop=(k == 15))
                S.act(R=[pg], W=[sg], out=sg[:], in_=pg[:, 0:DEXP], func=AF.Silu)
                S.dve('tensor_tensor', R=[sg, pu], W=[ab], out=ab[:], in0=sg[:], in1=pu[:, 0:DEXP], op=ALU.mult)

            def stage_DN(bi):
                e, r = blocks[bi]
                W_ = wb[e % 2]
                xb, mt, idx, xT, sg, ab, aT, yb = bufs(bi)
                for fc in range(4):
                    S.pe('transpose', R=[ab, c['idb']], W=[tpa], out=tpa[:, fc * 128:(fc + 1) * 128],
                         in_=ab[:, fc * 128:(fc + 1) * 128], identity=c['idb'][:])
                S.dve('tensor_copy', R=[tpa], W=[aT], out=aT[:], in_=tpa[:, 0:512].rearrange('p (k t) -> p k t', k=4))
                for cb in range(4):
                    py_ = py[cb % 2]
                    cs_ = slice(cb * 512, (cb + 1) * 512)
                    for fc in range(4):
                        S.pe('matmul', R=[aT, W_['d']], W=[py_], out=py_[:, 0:512], lhsT=aT[:, fc, :], rhs=W_['d'][:, fc, cs_],
                             start=(fc == 0), stop=(fc == 3))
                    if cb % 2 == 0:
                        S.act(R=[py_, mt], W=[yb], out=yb[:, cs_], in_=py_[:, 0:512], func=AF.Copy, scale=mt[:, 1:2])
                    else:
                        S.dve('tensor_scalar', R=[py_, mt], W=[yb], out=yb[:, cs_], in0=py_[:, 0:512], scalar1=mt[:, 1:2],
                              scalar2=None, op0=ALU.mult)
                S.dve('tensor_copy', R=[mt], W=[idx], out=idx[:], in_=mt[:, 0:1])
                for hh in range(2):
                    S.indirect(R=[yb, idx], out=io['Yt'][hh], out_offset=bass.IndirectOffsetOnAxis(ap=idx[:, :], axis=0),
                               in_=yb[:, hh * 1024:(hh + 1) * 1024], in_offset=None, bounds_check=breg_tok,
                               oob_is_err=False)

            load_w(0)
            stage_T(0)
            for bi, (e, r) in enumerate(blocks):
                if r == 0 and e + 1 < NEXP:
                    load_w(e + 1)
                stage_GU(bi)
                if bi + 1 < len(blocks):
                    stage_T(bi + 1)
                stage_DN(bi)
        S.barrier()
        with ExitStack() as e3:
            yts = [S.sbuf(e3, 'yt', [128, 2, D], F32) for _ in range(2)]
            xs_ = [S.sbuf(e3, 'xc', [128, D], F32) for _ in range(2)]
            for i in range(NT):
                yt, xc = yts[i % 2], xs_[i % 2]
                for hh in range(2):
                    S.dma('sp', yt[:, :, hh * 1024:(hh + 1) * 1024],
                          io['Yt'][hh][i * 256:(i + 1) * 256, :].rearrange('(p k) d -> p k d', k=2), W=[yt])
                S.dma('sp', xc[:], io['x2'][i * 128:(i + 1) * 128, :], W=[xc])
                S.dve('tensor_tensor', R=[xc, yt], W=[xc], out=xc[:], in0=xc[:], in1=yt[:, 0, :], op=ALU.add)
                S.pool('tensor_tensor', R=[xc, yt], W=[xc], out=xc[:], in0=xc[:], in1=yt[:, 1, :], op=ALU.add)
                S.dma('sp', io['xo'][i * 128:(i + 1) * 128, :], xc[:], R=[xc])
    S.barrier()
```
